# Optimizing a Trainium2 kernel written in Bass

```python
import math
import jax, jax.numpy as jnp
from jax import lax
import numpy as np

D_MODEL = 4096
BATCH = 4
SEQ = 4096
DEPTH = 1

CHUNK = 64
Q_BLOCK = 128
A_HEADS = 16
A_HEAD_DIM = 128
A_KV_DIM = 128
IDX_HEADS = 32
IDX_DIM = 128
TOPK_MAX = 256
B_HEADS = 16
Q_LORA = 1024
KV_LORA = 512
QK_NOPE = 128
QK_ROPE = 64
V_DIM = 128
ROPE_THETA = 10000.0
D_FF = 4 * D_MODEL
EPS = 1e-6

A_WIDTH = A_HEADS * A_HEAD_DIM
B_WIDTH = B_HEADS * V_DIM
MIX_WIDTH = A_WIDTH + B_WIDTH
IN_SPLITS = (A_WIDTH, A_KV_DIM, A_KV_DIM, IDX_HEADS * IDX_DIM, IDX_DIM, IDX_HEADS, Q_LORA, KV_LORA, QK_ROPE)
IN_WIDTH = A_WIDTH + 2 * A_KV_DIM + IDX_HEADS * IDX_DIM + IDX_DIM + IDX_HEADS + Q_LORA + KV_LORA + QK_ROPE

kernel_name = 'hybrid_dsa_mla_sqrelu_adaln_block'


def rmsnorm(x, g):
    xf = x.astype(jnp.float32)
    y = xf * lax.rsqrt(jnp.mean(xf * xf, axis=-1, keepdims=True) + EPS)
    return (y * g.astype(jnp.float32)).astype(x.dtype)


def alibi_slopes():
    return 2.0 ** (-8.0 * jnp.arange(1, A_HEADS + 1, dtype=jnp.float32) / A_HEADS)


def rope_tables(pos):
    inv = ROPE_THETA ** (-jnp.arange(0, QK_ROPE, 2, dtype=jnp.float32) / QK_ROPE)
    ang = pos.astype(jnp.float32)[..., None] * inv
    return jnp.cos(ang), jnp.sin(ang)


def apply_rope(x, cos, sin):
    x1, x2 = jnp.split(x.astype(jnp.float32), 2, axis=-1)
    out = jnp.concatenate([x1 * cos - x2 * sin, x1 * sin + x2 * cos], axis=-1)
    return out.astype(x.dtype)


def to_blocks(a):
    b, s = a.shape[0], a.shape[1]
    a = a.reshape((b, s // Q_BLOCK, Q_BLOCK) + a.shape[2:])
    return jnp.moveaxis(a, 1, 0)


def from_blocks(a):
    a = jnp.moveaxis(a, 0, 1)
    return a.reshape((a.shape[0], a.shape[1] * a.shape[2]) + a.shape[3:])


def sparse_indexer_attention(q, k, v, q_idx, k_idx, w_idx, pos):
    b, s = pos.shape
    topk = min(TOPK_MAX, s // 4)
    chunk_k = pos // CHUNK
    slopes = alibi_slopes()

    def block(xs):
        qb, qib, wb, pb = xs
        admissible = chunk_k[:, None, :] <= (pb // CHUNK)[:, :, None]
        logits = jnp.einsum('bqhd,bsd->bqhs', qib, k_idx).astype(jnp.float32) * (IDX_DIM ** -0.5)
        wts = wb.astype(jnp.float32) * (IDX_HEADS ** -0.5)
        score = jnp.einsum('bqh,bqhs->bqs', wts, jax.nn.relu(logits))
        score = jnp.where(admissible, score, -jnp.inf)
        vals, idx = lax.top_k(score, topk)
        valid = jnp.isfinite(vals)
        flat = idx.reshape(b, -1)
        k_sel = jnp.take_along_axis(k, flat[..., None], axis=1).reshape(b, Q_BLOCK, topk, A_KV_DIM)
        v_sel = jnp.take_along_axis(v, flat[..., None], axis=1).reshape(b, Q_BLOCK, topk, A_KV_DIM)
        p_sel = jnp.take_along_axis(pos, flat, axis=1).reshape(b, Q_BLOCK, topk)
        dist = jnp.abs(pb[:, :, None] - p_sel).astype(jnp.float32)
        sc = jnp.einsum('bqhd,bqkd->bhqk', qb, k_sel).astype(jnp.float32) * (A_HEAD_DIM ** -0.5)
        sc = sc - slopes[None, :, None, None] * dist[:, None]
        sc = jnp.where(valid[:, None], sc, -jnp.inf)
        p = jax.nn.softmax(sc, axis=-1).astype(v.dtype)
        return jnp.einsum('bhqk,bqkd->bqhd', p, v_sel)

    out = lax.map(block, (to_blocks(q), to_blocks(q_idx), to_blocks(w_idx), to_blocks(pos)))
    return from_blocks(out)


def mla_attention(q_nope, q_rope, k_nope, k_rope, v, pos):
    chunk_k = pos // CHUNK
    scale = (QK_NOPE + QK_ROPE) ** -0.5

    def block(xs):
        qn, qr, pb = xs
        admissible = chunk_k[:, None, :] <= (pb // CHUNK)[:, :, None]
        sc = jnp.einsum('bqhd,bshd->bhqs', qn, k_nope) + jnp.einsum('bqhr,bsr->bhqs', qr, k_rope)
        sc = jnp.where(admissible[:, None], sc.astype(jnp.float32) * scale, -jnp.inf)
        p = jax.nn.softmax(sc, axis=-1).astype(v.dtype)
        return jnp.einsum('bhqs,bshd->bqhd', p, v)

    out = lax.map(block, (to_blocks(q_nope), to_blocks(q_rope), to_blocks(pos)))
    return from_blocks(out)


def setup_inputs(seed: int = 0) -> dict:
    key = jax.random.key(seed)
    ks = jax.random.split(key, 20)
    f32 = jnp.float32

    def nrm(k, shape, scale):
        return jax.random.normal(k, shape, f32) * scale

    def gain(k, shape):
        return 1.0 + 0.1 * jax.random.normal(k, shape, f32)

    offsets = jax.random.randint(ks[2], (BATCH,), 0, 16) * CHUNK
    positions = (offsets[:, None] + jnp.arange(SEQ, dtype=jnp.int32)[None, :]).astype(jnp.int32)
    return {
        'x': nrm(ks[0], (BATCH, SEQ, D_MODEL), 1.0),
        'c': nrm(ks[1], (BATCH, D_MODEL), 1.0),
        'positions': positions,
        'w_ada': nrm(ks[3], (DEPTH, D_MODEL, 6 * D_MODEL), 0.3 * D_MODEL ** -0.5),
        'b_ada': nrm(ks[4], (DEPTH, 6 * D_MODEL), 0.1),
        'ln1_g': gain(ks[5], (DEPTH, D_MODEL)),
        'w_in': nrm(ks[6], (DEPTH, D_MODEL, IN_WIDTH), D_MODEL ** -0.5),
        'q_norm_g': gain(ks[7], (DEPTH, Q_LORA)),
        'kv_norm_g': gain(ks[8], (DEPTH, KV_LORA)),
        'w_uq': nrm(ks[9], (DEPTH, Q_LORA, B_HEADS * (QK_NOPE + QK_ROPE)), Q_LORA ** -0.5),
        'w_uk': nrm(ks[10], (DEPTH, KV_LORA, B_HEADS * QK_NOPE), KV_LORA ** -0.5),
        'w_uv': nrm(ks[11], (DEPTH, KV_LORA, B_HEADS * V_DIM), KV_LORA ** -0.5),
        'w_o': nrm(ks[12], (DEPTH, MIX_WIDTH, D_MODEL), MIX_WIDTH ** -0.5),
        'ln2_g': gain(ks[13], (DEPTH, D_MODEL)),
        'w_mlp_in': nrm(ks[14], (DEPTH, D_MODEL, D_FF), D_MODEL ** -0.5),
        'w_mlp_out': nrm(ks[15], (DEPTH, D_FF, D_MODEL), D_FF ** -0.5),
        'final_g': gain(ks[16], (D_MODEL,)),
    }


def reference(x, c, positions, w_ada, b_ada, ln1_g, w_in, q_norm_g, kv_norm_g, w_uq, w_uk, w_uv,
              w_o, ln2_g, w_mlp_in, w_mlp_out, final_g):
    b, s, _ = x.shape
    split_points = np.cumsum(IN_SPLITS)[:-1].tolist()
    cos, sin = rope_tables(positions)
    for l in range(DEPTH):
        mod = jnp.einsum('bd,de->be', jax.nn.silu(c), w_ada[l]) + b_ada[l]
        shift1, scale1, gate1, shift2, scale2, gate2 = jnp.split(mod, 6, axis=-1)

        h = rmsnorm(x, ln1_g[l]) * (1.0 + scale1[:, None]) + shift1[:, None]
        proj = jnp.einsum('bsd,de->bse', h, w_in[l])
        q_a, k_a, v_a, q_i, k_i, w_i, c_q, c_kv, k_r = jnp.split(proj, split_points, axis=-1)

        out_a = sparse_indexer_attention(
            q_a.reshape(b, s, A_HEADS, A_HEAD_DIM), k_a, v_a,
            q_i.reshape(b, s, IDX_HEADS, IDX_DIM), k_i, w_i, positions)

        q_full = jnp.einsum('bsr,re->bse', rmsnorm(c_q, q_norm_g[l]), w_uq[l])
        q_full = q_full.reshape(b, s, B_HEADS, QK_NOPE + QK_ROPE)
        q_nope, q_rope = q_full[..., :QK_NOPE], q_full[..., QK_NOPE:]
        q_rope = apply_rope(q_rope, cos[:, :, None], sin[:, :, None])
        kv_lat = rmsnorm(c_kv, kv_norm_g[l])
        k_nope = jnp.einsum('bsr,re->bse', kv_lat, w_uk[l]).reshape(b, s, B_HEADS, QK_NOPE)
        v_b = jnp.einsum('bsr,re->bse', kv_lat, w_uv[l]).reshape(b, s, B_HEADS, V_DIM)
        k_rope = apply_rope(k_r, cos, sin)
        out_b = mla_attention(q_nope, q_rope, k_nope, k_rope, v_b, positions)

        mix = jnp.concatenate([out_a.reshape(b, s, A_WIDTH), out_b.reshape(b, s, B_WIDTH)], axis=-1)
        x = x + gate1[:, None] * jnp.einsum('bse,ed->bsd', mix, w_o[l])

        h2 = rmsnorm(x, ln2_g[l]) * (1.0 + scale2[:, None]) + shift2[:, None]
        hid = jnp.square(jax.nn.relu(jnp.einsum('bsd,df->bsf', h2, w_mlp_in[l])))
        x = x + gate2[:, None] * jnp.einsum('bsf,fd->bsd', hid, w_mlp_out[l])
    return rmsnorm(x, final_g)
```

```python
import math
from contextlib import ExitStack
import numpy as np
import concourse.bass as bass
import concourse.mybir as mybir
from concourse.bass_utils import run_bass_kernel_spmd

F32 = mybir.dt.float32
BF16 = mybir.dt.bfloat16
I32 = mybir.dt.int32
AF = mybir.ActivationFunctionType
ALU = mybir.AluOpType
AX = mybir.AxisListType

ENGS = ("pe", "act", "dve", "pool", "sp")
SAME_ENGINE_SYNC = True
D = 4096
EPS = 1e-6
NBIS = 20


import os
MAXSTAGE = int(os.environ.get("KSTAGE", "99"))
DBG_OUT = set(filter(None, os.environ.get("KDBG", "").split(",")))


class StopBuild(Exception):
    pass


class Buf:
    __slots__ = ("writers", "readers")

    def __init__(self):
        self.writers = []
        self.readers = []


class Op:
    __slots__ = ("eng", "fn", "deps", "is_dma", "seq", "signals", "dsem", "dtarget", "emitted", "touch")

    def __init__(self, eng, fn, is_dma):
        self.eng = eng
        self.fn = fn
        self.deps = []
        self.is_dma = is_dma
        self.seq = None
        self.signals = False
        self.dsem = None
        self.dtarget = None
        self.emitted = False
        self.touch = False


class Prog:
    def __init__(self, nc, stack, n_dma_sems=10):
        self.nc = nc
        self.pending = {e: [] for e in ENGS}
        self.stage_deps = {e: [] for e in ENGS}
        self.csem = {e: stack.enter_context(nc.semaphore("c_" + e)) for e in ENGS}
        self.dsems = {e: [stack.enter_context(nc.semaphore("d_%s_%d" % (e, i))) for i in range(n_dma_sems)]
                      for e in ("sp", "pool")}
        self.cnt = {e: 0 for e in ENGS}
        self.dma_k = {e: 0 for e in self.dsems}
        self.dma_uses = {e: [0] * n_dma_sems for e in self.dsems}
        self.waited = {e: {} for e in ENGS}
        self.stage_no = 0

    def add(self, eng, fn, reads=(), writes=(), dma=False, extra_deps=()):
        op = Op(eng, fn, dma)
        deps = []
        for b in reads:
            deps.extend(b.writers)
        for b in writes:
            deps.extend(b.writers)
            deps.extend(b.readers)
        deps.extend(extra_deps)
        if self.stage_deps[eng]:
            deps.extend(self.stage_deps[eng])
            self.stage_deps[eng] = []
        seen = set()
        for d in deps:
            if d is op or id(d) in seen or (d.emitted and not d.touch):
                continue
            seen.add(id(d))
            op.deps.append(d)
            if not d.is_dma and not (d.eng == eng and (eng == "pe" or not SAME_ENGINE_SYNC)):
                d.signals = True
        for b in reads:
            b.readers.append(op)
            if len(b.readers) > 48:
                b.readers = b.readers[-48:]
        for b in writes:
            if b.readers:
                b.readers = []
                b.writers = [op]
            else:
                b.writers.append(op)
                if len(b.writers) > 48:
                    b.writers = b.writers[-48:]
        self.pending[eng].append(op)
        return op

    def pe(self, fn, reads=(), writes=(), **kw):
        return self.add("pe", fn, reads, writes, **kw)

    def act(self, fn, reads=(), writes=(), **kw):
        return self.add("act", fn, reads, writes, **kw)

    def dve(self, fn, reads=(), writes=(), **kw):
        return self.add("dve", fn, reads, writes, **kw)

    def pool(self, fn, reads=(), writes=(), **kw):
        return self.add("pool", fn, reads, writes, **kw)

    def dma(self, q, out, in_, reads=(), writes=(), slow=False, **kw):
        if slow:
            fn = lambda e: e.dma_start(out=out, in_=in_, allow_slow_non_contiguous=True)
        else:
            fn = lambda e: e.dma_start(out=out, in_=in_)
        return self.add(q, fn, reads, writes, dma=True, **kw)

    def end_stage(self, touch):
        nc = self.nc
        tops = []
        for e in ("act", "dve", "pool", "sp"):
            t = Op(e, touch[e], e == "sp")
            t.signals = True
            t.touch = True
            self.pending[e].append(("T", t))
            tops.append(t)
        for e in ENGS:
            for item in self.pending[e]:
                op = item[1] if isinstance(item, tuple) else item
                if op.is_dma:
                    i = self.dma_k[e] % len(self.dsems[e])
                    self.dma_k[e] += 1
                    self.dma_uses[e][i] += 1
                    op.dsem = (e, i)
                    op.dtarget = 16 * self.dma_uses[e][i]
                elif op.signals:
                    self.cnt[e] += 1
                    op.seq = self.cnt[e]
        with nc.Block() as block:
            engmap = {"pe": block.tensor, "act": block.scalar, "dve": block.vector, "pool": block.gpsimd,
                      "sp": block.sync}

            def make(ename):
                items = self.pending[ename]
                waited = self.waited[ename]

                def body(eng):
                    def wait(key, sem, val):
                        if waited.get(key, 0) >= val:
                            return
                        waited[key] = val
                        eng.wait_ge(sem, val)

                    def wait_all_dma():
                        if ename in self.dsems:
                            for i, s in enumerate(self.dsems[ename]):
                                tot = 16 * self.dma_uses[ename][i]
                                if tot:
                                    wait(("d", ename, i), s, tot)

                    for item in items:
                        if isinstance(item, tuple):
                            op = item[1]
                            if ename in self.dsems:
                                for i, s in enumerate(self.dsems[ename]):
                                    tot = 16 * self.dma_uses[ename][i]
                                    if op.is_dma and op.dsem == (ename, i):
                                        tot -= 16
                                    if tot:
                                        wait(("d", ename, i), s, tot)
                        else:
                            op = item
                        if op.is_dma and op.dtarget > 16:
                            wait(("d",) + op.dsem, self.dsems[op.dsem[0]][op.dsem[1]], op.dtarget - 16)
                        for d in op.deps:
                            if d.is_dma:
                                wait(("d",) + d.dsem, self.dsems[d.dsem[0]][d.dsem[1]], d.dtarget)
                            else:
                                if d.eng == ename and (ename == "pe" or not SAME_ENGINE_SYNC):
                                    continue
                                wait(("c", d.eng), self.csem[d.eng], d.seq)
                        ins = op.fn(eng)
                        if op.is_dma:
                            ins.then_inc(self.dsems[op.dsem[0]][op.dsem[1]], 16)
                        elif op.signals:
                            ins.then_inc(self.csem[ename], 1)
                    wait_all_dma()

                return body

            for e in ENGS:
                if self.pending[e]:
                    engmap[e](make(e))
        for e in ENGS:
            for item in self.pending[e]:
                (item[1] if isinstance(item, tuple) else item).emitted = True
        self.pending = {e: [] for e in ENGS}
        for e in ENGS:
            self.stage_deps[e] = list(tops)
        self.stage_no += 1
        if self.stage_no > MAXSTAGE:
            raise StopBuild()


def build_nc():
    nc = bass.Bass("TRN2", target_bir_lowering=False)

    def inp(name, shape, dt=F32):
        return nc.dram_tensor(name, list(shape), dt, kind="ExternalInput").ap()

    _uc = [0]

    def sbt(name, shape, dt):
        _uc[0] += 1
        return nc.sbuf_tensor("%s_u%d" % (name, _uc[0]), shape, dt)

    def scr(name, shape, dt):
        return nc.dram_tensor(name, list(shape), dt, kind=("ExternalOutput" if name in DBG_OUT else "Internal")).ap()

    xc = inp("xc", [4096, 4096])
    posr = inp("posr", [1, 4096], I32)
    posc = inp("posc", [128, 32], I32)
    cT = inp("cT", [128, 32])
    w_ada = inp("w_ada", [4096, 24576])
    b_adaT = inp("b_adaT", [128, 192])
    g1T = inp("g1T", [128, 32])
    w_in = inp("w_in", [4096, 8160])
    qgT = inp("qgT", [128, 8])
    kvgT = inp("kvgT", [128, 4])
    w_uq = inp("w_uq", [1024, 3072])
    w_uk = inp("w_uk", [512, 2048])
    w_uv = inp("w_uv", [512, 2048])
    w_o = inp("w_o", [4096, 4096])
    g2T = inp("g2T", [128, 32])
    w1 = inp("w1", [4096, 16384])
    w2 = inp("w2", [16384, 4096])
    fgT = inp("fgT", [128, 32])
    y = nc.dram_tensor("y", [2048, 4096], F32, kind="ExternalOutput").ap()

    h1T = scr("h1T", [4096, 4096], BF16)
    xT = scr("xT", [4096, 2048], F32)
    kaT = scr("kaT", [128, 4096], BF16)
    va = scr("va", [4096, 128], BF16)
    kiT = scr("kiT", [128, 4096], BF16)
    krT = scr("krT", [64, 4096], BF16)
    kvnT = scr("kvnT", [512, 4096], BF16)
    knT = scr("knT", [2048, 4096], BF16)
    vb = scr("vb", [4096, 2048], BF16)
    qaT = scr("qaT", [2048, 2048], BF16)
    qiT = scr("qiT", [4096, 2048], BF16)
    wi3 = scr("wi3", [16 * 32 * 128], F32)
    cqnT = scr("cqnT", [1024, 2048], BF16)
    qnT = scr("qnT", [2048, 2048], BF16)
    qrT = scr("qrT", [16 * 64, 2048], BF16)
    mixT = scr("mixT", [4096, 2048], BF16)
    x2T = scr("x2T", [4096, 2048], F32)
    h2T = scr("h2T", [4096, 2048], BF16)
    hidT = scr("hidT", [16384, 2048], BF16)
    x3T = scr("x3T", [4096, 2048], F32)
    tdr = scr("tdr", [1, 64], F32)

    try:
        _build_body(nc, locals())
    except StopBuild:
        pass
    return nc


def _build_body(nc, L):
    globals().update({k: v for k, v in L.items() if k not in ("nc",)})
    with ExitStack() as gst:
        P = Prog(nc, gst)

        def gsb(name, shape, dt):
            return gst.enter_context(sbt(name, list(shape), dt))

        ident = gsb("ident", [128, 128], F32)
        ones_bf = gsb("ones_bf", [128, 128], BF16)
        modT = gsb("modT", [128, 192], F32)
        gs1 = gsb("gs1", [128, 32], F32)
        gs2 = gsb("gs2", [128, 32], F32)
        fg = gsb("fg", [128, 32], F32)
        kvg = gsb("kvg", [128, 4], F32)
        qg = gsb("qg", [128, 8], F32)
        posf = gsb("posf", [128, 32], F32)
        chkf = gsb("chkf", [128, 32], F32)
        cosT = gsb("cosT", [128, 32, 32], F32)
        sinT = gsb("sinT", [128, 32, 32], F32)
        r2b = gsb("r2b", [128, 2048], F32)
        rstd3 = gsb("rstd3", [128, 16], F32)
        tch = gsb("tch", [128, 8], F32)
        B_const = Buf()
        B_mod = Buf()
        B_r2b = Buf()
        B_r3 = Buf()
        banks = [gst.enter_context(nc.psum_tensor("bank%d" % i, [128, 512], F32)) for i in range(8)]
        BK = [Buf() for _ in range(8)]
        sh1 = modT[:, 0:32]
        gate1 = modT[:, 64:96]
        sh2 = modT[:, 96:128]
        gate2 = modT[:, 160:192]

        touch = {
            "act": lambda e: e.activation(out=tch[0:1, 0:1], in_=tch[0:1, 1:2], func=AF.Copy),
            "dve": lambda e: e.tensor_copy(out=tch[0:1, 2:3], in_=tch[0:1, 3:4]),
            "pool": lambda e: e.memset(tch[0:1, 4:5], 0.0),
            "sp": lambda e: e.dma_start(out=tdr[0:1, 0:2], in_=tch[0:1, 6:8]),
        }

        def rsqrt_ops(dst, src, scale, reads, writes):
            P.dve(lambda e: e.tensor_scalar(out=dst, in0=src, scalar1=scale, scalar2=EPS, op0=ALU.mult, op1=ALU.add),
                  reads=reads, writes=writes)
            P.act(lambda e: e.activation(out=dst, in_=dst, func=AF.Sqrt), reads=writes, writes=writes)
            P.dve(lambda e: e.reciprocal(out=dst, in_=dst), reads=writes, writes=writes)

        with ExitStack() as st:
            def sb(name, shape, dt):
                return st.enter_context(sbt(name, list(shape), dt))

            io_i = sb("io_i", [128, 128], I32)
            P.pool(lambda e: e.iota(io_i[:], pattern=[[1, 128]], base=0, channel_multiplier=-1), writes=[B_const])
            P.dve(lambda e: e.tensor_copy(out=ident[:], in_=io_i[:]), reads=[B_const], writes=[B_const])
            P.dve(lambda e: e.tensor_scalar(out=ident[:], in0=ident[:], scalar1=0.0, scalar2=None, op0=ALU.is_equal),
                  reads=[B_const], writes=[B_const])
            P.pool(lambda e: e.memset(ones_bf[:], 1.0), writes=[B_const])
            P.pool(lambda e: e.memset(tch[:], 0.0), writes=[B_const])
            Bp = Buf()
            ct_sb = sb("ct_sb", [128, 32], F32)
            badaT = sb("badaT", [128, 192], F32)
            g1s = sb("g1s", [128, 32], F32)
            g2s = sb("g2s", [128, 32], F32)
            posc_i = sb("posc_i", [128, 32], I32)
            chk_i = sb("chk_i", [128, 32], I32)
            for (dst, src) in ((ct_sb, cT), (badaT, b_adaT), (g1s, g1T), (g2s, g2T), (fg, fgT), (kvg, kvgT),
                               (qg, qgT), (posc_i, posc)):
                P.dma("sp", dst[:], src, writes=[Bp])
            P.dve(lambda e: e.tensor_copy(out=posf[:], in_=posc_i[:]), reads=[Bp], writes=[B_const])
            P.dve(lambda e: e.tensor_scalar(out=chk_i[:], in0=posc_i[:], scalar1=6, scalar2=None,
                                            op0=ALU.arith_shift_right), reads=[Bp], writes=[Bp])
            P.dve(lambda e: e.tensor_copy(out=chkf[:], in_=chk_i[:]), reads=[Bp], writes=[B_const])
            inv_i = sb("inv_i", [128, 32], I32)
            invf = sb("invf", [128, 32], F32)
            for i_ in range(32):
                P.pool(lambda e, i_=i_: e.memset(invf[:, i_:i_ + 1], float(np.float32(10000.0) ** np.float32(-2.0 * i_ / 64.0))),
                       writes=[Bp])
            rr = sb("rr", [128, 32, 32], F32)
            ri2 = [sb("ri%d" % i_, [128, 32, 32], I32) for i_ in range(2)]
            rf2 = [sb("rf%d" % i_, [128, 32, 32], F32) for i_ in range(2)]
            Br = Buf()
            for blk in range(32):
                P.dve(lambda e, blk=blk: e.tensor_scalar(out=rr[:, blk, :], in0=invf[:], scalar1=posf[:, blk:blk + 1],
                                                         scalar2=1.0 / (2 * math.pi), op0=ALU.mult, op1=ALU.mult),
                      reads=[Bp, B_const], writes=[Br])
            Bt = Buf()
            for (tab, off) in ((cosT, 0.25), (sinT, 0.0)):
                rrf = rr[:].rearrange("p a b -> p (a b)")
                rif = ri2[0 if off else 1][:].rearrange("p a b -> p (a b)")
                rff = rf2[0 if off else 1][:].rearrange("p a b -> p (a b)")
                tabf = tab[:].rearrange("p a b -> p (a b)")
                P.dve(lambda e, off=off, rff=rff, rrf=rrf: e.tensor_scalar(out=rff, in0=rrf, scalar1=off, scalar2=None,
                                                                           op0=ALU.add), reads=[Br], writes=[Bt])
                P.dve(lambda e, rff=rff, rif=rif: e.tensor_copy(out=rif, in_=rff), reads=[Bt], writes=[Bt])
                P.dve(lambda e, tabf=tabf, rif=rif: e.tensor_copy(out=tabf, in_=rif), reads=[Bt], writes=[B_const])
                P.dve(lambda e, rff=rff, tabf=tabf: e.tensor_tensor(out=rff, in0=rff, in1=tabf, op=ALU.subtract),
                      reads=[Bt, B_const], writes=[Bt])
                P.dve(lambda e, rff=rff, tabf=tabf: e.tensor_scalar(out=tabf, in0=rff, scalar1=0.5, scalar2=None,
                                                                    op0=ALU.is_gt), reads=[Bt], writes=[B_const])
                P.dve(lambda e, rff=rff, tabf=tabf: e.tensor_tensor(out=rff, in0=rff, in1=tabf, op=ALU.subtract),
                      reads=[Bt, B_const], writes=[Bt])
                P.dve(lambda e, rff=rff, tabf=tabf: e.tensor_scalar(out=tabf, in0=rff, scalar1=-0.5, scalar2=None,
                                                                    op0=ALU.is_lt), reads=[Bt], writes=[B_const])
                P.dve(lambda e, rff=rff, tabf=tabf: e.tensor_tensor(out=rff, in0=rff, in1=tabf, op=ALU.add),
                      reads=[Bt, B_const], writes=[Bt])
                P.act(lambda e, rff=rff, tabf=tabf: e.activation(out=tabf, in_=rff, func=AF.Sin, scale=2 * math.pi),
                      reads=[Bt], writes=[B_const])
            if "dbgtab" in DBG_OUT:
                dbgtab = scr("dbgtab", [128, 3072], F32)
                P.dma("sp", dbgtab[:, 0:1024], cosT[:].rearrange("p a b -> p (a b)"), reads=[B_const, Bt])
                P.dma("sp", dbgtab[:, 1024:2048], sinT[:].rearrange("p a b -> p (a b)"), reads=[B_const, Bt])
                P.dma("sp", dbgtab[:, 2048:3072], rr[:].rearrange("p a b -> p (a b)"), reads=[Br, Bt])
            scT = sb("scT", [128, 32], BF16)
            P.act(lambda e: e.activation(out=scT[:], in_=ct_sb[:], func=AF.Silu), reads=[Bp], writes=[Bp])
            wts = [sb("wa%d" % i, [128, 32, 128], BF16) for i in range(3)]
            Bw = [Buf() for _ in range(3)]
            war = w_ada.rearrange("(k p) e -> p k e", p=128)
            for ec in range(192):
                s = ec % 3
                P.dma("pool", wts[s][:], war[:, :, ec * 128:(ec + 1) * 128], writes=[Bw[s]])
                for k in range(32):
                    P.pe(lambda e, s=s, k=k, ec=ec: e.matmul(banks[0][:, ec:ec + 1], lhsT=wts[s][:, k, :],
                                                             rhs=scT[:, k:k + 1], start=(k == 0), stop=(k == 31)),
                         reads=[Bw[s], Bp], writes=[BK[0]])
            P.dve(lambda e: e.tensor_tensor(out=modT[:], in0=banks[0][:, 0:192], in1=badaT[:], op=ALU.add),
                  reads=[BK[0], Bp], writes=[B_mod])
            P.dve(lambda e: e.scalar_tensor_tensor(out=gs1[:], in0=modT[:, 32:64], scalar=1.0, in1=g1s[:],
                                                   op0=ALU.add, op1=ALU.mult), reads=[B_mod, Bp], writes=[B_mod])
            P.dve(lambda e: e.scalar_tensor_tensor(out=gs2[:], in0=modT[:, 128:160], scalar=1.0, in1=g2s[:],
                                                   op0=ALU.add, op1=ALU.mult), reads=[B_mod, Bp], writes=[B_mod])
            P.end_stage(touch)

        with ExitStack() as st:
            def sb(name, shape, dt):
                return st.enter_context(sbt(name, list(shape), dt))

            xb = [sb("xb%d" % i, [128, 4096], F32) for i in range(2)]
            Bx = [Buf() for _ in range(2)]
            junk = sb("junk", [128, 4096], BF16)
            Bj = Buf()
            hst = [sb("hst%d" % i, [128, 32, 512], BF16) for i in range(2)]
            Bh = [Buf() for _ in range(2)]
            xst = [sb("xst%d" % i, [128, 32, 128], F32) for i in range(2)]
            Bxs = [Buf() for _ in range(2)]
            ssq = sb("ssq", [128, 32], F32)
            Bs = Buf()
            h1Tr = h1T.rearrange("(k p) t -> p k t", p=128)
            xTr = xT.rearrange("(k p) t -> p k t", p=128)
            bi = 0
            for blk in range(32):
                s = blk % 2
                x_ = xb[s]
                P.dma("sp", x_[:], xc[blk * 128:(blk + 1) * 128, :], writes=[Bx[s]])
                if blk < 16:
                    xs = xst[blk % 2]
                    for k in range(32):
                        b = bi % 2
                        bi_k = k % 4
                        P.pe(lambda e, x_=x_, k=k, b=b, bi_k=bi_k: e.transpose(
                            banks[b][:, bi_k * 128:(bi_k + 1) * 128], x_[:, k * 128:(k + 1) * 128], ident[:]),
                            reads=[Bx[s], B_const], writes=[BK[b]])
                        if bi_k == 3:
                            P.dve(lambda e, xs=xs, k=k, b=b: e.tensor_copy(
                                out=xs[:, k - 3:k + 1, :], in_=banks[b][:].rearrange("p (a t) -> p a t", a=4)),
                                reads=[BK[b]], writes=[Bxs[blk % 2]])
                            bi += 1
                    P.dma("sp", xTr[:, :, blk * 128:(blk + 1) * 128], xs[:], reads=[Bxs[blk % 2]])
                P.act(lambda e, x_=x_, blk=blk: e.activation(out=junk[:], in_=x_[:], func=AF.Square,
                                                             accum_out=ssq[:, blk:blk + 1]),
                      reads=[Bx[s]], writes=[Bj, Bs])
                rsqrt_ops(ssq[:, blk:blk + 1], ssq[:, blk:blk + 1], 1.0 / D, [Bs], [Bs])
                P.dve(lambda e, x_=x_, blk=blk: e.tensor_scalar(out=x_[:], in0=x_[:], scalar1=ssq[:, blk:blk + 1],
                                                                scalar2=None, op0=ALU.mult),
                      reads=[Bx[s], Bs], writes=[Bx[s]])
                hs = hst[(blk // 4) % 2]
                Bhs = Bh[(blk // 4) % 2]
                tb = blk % 4
                for k in range(32):
                    b = 2 + (bi % 2)
                    bi_k = k % 4
                    P.pe(lambda e, x_=x_, k=k, b=b, bi_k=bi_k: e.transpose(
                        banks[b][:, bi_k * 128:(bi_k + 1) * 128], x_[:, k * 128:(k + 1) * 128], ident[:]),
                        reads=[Bx[s], B_const], writes=[BK[b]])
                    if bi_k == 3:
                        for kk in range(k - 3, k + 1):
                            P.act(lambda e, hs=hs, kk=kk, b=b, tb=tb: e.activation(
                                out=hs[:, kk, tb * 128:(tb + 1) * 128], in_=banks[b][:, (kk % 4) * 128:(kk % 4 + 1) * 128],
                                func=AF.Identity, scale=gs1[:, kk:kk + 1], bias=sh1[:, kk:kk + 1]),
                                reads=[BK[b], B_mod], writes=[Bhs])
                        bi += 1
                if tb == 3:
                    t0 = (blk // 4) * 512
                    P.dma("sp", h1Tr[:, :, t0:t0 + 512], hs[:], reads=[Bhs])
            P.end_stage(touch)

        def run_gemm(st, XT, KC, n_tt, jobs, xbufs=1, pre_tt=None):
            xts = [st.enter_context(sbt("xt%d" % i, [128, KC, 512], BF16)) for i in range(xbufs)]
            Bxt = [Buf() for _ in range(xbufs)]
            XTr = XT.rearrange("(k p) t -> p k t", p=128)
            for tt in range(n_tt):
                s = tt % xbufs
                for k0 in range(0, KC, 32):
                    k1 = min(KC, k0 + 32)
                    P.dma("sp", xts[s][:, k0:k1, :], XTr[:, k0:k1, tt * 512:(tt + 1) * 512], writes=[Bxt[s]])
                if pre_tt is not None:
                    pre_tt(tt)
                for job in jobs:
                    job(xts[s], Bxt[s], tt)

        class WStream:
            def __init__(self, st, n=3):
                self.t = [st.enter_context(sbt("ws%d" % i, [128, 32, 128], BF16)) for i in range(n)]
                self.B = [Buf() for _ in range(n)]
                self.i = 0

            def load(self, W, r0, nk, c0, ncol):
                s = self.i % len(self.t)
                self.i += 1
                src = W[r0:r0 + nk * 128, c0:c0 + ncol].rearrange("(k p) e -> p k e", p=128)
                P.dma("pool", self.t[s][:, 0:nk, 0:ncol], src, writes=[self.B[s]])
                return self.t[s], self.B[s]

        gb = [0]

        def gbank():
            gb[0] += 1
            return gb[0] % 2

        def fm_job(ws, W, KC, c0, ncol, epi):
            def job(xt, Bxt, tt):
                b = gbank()
                for k0 in range(0, KC, 32):
                    nk = min(32, KC - k0)
                    wt, Bw = ws.load(W, k0 * 128, nk, c0, ncol)
                    for k in range(nk):
                        P.pe(lambda e, wt=wt, k=k, k0=k0, b=b: e.matmul(
                            banks[b][0:ncol, :], lhsT=wt[:, k, 0:ncol], rhs=xt[:, k0 + k, :],
                            start=(k0 + k == 0), stop=(k0 + k == KC - 1)), reads=[Bw, Bxt], writes=[BK[b]])
                epi(tt, b)
            return job

        with ExitStack() as st:
            def sb(name, shape, dt):
                return st.enter_context(sbt(name, list(shape), dt))

            ws = WStream(st)
            ost = [sb("ost%d" % i, [128, 512], BF16) for i in range(3)]
            Bo = [Buf() for _ in range(3)]
            oi = [0]

            def copy_out(dst_fn, scale=1.0):
                def epi(tt, b):
                    s = oi[0] % 3
                    oi[0] += 1
                    P.act(lambda e, s=s, b=b: e.activation(out=ost[s][:], in_=banks[b][:], func=AF.Copy, scale=scale),
                          reads=[BK[b]], writes=[Bo[s]])
                    P.dma("sp", dst_fn(tt), ost[s][:], reads=[Bo[s]])
                return epi

            ckv = sb("ckv", [128, 4, 512], F32)
            sqb = sb("sqb", [128, 4, 512], BF16)
            rb = sb("rb", [128, 512], F32)
            kvn = sb("kvn", [128, 4, 512], BF16)
            Bc = Buf()
            Bsq = Buf()
            Brb = Buf()
            Bkvn = Buf()
            kvnTr = kvnT.rearrange("(k p) t -> p k t", p=128)

            def ckv_epi(j):
                def epi(tt, b):
                    P.dve(lambda e, b=b: e.tensor_copy(out=ckv[:, j, :], in_=banks[b][:]), reads=[BK[b]], writes=[Bc])
                    P.act(lambda e, b=b: e.activation(out=sqb[:, j, :], in_=ckv[:, j, :], func=AF.Square),
                          reads=[Bc], writes=[Bsq])
                    if j == 3:
                        for jj in range(4):
                            P.pe(lambda e, jj=jj: e.matmul(banks[4][:], lhsT=ones_bf[:], rhs=sqb[:, jj, :],
                                                           start=(jj == 0), stop=(jj == 3)),
                                 reads=[Bsq, B_const], writes=[BK[4]])
                        rsqrt_ops(rb[:], banks[4][:], 1.0 / 512, [BK[4]], [Brb])
                        for jj in range(4):
                            P.dve(lambda e, jj=jj: e.scalar_tensor_tensor(
                                out=kvn[:, jj, :], in0=ckv[:, jj, :], scalar=kvg[:, jj:jj + 1], in1=rb[:],
                                op0=ALU.mult, op1=ALU.mult), reads=[Bc, Brb, Bp0], writes=[Bkvn])
                        P.dma("sp", kvnTr[:, :, tt * 512:(tt + 1) * 512], kvn[:], reads=[Bkvn])
                return epi

            Bp0 = B_const
            wtm = sb("wtm", [128, 32, 192], BF16)
            Bwtm = Buf()
            w_in_r = w_in.rearrange("(k p) e -> p k e", p=128)
            P.dma("pool", wtm[:, :, 0:128], w_in_r[:, :, 2176:2304], writes=[Bwtm])
            P.dma("pool", wtm[:, :, 128:192], w_in_r[:, :, 8096:8160], writes=[Bwtm])
            vst = [sb("vst%d" % i, [128, 128], BF16) for i in range(2)]
            Bv = [Buf() for _ in range(2)]
            krf = sb("krf", [128, 64], F32)
            kro = sb("kro", [128, 64], F32)
            tmp1 = sb("tmp1", [128, 32], F32)
            Bkr = Buf()
            krst = sb("krst", [64, 512], BF16)
            Bkrst = Buf()

            def rope_tok(e_list, src, dst, blk, nh, tmp):
                c = cosT[:, blk, :].unsqueeze(1).to_broadcast([128, nh, 32])
                s_ = sinT[:, blk, :].unsqueeze(1).to_broadcast([128, nh, 32])
                x1 = src[:, :, 0:32]
                x2 = src[:, :, 32:64]
                ops = [
                    (dst[:, :, 0:32], x1, c, ALU.mult), (tmp, x2, s_, ALU.mult),
                    (dst[:, :, 0:32], dst[:, :, 0:32], tmp, ALU.subtract),
                    (dst[:, :, 32:64], x1, s_, ALU.mult), (tmp, x2, c, ALU.mult),
                    (dst[:, :, 32:64], dst[:, :, 32:64], tmp, ALU.add)]
                for (o, a, b_, op) in ops:
                    P.dve(lambda e, o=o, a=a, b_=b_, op=op: e.tensor_tensor(out=o, in0=a, in1=b_, op=op),
                          reads=e_list[0], writes=e_list[1])

            def tm_job_k(xt, Bxt, tt):
                for tb in range(4):
                    blk = tt * 4 + tb
                    b = 2 + (tb % 2)
                    for k in range(32):
                        P.pe(lambda e, k=k, b=b, tb=tb: e.matmul(banks[b][:, 0:192], lhsT=xt[:, k, tb * 128:(tb + 1) * 128],
                                                                 rhs=wtm[:, k, :], start=(k == 0), stop=(k == 31)),
                             reads=[Bxt, Bwtm], writes=[BK[b]])
                    s = blk % 2
                    P.act(lambda e, s=s, b=b: e.activation(out=vst[s][:], in_=banks[b][:, 0:128], func=AF.Copy),
                          reads=[BK[b]], writes=[Bv[s]])
                    P.dma("sp", va[blk * 128:(blk + 1) * 128, :], vst[s][:], reads=[Bv[s]])
                    P.act(lambda e, b=b: e.activation(out=krf[:], in_=banks[b][:, 128:192], func=AF.Copy), reads=[BK[b]],
                          writes=[Bkr])
                    rope_tok(([Bkr, B_const], [Bkr]), krf[:].unsqueeze(1), kro[:].unsqueeze(1), blk, 1,
                             tmp1[:].unsqueeze(1))
                    P.pe(lambda e: e.transpose(banks[5][0:64, 0:128], kro[:], ident[:]), reads=[Bkr, B_const],
                         writes=[BK[5]])
                    P.act(lambda e, tb=tb: e.activation(out=krst[:, tb * 128:(tb + 1) * 128], in_=banks[5][0:64, 0:128],
                                                        func=AF.Copy), reads=[BK[5]], writes=[Bkrst])
                P.dma("sp", krT[:, tt * 512:(tt + 1) * 512], krst[:], reads=[Bkrst])

            jobs = [fm_job(ws, w_in, 32, 2048, 128, copy_out(lambda tt: kaT[:, tt * 512:(tt + 1) * 512])),
                    fm_job(ws, w_in, 32, 6400, 128, copy_out(lambda tt: kiT[:, tt * 512:(tt + 1) * 512]))]
            for j in range(4):
                jobs.append(fm_job(ws, w_in, 32, 7584 + 128 * j, 128, ckv_epi(j)))
            jobs.append(tm_job_k)
            _sub = int(os.environ.get("KSUB", "255"))
            jobs = [jb for n_, jb in enumerate(jobs) if (_sub >> n_) & 1]
            run_gemm(st, h1T, 32, 8, jobs, xbufs=2)
            P.end_stage(touch)

        with ExitStack() as st:
            def sb(name, shape, dt):
                return st.enter_context(sbt(name, list(shape), dt))

            ws = WStream(st)
            ost = [sb("ost%d" % i, [128, 512], BF16) for i in range(3)]
            Bo = [Buf() for _ in range(3)]
            oi = [0]

            def kn_epi(h):
                def epi(tt, b):
                    s = oi[0] % 3
                    oi[0] += 1
                    P.act(lambda e, s=s, b=b: e.activation(out=ost[s][:], in_=banks[b][:], func=AF.Copy),
                          reads=[BK[b]], writes=[Bo[s]])
                    P.dma("sp", knT[h * 128:(h + 1) * 128, tt * 512:(tt + 1) * 512], ost[s][:], reads=[Bo[s]])
                return epi

            wuv = sb("wuv", [128, 4, 2048], BF16)
            Bwuv = Buf()
            P.dma("pool", wuv[:], w_uv.rearrange("(k p) e -> p k e", p=128), writes=[Bwuv])
            vbst = [sb("vbst%d" % i, [128, 2048], BF16) for i in range(2)]
            Bvb = [Buf() for _ in range(2)]

            def tm_job_vb(xt, Bxt, tt):
                for tb in range(4):
                    blk = tt * 4 + tb
                    s = blk % 2
                    for eg in range(4):
                        b = 2 + (eg % 2)
                        for k in range(4):
                            P.pe(lambda e, k=k, b=b, tb=tb, eg=eg: e.matmul(
                                banks[b][:], lhsT=xt[:, k, tb * 128:(tb + 1) * 128],
                                rhs=wuv[:, k, eg * 512:(eg + 1) * 512], start=(k == 0), stop=(k == 3)),
                                reads=[Bxt, Bwuv], writes=[BK[b]])
                        P.act(lambda e, s=s, b=b, eg=eg: e.activation(out=vbst[s][:, eg * 512:(eg + 1) * 512],
                                                                      in_=banks[b][:], func=AF.Copy),
                              reads=[BK[b]], writes=[Bvb[s]])
                    P.dma("sp", vb[blk * 128:(blk + 1) * 128, :], vbst[s][:], reads=[Bvb[s]])

            jobs = [fm_job(ws, w_uk, 4, h * 128, 128, kn_epi(h)) for h in range(16)]
            jobs.append(tm_job_vb)
            run_gemm(st, kvnT, 4, 8, jobs, xbufs=2)
            P.end_stage(touch)

        with ExitStack() as st:
            def sb(name, shape, dt):
                return st.enter_context(sbt(name, list(shape), dt))

            ws = WStream(st)
            ost = [sb("ost%d" % i, [128, 512], BF16) for i in range(3)]
            Bo = [Buf() for _ in range(3)]
            oi = [0]

            def copy_out(dst_fn, scale=1.0):
                def epi(tt, b):
                    s = oi[0] % 3
                    oi[0] += 1
                    P.act(lambda e, s=s, b=b: e.activation(out=ost[s][:], in_=banks[b][:], func=AF.Copy, scale=scale),
                          reads=[BK[b]], writes=[Bo[s]])
                    P.dma("sp", dst_fn(tt), ost[s][:], reads=[Bo[s]])
                return epi

            cq = sb("cq", [128, 8, 512], F32)
            sqb = sb("sqb", [128, 8, 512], BF16)
            rb = sb("rb", [128, 512], F32)
            cqn = sb("cqn", [128, 8, 512], BF16)
            Bc, Bsq, Brb, Bcqn = Buf(), Buf(), Buf(), Buf()
            cqnTr = cqnT.rearrange("(k p) t -> p k t", p=128)

            def cq_epi(j):
                def epi(tt, b):
                    P.dve(lambda e, b=b: e.tensor_copy(out=cq[:, j, :], in_=banks[b][:]), reads=[BK[b]], writes=[Bc])
                    P.act(lambda e, b=b: e.activation(out=sqb[:, j, :], in_=cq[:, j, :], func=AF.Square),
                          reads=[Bc], writes=[Bsq])
                    if j == 7:
                        for jj in range(8):
                            P.pe(lambda e, jj=jj: e.matmul(banks[4][:], lhsT=ones_bf[:], rhs=sqb[:, jj, :],
                                                           start=(jj == 0), stop=(jj == 7)),
                                 reads=[Bsq, B_const], writes=[BK[4]])
                        rsqrt_ops(rb[:], banks[4][:], 1.0 / 1024, [BK[4]], [Brb])
                        for jj in range(8):
                            P.dve(lambda e, jj=jj: e.scalar_tensor_tensor(
                                out=cqn[:, jj, :], in0=cq[:, jj, :], scalar=qg[:, jj:jj + 1], in1=rb[:],
                                op0=ALU.mult, op1=ALU.mult), reads=[Bc, Brb, B_const], writes=[Bcqn])
                        P.dma("sp", cqnTr[:, :, tt * 512:(tt + 1) * 512], cqn[:], reads=[Bcqn])
                return epi

            wist = sb("wist", [32, 512], F32)
            Bwi = Buf()
            IDX_SCALE = (128 ** -0.5) * (32 ** -0.5)

            def wi_epi(tt, b):
                P.act(lambda e, b=b: e.activation(out=wist[:], in_=banks[b][0:32, :], func=AF.Copy, scale=IDX_SCALE),
                      reads=[BK[b]], writes=[Bwi])
                dst = wi3[tt * 4 * 4096:(tt + 1) * 4 * 4096].rearrange("(bg f h) -> h bg f", h=32, f=4)
                P.dma("sp", dst, wist[:].rearrange("h (bg f) -> h bg f", f=4), reads=[Bwi], slow=True)

            jobs = []
            for h in range(16):
                jobs.append(fm_job(ws, w_in, 32, h * 128, 128,
                                   copy_out(lambda tt, h=h: qaT[h * 128:(h + 1) * 128, tt * 512:(tt + 1) * 512],
                                            scale=128 ** -0.5)))
            for h in range(32):
                jobs.append(fm_job(ws, w_in, 32, 2304 + h * 128, 128,
                                   copy_out(lambda tt, h=h: qiT[h * 128:(h + 1) * 128, tt * 512:(tt + 1) * 512])))
            jobs.append(fm_job(ws, w_in, 32, 6528, 32, wi_epi))
            for j in range(8):
                jobs.append(fm_job(ws, w_in, 32, 6560 + 128 * j, 128, cq_epi(j)))
            run_gemm(st, h1T, 32, 4, jobs, xbufs=2)
            P.end_stage(touch)

        with ExitStack() as st:
            def sb(name, shape, dt):
                return st.enter_context(sbt(name, list(shape), dt))

            QS = 192 ** -0.5
            wq = sb("wq", [128, 8, 3072], BF16)
            Bwq = Buf()
            wqr = w_uq.rearrange("(k p) e -> p k e", p=128)
            for k in range(8):
                P.dma("pool", wq[:, k, :], wqr[:, k, :], writes=[Bwq])
            ost = [sb("ost%d" % i, [128, 512], BF16) for i in range(3)]
            Bo = [Buf() for _ in range(3)]
            oi = [0]
            qrf = sb("qrf", [128, 8, 64], F32)
            qro = sb("qro", [128, 8, 64], F32)
            tmp8 = sb("tmp8", [128, 8, 32], F32)
            Bqr = Buf()
            qrst = sb("qrst", [64, 16, 512], BF16)
            Bqrst = Buf()
            wqrp = sb("wqrp", [128, 8, 16, 64], BF16)
            for k in range(8):
                P.dma("pool", wqrp[:, k, :, :], wqr[:, k, :].rearrange("p (h c) -> p h c", c=192)[:, :, 128:192],
                      writes=[Bwq])

            def qup_job(xt, Bxt, tt):
                for h in range(16):
                    b = gbank()
                    for k in range(8):
                        P.pe(lambda e, k=k, b=b, h=h: e.matmul(banks[b][:], lhsT=wq[:, k, h * 192:h * 192 + 128],
                                                               rhs=xt[:, k, :], start=(k == 0), stop=(k == 7)),
                             reads=[Bxt, Bwq], writes=[BK[b]])
                    s = oi[0] % 3
                    oi[0] += 1
                    P.act(lambda e, s=s, b=b: e.activation(out=ost[s][:], in_=banks[b][:], func=AF.Copy, scale=QS),
                          reads=[BK[b]], writes=[Bo[s]])
                    P.dma("sp", qnT[h * 128:(h + 1) * 128, tt * 512:(tt + 1) * 512], ost[s][:], reads=[Bo[s]])
                for tb in range(4):
                    blk = tt * 4 + tb
                    for hg in range(2):
                        b = 2 + hg
                        for k in range(8):
                            P.pe(lambda e, k=k, b=b, tb=tb, hg=hg: e.matmul(
                                banks[b][:],
                                lhsT=xt[:, k, tb * 128:(tb + 1) * 128], rhs=wqrp[:, k, hg * 8:(hg + 1) * 8, :],
                                start=(k == 0), stop=(k == 7)), reads=[Bxt, Bwq], writes=[BK[b]])
                        P.act(lambda e, b=b: e.activation(out=qrf[:].rearrange("p h c -> p (h c)"), in_=banks[b][:],
                                                          func=AF.Copy, scale=QS), reads=[BK[b]], writes=[Bqr])
                        rope_tok(([Bqr, B_const], [Bqr]), qrf[:], qro[:], blk, 8, tmp8[:])
                        for hh in range(8):
                            h = hg * 8 + hh
                            pb = 4 + (hh % 2)
                            P.pe(lambda e, hh=hh, pb=pb: e.transpose(banks[pb][0:64, 0:128], qro[:, hh, :], ident[:]),
                                 reads=[Bqr, B_const], writes=[BK[pb]])
                            P.act(lambda e, h=h, pb=pb, tb=tb: e.activation(
                                out=qrst[:, h, tb * 128:(tb + 1) * 128], in_=banks[pb][0:64, 0:128], func=AF.Copy),
                                reads=[BK[pb]], writes=[Bqrst])
                P.dma("sp", qrT.rearrange("(h c) t -> c h t", c=64)[:, :, tt * 512:(tt + 1) * 512], qrst[:],
                      reads=[Bqrst])

            run_gemm(st, cqnT, 8, 4, [qup_job], xbufs=2)
            P.end_stage(touch)

        slopes = [2.0 ** (-8.0 * (h + 1) / 16.0) for h in range(16)]

        with ExitStack() as st:
            def sb(name, shape, dt):
                return st.enter_context(sbt(name, list(shape), dt))

            kis = sb("kis", [128, 4096], BF16)
            kas = sb("kas", [128, 4096], BF16)
            vas = sb("vas", [128, 32, 128], BF16)
            Bk = Buf()
            P.dma("sp", kis[:], kiT, writes=[Bk])
            P.dma("sp", kas[:], kaT, writes=[Bk])
            P.dma("sp", vas[:], va.rearrange("(b p) d -> p b d", p=128), writes=[Bk])
            chkb = sb("chkb", [128, 4096], F32)
            posb = sb("posb", [128, 2048], F32)
            pb_i = sb("pb_i", [128, 4096], I32)
            Bpb = Buf()
            P.dma("sp", pb_i[:], posr.partition_broadcast(128)[:, 0, :], writes=[Bpb])
            P.dve(lambda e: e.tensor_copy(out=posb[:], in_=pb_i[:, 0:2048]), reads=[Bpb], writes=[Bk])
            P.dve(lambda e: e.tensor_scalar(out=pb_i[:], in0=pb_i[:], scalar1=6, scalar2=None,
                                            op0=ALU.arith_shift_right), reads=[Bpb, Bk], writes=[Bpb])
            P.dve(lambda e: e.tensor_copy(out=chkb[:], in_=pb_i[:]), reads=[Bpb], writes=[Bk])
            d4i = sb("d4i", [128, 4], I32)
            d4 = sb("d4", [128, 4], F32)
            d4b = sb("d4b", [128, 4], F32)
            P.pool(lambda e: e.iota(d4i[:], pattern=[[-32, 4]], base=0, channel_multiplier=1), writes=[Bk])
            P.dve(lambda e: e.tensor_copy(out=d4[:], in_=d4i[:]), reads=[Bk], writes=[Bk])
            P.dve(lambda e: e.tensor_scalar(out=d4b[:], in0=d4[:], scalar1=31.0, scalar2=None, op0=ALU.is_le),
                  reads=[Bk], writes=[Bk])
            P.dve(lambda e: e.tensor_scalar(out=d4[:], in0=d4[:], scalar1=0.0, scalar2=None, op0=ALU.is_ge),
                  reads=[Bk], writes=[Bk])
            P.dve(lambda e: e.tensor_tensor(out=d4[:], in0=d4[:], in1=d4b[:], op=ALU.mult), reads=[Bk], writes=[Bk])
            wbd = sb("wbd", [128, 32, 128], BF16)
            Bwbd = Buf()
            P.pool(lambda e: e.memset(wbd[:], 0.0), writes=[Bwbd])
            WT = sb("WT", [128, 32], F32)
            BWT = Buf()
            qib = [sb("qib%d" % i, [128, 128, 32], BF16) for i in range(2)]
            Bqi = [Buf() for _ in range(2)]
            qil = sb("qil", [128, 32, 128], BF16)
            Bqil = Buf()
            qab = [sb("qab%d" % i, [128, 16, 128], BF16) for i in range(2)]
            Bqa = [Buf() for _ in range(2)]
            Rt = [sb("Rt%d" % i, [128, 512], BF16) for i in range(4)]
            BR = [Buf() for _ in range(4)]
            Isc = sb("Isc", [128, 4096], F32)
            BI = Buf()
            pen = sb("pen", [128, 512], F32)
            Bpen = Buf()
            jk = sb("jk", [128, 4096], BF16)
            Bjk = Buf()
            sm = sb("sm", [128, 16], F32)
            Bsm = Buf()
            DmT = sb("DmT", [128, 32, 128], F32)
            BDm = Buf()
            mtmp = sb("mtmp", [128, 128], F32)
            Bmt = Buf()
            Zt = [sb("Zt%d" % i, [128, 512], F32) for i in range(2)]
            BZ = [Buf() for _ in range(2)]
            Pt = [sb("Pt%d" % i, [128, 512], BF16) for i in range(2)]
            BP = [Buf() for _ in range(2)]
            rden = sb("rden", [128, 512], F32)
            Brd = Buf()
            ostA = [sb("ostA%d" % i, [128, 4, 128], BF16) for i in range(2)]
            BoA = [Buf() for _ in range(2)]
            qiTr = qiT.rearrange("(h d) t -> d h t", d=128)
            qaTr = qaT.rearrange("(h d) t -> d h t", d=128)
            mixTr = mixT.rearrange("(h d) t -> d h t", d=128)
            ri_ = [0]
            for j in range(16):
                i = j // 4
                nkb = 4 * (i + 1)
                kbl = list(range(0, nkb)) + list(range(16, 16 + nkb))
                kgl = [kb for kb in kbl if kb % 4 == 0]
                nk = len(kbl) * 128
                qi_ = qib[j % 2]
                qa_ = qab[j % 2]
                P.dma("sp", qil[:], qiTr[:, :, j * 128:(j + 1) * 128], writes=[Bqil])
                P.pool(lambda e, qi_=qi_: e.tensor_copy(out=qi_[:], in_=qil[:].rearrange("p h t -> p t h")),
                       reads=[Bqil], writes=[Bqi[j % 2]])
                P.dma("sp", qa_[:], qaTr[:, :, j * 128:(j + 1) * 128], writes=[Bqa[j % 2]])
                P.dma("sp", WT[:], wi3[j * 4096:(j + 1) * 4096].rearrange("(g p) -> p g", p=128), writes=[BWT],
                      slow=True)
                for c4 in range(4):
                    dst = wbd[:].rearrange("p g c -> p (g c)")[:, c4:4096:132]
                    P.dve(lambda e, dst=dst, c4=c4: e.tensor_scalar(out=dst, in0=WT[:], scalar1=d4[:, c4:c4 + 1],
                                                                    scalar2=None, op0=ALU.mult),
                          reads=[BWT, Bk], writes=[Bwbd])
                for gi, kb0 in enumerate(kgl):
                    for g in range(32):
                        lb = 2 + (g % 2)
                        P.pe(lambda e, g=g, lb=lb, kb0=kb0, qi_=qi_: e.matmul(
                            banks[lb][:], lhsT=qi_[:, 4 * g:4 * g + 4, :], rhs=kis[:, kb0 * 128:kb0 * 128 + 512],
                            start=True, stop=True), reads=[Bqi[j % 2], Bk], writes=[BK[lb]])
                        r = ri_[0] % 4
                        ri_[0] += 1
                        if r % 2 == 0:
                            P.act(lambda e, r=r, lb=lb: e.activation(out=Rt[r][:], in_=banks[lb][:], func=AF.Relu),
                                  reads=[BK[lb]], writes=[BR[r]])
                        else:
                            P.dve(lambda e, r=r, lb=lb: e.tensor_scalar(out=Rt[r][:], in0=banks[lb][:], scalar1=0.0,
                                                                        scalar2=None, op0=ALU.max),
                                  reads=[BK[lb]], writes=[BR[r]])
                        P.pe(lambda e, g=g, r=r: e.matmul(banks[4][:], lhsT=wbd[:, g, :], rhs=Rt[r][:],
                                                          start=(g == 0), stop=(g == 31)),
                             reads=[Bwbd, BR[r]], writes=[BK[4]])
                    P.dve(lambda e, kb0=kb0, j=j: e.tensor_scalar(
                        out=pen[:], in0=chkb[:, kb0 * 128:kb0 * 128 + 512], scalar1=chkf[:, j:j + 1], scalar2=-1e30,
                        op0=ALU.is_gt, op1=ALU.mult), reads=[Bk, B_const], writes=[Bpen])
                    P.dve(lambda e, gi=gi: e.tensor_tensor(out=Isc[:, gi * 512:(gi + 1) * 512], in0=banks[4][:],
                                                           in1=pen[:], op=ALU.add),
                          reads=[BK[4], Bpen], writes=[BI])
                Iv = Isc[:, 0:nk]
                P.dve(lambda e, Iv=Iv: e.reduce_max(out=sm[:, 0:1], in_=Iv, axis=AX.X), reads=[BI], writes=[Bsm])
                P.dve(lambda e: e.tensor_scalar(out=sm[:, 0:1], in0=sm[:, 0:1], scalar1=-16.0, scalar2=None,
                                                op0=ALU.add), reads=[Bsm], writes=[Bsm])
                wdt = 17.0
                for it in range(NBIS):
                    wdt *= 0.5
                    P.dve(lambda e, wdt=wdt: e.tensor_scalar(out=sm[:, 1:2], in0=sm[:, 0:1], scalar1=wdt, scalar2=None,
                                                             op0=ALU.add), reads=[Bsm], writes=[Bsm])
                    P.dve(lambda e, Iv=Iv, nk=nk: e.tensor_scalar(out=jk[:, 0:nk], in0=Iv, scalar1=sm[:, 1:2],
                                                                  scalar2=None, op0=ALU.is_ge, op1=ALU.add,
                                                                  accum_out=sm[:, 2:3]),
                          reads=[BI, Bsm], writes=[Bjk, Bsm])
                    P.dve(lambda e, wdt=wdt: e.tensor_scalar(out=sm[:, 3:4], in0=sm[:, 2:3], scalar1=255.5, scalar2=wdt,
                                                             op0=ALU.is_ge, op1=ALU.mult), reads=[Bsm], writes=[Bsm])
                    P.dve(lambda e: e.tensor_tensor(out=sm[:, 0:1], in0=sm[:, 0:1], in1=sm[:, 3:4], op=ALU.add),
                          reads=[Bsm], writes=[Bsm])
                P.dve(lambda e, Iv=Iv, nk=nk: e.tensor_scalar(out=jk[:, 0:nk], in0=Iv, scalar1=-1e29, scalar2=None,
                                                              op0=ALU.is_ge, op1=ALU.add, accum_out=sm[:, 4:5]),
                      reads=[BI, Bsm], writes=[Bjk, Bsm])
                P.dve(lambda e: e.tensor_scalar(out=sm[:, 5:6], in0=sm[:, 4:5], scalar1=256.5, scalar2=None,
                                                op0=ALU.is_gt), reads=[Bsm], writes=[Bsm])
                P.dve(lambda e: e.tensor_tensor(out=sm[:, 6:7], in0=sm[:, 0:1], in1=sm[:, 5:6], op=ALU.mult),
                      reads=[Bsm], writes=[Bsm])
                P.dve(lambda e: e.tensor_scalar(out=sm[:, 7:8], in0=sm[:, 5:6], scalar1=-1.0, scalar2=1e29,
                                                op0=ALU.add, op1=ALU.mult), reads=[Bsm], writes=[Bsm])
                P.dve(lambda e: e.tensor_tensor(out=sm[:, 0:1], in0=sm[:, 6:7], in1=sm[:, 7:8], op=ALU.add),
                      reads=[Bsm], writes=[Bsm])
                P.dve(lambda e, Iv=Iv: e.tensor_scalar(out=Iv, in0=Iv, scalar1=sm[:, 0:1], scalar2=None, op0=ALU.is_ge),
                      reads=[BI, Bsm], writes=[BI])
                for kbi, kb in enumerate(kbl):
                    tbk = 5
                    P.pe(lambda e, kbi=kbi: e.transpose(banks[5][:, 0:128], Isc[:, kbi * 128:(kbi + 1) * 128], ident[:]),
                         reads=[BI, B_const], writes=[BK[5]])
                    P.dve(lambda e: e.tensor_scalar(out=mtmp[:], in0=banks[5][:, 0:128], scalar1=-1e6, scalar2=1e6,
                                                    op0=ALU.mult, op1=ALU.add), reads=[BK[5]], writes=[Bmt])
                    P.dve(lambda e, kbi=kbi, kb=kb, j=j: e.tensor_scalar(
                        out=DmT[:, kbi, :], in0=posb[:, j * 128:(j + 1) * 128], scalar1=posf[:, kb:kb + 1], scalar2=None,
                        op0=ALU.subtract), reads=[Bk, B_const], writes=[BDm])
                    P.dve(lambda e, kbi=kbi: e.scalar_tensor_tensor(
                        out=DmT[:, kbi, :], in0=DmT[:, kbi, :], scalar=-1.0, in1=DmT[:, kbi, :], op0=ALU.mult,
                        op1=ALU.max), reads=[BDm], writes=[BDm])
                    P.dve(lambda e, kbi=kbi: e.tensor_tensor(out=DmT[:, kbi, :], in0=DmT[:, kbi, :], in1=mtmp[:],
                                                             op=ALU.add), reads=[BDm, Bmt], writes=[BDm])
                for hg in range(4):
                    ob, db = 6, 7
                    for kbi, kb in enumerate(kbl):
                        sbk = kbi % 2
                        P.pe(lambda e, sbk=sbk, kb=kb, hg=hg, qa_=qa_: e.matmul(
                            banks[sbk][:], lhsT=kas[:, kb * 128:(kb + 1) * 128],
                            rhs=qa_[:, 4 * hg:4 * hg + 4, :], start=True, stop=True),
                            reads=[Bk, Bqa[j % 2]], writes=[BK[sbk]])
                        z = Zt[kbi % 2]
                        for hh in range(4):
                            h = 4 * hg + hh
                            P.dve(lambda e, z=z, hh=hh, h=h, kbi=kbi, sbk=sbk: e.scalar_tensor_tensor(
                                out=z[:, hh * 128:(hh + 1) * 128], in0=DmT[:, kbi, :], scalar=-slopes[h],
                                in1=banks[sbk][:, hh * 128:(hh + 1) * 128], op0=ALU.mult, op1=ALU.add),
                                reads=[BDm, BK[sbk]], writes=[BZ[kbi % 2]])
                        p_ = Pt[kbi % 2]
                        P.act(lambda e, z=z, p_=p_: e.activation(out=p_[:], in_=z[:], func=AF.Exp),
                              reads=[BZ[kbi % 2]], writes=[BP[kbi % 2]])
                        P.pe(lambda e, p_=p_, kb=kb, kbi=kbi, nl=len(kbl): e.matmul(banks[ob][:], lhsT=vas[:, kb, :], rhs=p_[:],
                                                                       start=(kbi == 0), stop=(kbi == nl - 1)),
                             reads=[Bk, BP[kbi % 2]], writes=[BK[ob]])
                        P.pe(lambda e, p_=p_, kbi=kbi, nl=len(kbl): e.matmul(banks[db][:], lhsT=ones_bf[:], rhs=p_[:],
                                                                start=(kbi == 0), stop=(kbi == nl - 1)),
                             reads=[B_const, BP[kbi % 2]], writes=[BK[db]])
                    P.dve(lambda e: e.reciprocal(out=rden[:], in_=banks[db][:]), reads=[BK[db]], writes=[Brd])
                    oa = ostA[hg % 2]
                    P.dve(lambda e, oa=oa: e.tensor_tensor(out=oa[:].rearrange("p h t -> p (h t)"), in0=banks[ob][:],
                                                           in1=rden[:], op=ALU.mult),
                          reads=[BK[ob], Brd], writes=[BoA[hg % 2]])
                    P.dma("sp", mixTr[:, 4 * hg:4 * hg + 4, j * 128:(j + 1) * 128], oa[:], reads=[BoA[hg % 2]])
            P.end_stage(touch)

        with ExitStack() as st:
            def sb(name, shape, dt):
                return st.enter_context(sbt(name, list(shape), dt))

            krs = sb("krs", [64, 4096], BF16)
            Bk = Buf()
            P.dma("sp", krs[:], krT, writes=[Bk])
            ctb = sb("ctb", [128, 2048], F32)
            pb_i = sb("pb_i", [128, 2048], I32)
            P.dma("sp", pb_i[:], posr[:, 0:2048].partition_broadcast(128)[:, 0, :], writes=[Bk])
            P.dve(lambda e: e.tensor_scalar(out=pb_i[:], in0=pb_i[:], scalar1=6, scalar2=None,
                                            op0=ALU.arith_shift_right), reads=[Bk], writes=[Bk])
            P.dve(lambda e: e.tensor_copy(out=ctb[:], in_=pb_i[:]), reads=[Bk], writes=[Bk])
            cm = sb("cm", [128, 8, 512], BF16)
            Bcm = Buf()
            kn = [sb("kn%d" % i, [128, 4096], BF16) for i in range(2)]
            vbh = [sb("vbh%d" % i, [128, 32, 128], BF16) for i in range(2)]
            Bkn = [Buf() for _ in range(2)]
            qn = [sb("qn%d" % i, [128, 512], BF16) for i in range(2)]
            qr = [sb("qr%d" % i, [64, 512], BF16) for i in range(2)]
            Bq = [Buf() for _ in range(2)]
            Pt = [sb("PtB%d" % i, [128, 512], BF16) for i in range(3)]
            BP = [Buf() for _ in range(3)]
            rden = sb("rdenB", [128, 512], F32)
            Brd = Buf()
            ostB = [sb("ostB%d" % i, [128, 512], BF16) for i in range(2)]
            BoB = [Buf() for _ in range(2)]
            vbr = vb.rearrange("(b p) e -> p b e", p=128)
            hi_ = 0
            pi_ = 0
            for i in range(4):
                nkb = 4 * (i + 1)
                kbl = list(range(0, nkb)) + list(range(16, 16 + nkb))
                band = list(range(4 * i, 4 * i + 4)) + list(range(16 + 4 * i, 16 + 4 * i + 4))
                for bi_, kb in enumerate(band):
                    P.dve(lambda e, bi_=bi_, kb=kb, i=i: e.tensor_scalar(
                        out=cm[:, bi_, :], in0=ctb[:, i * 512:(i + 1) * 512], scalar1=chkf[:, kb:kb + 1], scalar2=None,
                        op0=ALU.is_ge), reads=[Bk, B_const], writes=[Bcm])
                for h in range(16):
                    s = hi_ % 2
                    hi_ += 1
                    P.dma("sp", kn[s][:, 0:nkb * 128], knT[h * 128:(h + 1) * 128, 0:nkb * 128], writes=[Bkn[s]])
                    P.dma("sp", kn[s][:, 2048:2048 + nkb * 128], knT[h * 128:(h + 1) * 128, 2048:2048 + nkb * 128],
                          writes=[Bkn[s]])
                    P.dma("sp", vbh[s][:, 0:nkb, :], vbr[:, 0:nkb, h * 128:(h + 1) * 128], writes=[Bkn[s]])
                    P.dma("sp", vbh[s][:, 16:16 + nkb, :], vbr[:, 16:16 + nkb, h * 128:(h + 1) * 128], writes=[Bkn[s]])
                    P.dma("sp", qn[s][:], qnT[h * 128:(h + 1) * 128, i * 512:(i + 1) * 512], writes=[Bq[s]])
                    P.dma("sp", qr[s][:], qrT[h * 64:(h + 1) * 64, i * 512:(i + 1) * 512], writes=[Bq[s]])
                    ob = 4 + (h % 2)
                    db = 6 + (h % 2)
                    for kbi, kb in enumerate(kbl):
                        sbk = kbi % 2
                        P.pe(lambda e, sbk=sbk, kb=kb, s=s: e.matmul(banks[sbk][:], lhsT=kn[s][:, kb * 128:(kb + 1) * 128],
                                                                     rhs=qn[s][:], start=True, stop=False),
                             reads=[Bkn[s], Bq[s]], writes=[BK[sbk]])
                        P.pe(lambda e, sbk=sbk, kb=kb, s=s: e.matmul(banks[sbk][:], lhsT=krs[:, kb * 128:(kb + 1) * 128],
                                                                     rhs=qr[s][:], start=False, stop=True),
                             reads=[Bk, Bq[s]], writes=[BK[sbk]])
                        pp = pi_ % 3
                        pi_ += 1
                        p_ = Pt[pp]
                        P.act(lambda e, p_=p_, sbk=sbk: e.activation(out=p_[:], in_=banks[sbk][:], func=AF.Exp),
                              reads=[BK[sbk]], writes=[BP[pp]])
                        if kb in band:
                            bi_ = band.index(kb)
                            P.dve(lambda e, p_=p_, bi_=bi_: e.tensor_tensor(out=p_[:], in0=p_[:], in1=cm[:, bi_, :],
                                                                            op=ALU.mult),
                                  reads=[BP[pp], Bcm], writes=[BP[pp]])
                        P.pe(lambda e, p_=p_, kb=kb, kbi=kbi, s=s, ob=ob, nl=len(kbl): e.matmul(
                            banks[ob][:], lhsT=vbh[s][:, kb, :], rhs=p_[:], start=(kbi == 0),
                            stop=(kbi == nl - 1)), reads=[Bkn[s], BP[pp]], writes=[BK[ob]])
                        P.pe(lambda e, p_=p_, kbi=kbi, db=db, nl=len(kbl): e.matmul(
                            banks[db][:], lhsT=ones_bf[:], rhs=p_[:], start=(kbi == 0), stop=(kbi == nl - 1)),
                            reads=[B_const, BP[pp]], writes=[BK[db]])
                    P.dve(lambda e, db=db: e.reciprocal(out=rden[:], in_=banks[db][:]), reads=[BK[db]], writes=[Brd])
                    o_ = ostB[h % 2]
                    P.dve(lambda e, o_=o_, ob=ob: e.tensor_tensor(out=o_[:], in0=banks[ob][:], in1=rden[:], op=ALU.mult),
                          reads=[BK[ob], Brd], writes=[BoB[h % 2]])
                    P.dma("sp", mixT[(16 + h) * 128:(17 + h) * 128, i * 512:(i + 1) * 512], o_[:], reads=[BoB[h % 2]])
            P.end_stage(touch)

        with ExitStack() as st:
            def sb(name, shape, dt):
                return st.enter_context(sbt(name, list(shape), dt))

            ws = WStream(st)
            xch = [sb("xch%d" % i, [128, 512], F32) for i in range(2)]
            Bxc = [Buf() for _ in range(2)]
            sq = [sb("sq%d" % i, [128, 512], BF16) for i in range(2)]
            Bsq = [Buf() for _ in range(2)]
            ci = [0]

            def wo_epi(ec):
                def epi(tt, b):
                    s = ci[0] % 2
                    ci[0] += 1
                    P.dma("sp", xch[s][:], xT[ec * 128:(ec + 1) * 128, tt * 512:(tt + 1) * 512], writes=[Bxc[s]])
                    P.dve(lambda e, s=s, b=b: e.scalar_tensor_tensor(
                        out=xch[s][:], in0=banks[b][:], scalar=gate1[:, ec:ec + 1], in1=xch[s][:], op0=ALU.mult,
                        op1=ALU.add), reads=[BK[b], B_mod, Bxc[s]], writes=[Bxc[s]])
                    P.dma("sp", x2T[ec * 128:(ec + 1) * 128, tt * 512:(tt + 1) * 512], xch[s][:], reads=[Bxc[s]])
                    P.act(lambda e, s=s: e.activation(out=sq[s][:], in_=xch[s][:], func=AF.Square), reads=[Bxc[s]],
                          writes=[Bsq[s]])
                    P.pe(lambda e, s=s: e.matmul(banks[4][:], lhsT=ones_bf[:], rhs=sq[s][:], start=(ec == 0),
                                                 stop=(ec == 31)), reads=[B_const, Bsq[s]], writes=[BK[4]])
                    if ec == 31:
                        rsqrt_ops(r2b[:, tt * 512:(tt + 1) * 512], banks[4][:], 1.0 / D, [BK[4]], [B_r2b])
                return epi

            jobs = [fm_job(ws, w_o, 32, ec * 128, 128, wo_epi(ec)) for ec in range(32)]
            run_gemm(st, mixT, 32, 4, jobs, xbufs=2)
            P.end_stage(touch)

        with ExitStack() as st:
            def sb(name, shape, dt):
                return st.enter_context(sbt(name, list(shape), dt))

            xch = [sb("xch%d" % i, [128, 512], F32) for i in range(3)]
            Bxc = [Buf() for _ in range(3)]
            hch = [sb("hch%d" % i, [128, 512], BF16) for i in range(3)]
            Bhc = [Buf() for _ in range(3)]
            n = 0
            for tt in range(4):
                for k in range(32):
                    s = n % 3
                    n += 1
                    P.dma("sp", xch[s][:], x2T[k * 128:(k + 1) * 128, tt * 512:(tt + 1) * 512], writes=[Bxc[s]])
                    P.dve(lambda e, s=s, k=k, tt=tt: e.scalar_tensor_tensor(
                        out=xch[s][:], in0=xch[s][:], scalar=gs2[:, k:k + 1], in1=r2b[:, tt * 512:(tt + 1) * 512],
                        op0=ALU.mult, op1=ALU.mult), reads=[Bxc[s], B_mod, B_r2b], writes=[Bxc[s]])
                    P.act(lambda e, s=s, k=k: e.activation(out=hch[s][:], in_=xch[s][:], func=AF.Identity,
                                                           bias=sh2[:, k:k + 1], scale=1.0),
                          reads=[Bxc[s], B_mod], writes=[Bhc[s]])
                    P.dma("sp", h2T[k * 128:(k + 1) * 128, tt * 512:(tt + 1) * 512], hch[s][:], reads=[Bhc[s]])
            P.end_stage(touch)

        with ExitStack() as st:
            def sb(name, shape, dt):
                return st.enter_context(sbt(name, list(shape), dt))

            ws = WStream(st)
            u = [sb("u%d" % i, [128, 512], BF16) for i in range(3)]
            Bu = [Buf() for _ in range(3)]
            ci = [0]

            def m1_epi(fc):
                def epi(tt, b):
                    s = ci[0] % 3
                    ci[0] += 1
                    P.act(lambda e, s=s, b=b: e.activation(out=u[s][:], in_=banks[b][:], func=AF.Relu),
                          reads=[BK[b]], writes=[Bu[s]])
                    P.pool(lambda e, s=s: e.tensor_tensor(out=u[s][:], in0=u[s][:], in1=u[s][:], op=ALU.mult),
                           reads=[Bu[s]], writes=[Bu[s]])
                    P.dma("sp", hidT[fc * 128:(fc + 1) * 128, tt * 512:(tt + 1) * 512], u[s][:], reads=[Bu[s]])
                return epi

            jobs = [fm_job(ws, w1, 32, fc * 128, 128, m1_epi(fc)) for fc in range(128)]
            run_gemm(st, h2T, 32, 4, jobs, xbufs=2)
            P.end_stage(touch)

        with ExitStack() as st:
            def sb(name, shape, dt):
                return st.enter_context(sbt(name, list(shape), dt))

            ws = WStream(st)
            xch = [sb("xch%d" % i, [128, 512], F32) for i in range(2)]
            Bxc = [Buf() for _ in range(2)]
            sq = [sb("sq%d" % i, [128, 512], BF16) for i in range(2)]
            Bsq = [Buf() for _ in range(2)]
            sscol = sb("sscol", [128, 16], F32)
            Bss = Buf()
            P.pool(lambda e: e.memset(sscol[:], 0.0), writes=[Bss])
            ci = [0]

            def m2_epi(ec):
                def epi(tt, b):
                    s = ci[0] % 2
                    ci[0] += 1
                    P.dma("sp", xch[s][:], x2T[ec * 128:(ec + 1) * 128, tt * 512:(tt + 1) * 512], writes=[Bxc[s]])
                    P.dve(lambda e, s=s, b=b: e.scalar_tensor_tensor(
                        out=xch[s][:], in0=banks[b][:], scalar=gate2[:, ec:ec + 1], in1=xch[s][:], op0=ALU.mult,
                        op1=ALU.add), reads=[BK[b], B_mod, Bxc[s]], writes=[Bxc[s]])
                    P.act(lambda e, s=s: e.activation(out=sq[s][:], in_=xch[s][:], func=AF.Square), reads=[Bxc[s]],
                          writes=[Bsq[s]])
                    for tb in range(4):
                        P.pe(lambda e, s=s, tb=tb: e.matmul(banks[4][:, tb:tb + 1], lhsT=sq[s][:, tb * 128:(tb + 1) * 128],
                                                            rhs=ones_bf[:, 0:1], start=True, stop=True),
                             reads=[B_const, Bsq[s]], writes=[BK[4]])
                    P.dve(lambda e, tt=tt: e.tensor_tensor(out=sscol[:, tt * 4:tt * 4 + 4], in0=sscol[:, tt * 4:tt * 4 + 4],
                                                           in1=banks[4][:, 0:4], op=ALU.add),
                          reads=[BK[4], Bss], writes=[Bss])
                    P.dve(lambda e, s=s: e.tensor_scalar(out=xch[s][:], in0=xch[s][:], scalar1=fg[:, ec:ec + 1],
                                                         scalar2=None, op0=ALU.mult),
                          reads=[Bxc[s], Bsq[s], B_const], writes=[Bxc[s]])
                    P.dma("sp", x3T[ec * 128:(ec + 1) * 128, tt * 512:(tt + 1) * 512], xch[s][:], reads=[Bxc[s]])
                    if ec == 31 and tt == 3:
                        rsqrt_ops(rstd3[:], sscol[:], 1.0 / D, [Bss], [B_r3])
                return epi

            jobs = [fm_job(ws, w2, 128, ec * 128, 128, m2_epi(ec)) for ec in range(32)]
            run_gemm(st, hidT, 128, 4, jobs, xbufs=1)
            P.end_stage(touch)

        with ExitStack() as st:
            def sb(name, shape, dt):
                return st.enter_context(sbt(name, list(shape), dt))

            x3s = [sb("x3s%d" % i, [128, 32, 128], F32) for i in range(2)]
            Bx3 = [Buf() for _ in range(2)]
            yst = [sb("yst%d" % i, [128, 4096], F32) for i in range(2)]
            By = [Buf() for _ in range(2)]
            x3Tr = x3T.rearrange("(k p) t -> p k t", p=128)
            for blk in range(16):
                s = blk % 2
                P.dma("sp", x3s[s][:], x3Tr[:, :, blk * 128:(blk + 1) * 128], writes=[Bx3[s]])
                for k in range(32):
                    b = (k // 4) % 2
                    P.pe(lambda e, s=s, k=k, b=b: e.transpose(banks[b][:, (k % 4) * 128:(k % 4 + 1) * 128], x3s[s][:, k, :],
                                                              ident[:]), reads=[Bx3[s], B_const], writes=[BK[b]])
                    if k % 4 == 3:
                        P.act(lambda e, s=s, k=k, b=b, blk=blk: e.activation(
                            out=yst[s][:, (k - 3) * 128:(k + 1) * 128], in_=banks[b][:], func=AF.Identity,
                            scale=rstd3[:, blk:blk + 1]), reads=[BK[b], B_r3], writes=[By[s]])
                P.dma("sp", y[blk * 128:(blk + 1) * 128, :], yst[s][:], reads=[By[s]])
            P.end_stage(touch)
    return nc


_NC_CACHE = {}


def kernel(x, c, positions, w_ada, b_ada, ln1_g, w_in, q_norm_g, kv_norm_g, w_uq, w_uk, w_uv, w_o, ln2_g,
           w_mlp_in, w_mlp_out, final_g):
    x = np.asarray(x)
    positions = np.asarray(positions)
    B, S, _ = x.shape

    def colT(v, k):
        return np.ascontiguousarray(np.asarray(v, dtype=np.float32).reshape(k, 128).T)

    shared = {
        "w_ada": np.ascontiguousarray(np.asarray(w_ada)[0]), "b_adaT": colT(np.asarray(b_ada)[0], 192),
        "g1T": colT(np.asarray(ln1_g)[0], 32), "w_in": np.ascontiguousarray(np.asarray(w_in)[0]),
        "qgT": colT(np.asarray(q_norm_g)[0], 8), "kvgT": colT(np.asarray(kv_norm_g)[0], 4),
        "w_uq": np.ascontiguousarray(np.asarray(w_uq)[0]), "w_uk": np.ascontiguousarray(np.asarray(w_uk)[0]),
        "w_uv": np.ascontiguousarray(np.asarray(w_uv)[0]), "w_o": np.ascontiguousarray(np.asarray(w_o)[0]),
        "g2T": colT(np.asarray(ln2_g)[0], 32), "w1": np.ascontiguousarray(np.asarray(w_mlp_in)[0]),
        "w2": np.ascontiguousarray(np.asarray(w_mlp_out)[0]), "fgT": colT(np.asarray(final_g), 32),
    }
    in_maps = []
    perms = []
    for core in range(8):
        b, p = core // 2, core % 2
        own = [2 * j + p for j in range(16)]
        oth = [2 * j + 1 - p for j in range(16)]
        blocks = own + oth
        idx = np.concatenate([np.arange(g * 128, (g + 1) * 128) for g in blocks])
        perms.append((b, own))
        m = dict(shared)
        m["xc"] = np.ascontiguousarray(x[b][idx])
        pp = np.ascontiguousarray(positions[b][idx].astype(np.int32))
        m["posr"] = pp.reshape(1, 4096)
        m["posc"] = np.ascontiguousarray(pp.reshape(32, 128).T)
        m["cT"] = colT(np.asarray(c)[b], 32)
        in_maps.append(m)
    if "nc" not in _NC_CACHE:
        _NC_CACHE["nc"] = build_nc()
    res = run_bass_kernel_spmd(_NC_CACHE["nc"], in_maps, core_ids=list(range(8)))
    out = np.empty((B, S, D), dtype=np.float32)
    for core in range(8):
        b, own = perms[core]
        yv = np.asarray(res.results[core]["y"]).reshape(16, 128, D)
        for j, g in enumerate(own):
            out[b, g * 128:(g + 1) * 128, :] = yv[j]
    return out
```

```python
import math
import os
from contextlib import ExitStack
import numpy as np
import concourse.bass as bass
import concourse.mybir as mybir
from concourse.bass_utils import run_bass_kernel_spmd

F32 = mybir.dt.float32
BF16 = mybir.dt.bfloat16
I32 = mybir.dt.int32
AF = mybir.ActivationFunctionType
ALU = mybir.AluOpType
AX = mybir.AxisListType

ENGS = ("pe", "act", "dve", "pool", "sp")
SAME_ENGINE_SYNC = os.environ.get("KSES", "1") == "1"
D = 4096
EPS = 1e-6
NBIS = 20


import os
MAXSTAGE = int(os.environ.get("KSTAGE", "99"))
DBG_OUT = set(filter(None, os.environ.get("KDBG", "").split(",")))


class StopBuild(Exception):
    pass


class Buf:
    __slots__ = ("writers", "readers")

    def __init__(self):
        self.writers = []
        self.readers = []


class Op:
    __slots__ = ("eng", "fn", "deps", "is_dma", "seq", "signals", "dsem", "dtarget", "emitted", "touch")

    def __init__(self, eng, fn, is_dma):
        self.eng = eng
        self.fn = fn
        self.deps = []
        self.is_dma = is_dma
        self.seq = None
        self.signals = False
        self.dsem = None
        self.dtarget = None
        self.emitted = False
        self.touch = False


class Prog:
    def __init__(self, nc, stack, n_dma_sems=10):
        self.nc = nc
        self.pending = {e: [] for e in ENGS}
        self.stage_deps = {e: [] for e in ENGS}
        self.csem = {e: stack.enter_context(nc.semaphore("c_" + e)) for e in ENGS}
        self.dsems = {e: [stack.enter_context(nc.semaphore("d_%s_%d" % (e, i))) for i in range(n_dma_sems)]
                      for e in ("sp", "pool")}
        self.cnt = {e: 0 for e in ENGS}
        self.dma_k = {e: 0 for e in self.dsems}
        self.dma_uses = {e: [0] * n_dma_sems for e in self.dsems}
        self.waited = {e: {} for e in ENGS}
        self.stage_no = 0

    def add(self, eng, fn, reads=(), writes=(), dma=False, extra_deps=()):
        op = Op(eng, fn, dma)
        deps = []
        for b in reads:
            deps.extend(b.writers)
        for b in writes:
            deps.extend(b.writers)
            deps.extend(b.readers)
        deps.extend(extra_deps)
        if self.stage_deps[eng]:
            deps.extend(self.stage_deps[eng])
            self.stage_deps[eng] = []
        seen = set()
        for d in deps:
            if d is op or id(d) in seen or (d.emitted and not d.touch):
                continue
            seen.add(id(d))
            op.deps.append(d)
            if not d.is_dma and not (d.eng == eng and (eng == "pe" or not SAME_ENGINE_SYNC)):
                d.signals = True
        for b in reads:
            b.readers.append(op)
            if len(b.readers) > 48:
                b.readers = b.readers[-48:]
        for b in writes:
            if b.readers:
                b.readers = []
                b.writers = [op]
            else:
                b.writers.append(op)
                if len(b.writers) > 48:
                    b.writers = b.writers[-48:]
        self.pending[eng].append(op)
        return op

    def pe(self, fn, reads=(), writes=(), **kw):
        return self.add("pe", fn, reads, writes, **kw)

    def act(self, fn, reads=(), writes=(), **kw):
        return self.add("act", fn, reads, writes, **kw)

    def dve(self, fn, reads=(), writes=(), **kw):
        return self.add("dve", fn, reads, writes, **kw)

    def pool(self, fn, reads=(), writes=(), **kw):
        return self.add("pool", fn, reads, writes, **kw)

    def dma(self, q, out, in_, reads=(), writes=(), slow=False, **kw):
        if slow:
            fn = lambda e: e.dma_start(out=out, in_=in_, allow_slow_non_contiguous=True)
        else:
            fn = lambda e: e.dma_start(out=out, in_=in_)
        return self.add(q, fn, reads, writes, dma=True, **kw)

    def end_stage(self, touch):
        nc = self.nc
        tops = []
        for e in ("act", "dve", "pool", "sp"):
            t = Op(e, touch[e], e == "sp")
            t.signals = True
            t.touch = True
            self.pending[e].append(("T", t))
            tops.append(t)
        for e in ENGS:
            for item in self.pending[e]:
                op = item[1] if isinstance(item, tuple) else item
                if op.is_dma:
                    i = self.dma_k[e] % len(self.dsems[e])
                    self.dma_k[e] += 1
                    self.dma_uses[e][i] += 1
                    op.dsem = (e, i)
                    op.dtarget = 16 * self.dma_uses[e][i]
                elif op.signals:
                    self.cnt[e] += 1
                    op.seq = self.cnt[e]
        with nc.Block() as block:
            engmap = {"pe": block.tensor, "act": block.scalar, "dve": block.vector, "pool": block.gpsimd,
                      "sp": block.sync}

            def make(ename):
                items = self.pending[ename]
                waited = self.waited[ename]

                def body(eng):
                    def wait(key, sem, val):
                        if waited.get(key, 0) >= val:
                            return
                        waited[key] = val
                        eng.wait_ge(sem, val)

                    def wait_all_dma():
                        if ename in self.dsems:
                            for i, s in enumerate(self.dsems[ename]):
                                tot = 16 * self.dma_uses[ename][i]
                                if tot:
                                    wait(("d", ename, i), s, tot)

                    for item in items:
                        if isinstance(item, tuple):
                            op = item[1]
                            if ename in self.dsems:
                                for i, s in enumerate(self.dsems[ename]):
                                    tot = 16 * self.dma_uses[ename][i]
                                    if op.is_dma and op.dsem == (ename, i):
                                        tot -= 16
                                    if tot:
                                        wait(("d", ename, i), s, tot)
                        else:
                            op = item
                        if op.is_dma and op.dtarget > 16:
                            wait(("d",) + op.dsem, self.dsems[op.dsem[0]][op.dsem[1]], op.dtarget - 16)
                        for d in op.deps:
                            if d.is_dma:
                                wait(("d",) + d.dsem, self.dsems[d.dsem[0]][d.dsem[1]], d.dtarget)
                            else:
                                if d.eng == ename and (ename == "pe" or not SAME_ENGINE_SYNC):
                                    continue
                                wait(("c", d.eng), self.csem[d.eng], d.seq)
                        ins = op.fn(eng)
                        if op.is_dma:
                            ins.then_inc(self.dsems[op.dsem[0]][op.dsem[1]], 16)
                        elif op.signals:
                            ins.then_inc(self.csem[ename], 1)
                    wait_all_dma()

                return body

            for e in ENGS:
                if self.pending[e]:
                    engmap[e](make(e))
        for e in ENGS:
            for item in self.pending[e]:
                (item[1] if isinstance(item, tuple) else item).emitted = True
        self.pending = {e: [] for e in ENGS}
        for e in ENGS:
            self.stage_deps[e] = list(tops)
        self.stage_no += 1
        if self.stage_no > MAXSTAGE:
            raise StopBuild()


def build_nc():
    nc = bass.Bass("TRN2", target_bir_lowering=False)

    def inp(name, shape, dt=F32):
        return nc.dram_tensor(name, list(shape), dt, kind="ExternalInput").ap()

    _uc = [0]

    def sbt(name, shape, dt):
        _uc[0] += 1
        return nc.sbuf_tensor("%s_u%d" % (name, _uc[0]), shape, dt)

    def scr(name, shape, dt):
        return nc.dram_tensor(name, list(shape), dt, kind=("ExternalOutput" if name in DBG_OUT else "Internal")).ap()

    xc = inp("xc", [4096, 4096])
    posr = inp("posr", [1, 4096], I32)
    posc = inp("posc", [128, 32], I32)
    cT = inp("cT", [128, 32])
    w_ada = inp("w_ada", [4096, 24576])
    b_adaT = inp("b_adaT", [128, 192])
    g1T = inp("g1T", [128, 32])
    w_in = inp("w_in", [4096, 8160])
    qgT = inp("qgT", [128, 8])
    kvgT = inp("kvgT", [128, 4])
    w_uq = inp("w_uq", [1024, 3072])
    w_uk = inp("w_uk", [512, 2048])
    w_uv = inp("w_uv", [512, 2048])
    w_o = inp("w_o", [4096, 4096])
    g2T = inp("g2T", [128, 32])
    w1 = inp("w1", [4096, 16384])
    w2 = inp("w2", [16384, 4096])
    fgT = inp("fgT", [128, 32])
    y = nc.dram_tensor("y", [2048, 4096], F32, kind="ExternalOutput").ap()

    h1T = scr("h1T", [4096, 4096], BF16)
    xT = scr("xT", [4096, 2048], F32)
    kaT = scr("kaT", [128, 4096], BF16)
    va = scr("va", [4096, 128], BF16)
    kiT = scr("kiT", [128, 4096], BF16)
    krT = scr("krT", [64, 4096], BF16)
    kvnT = scr("kvnT", [512, 4096], BF16)
    knT = scr("knT", [2048, 4096], BF16)
    vb = scr("vb", [4096, 2048], BF16)
    qaT = scr("qaT", [2048, 2048], BF16)
    qiT = scr("qiT", [4096, 2048], BF16)
    wi3 = scr("wi3", [16 * 32 * 128], F32)
    cqnT = scr("cqnT", [1024, 2048], BF16)
    qnT = scr("qnT", [2048, 2048], BF16)
    qrT = scr("qrT", [16 * 64, 2048], BF16)
    mixT = scr("mixT", [4096, 2048], BF16)
    x2T = scr("x2T", [4096, 2048], F32)
    h2T = scr("h2T", [4096, 2048], BF16)
    hidT = scr("hidT", [16384, 2048], BF16)
    x3T = scr("x3T", [4096, 2048], F32)
    tdr = scr("tdr", [1, 64], F32)

    try:
        _build_body(nc, locals())
    except StopBuild:
        pass
    return nc


def _build_body(nc, L):
    globals().update({k: v for k, v in L.items() if k not in ("nc",)})
    with ExitStack() as gst:
        P = Prog(nc, gst)

        def gsb(name, shape, dt):
            return gst.enter_context(sbt(name, list(shape), dt))

        ident = gsb("ident", [128, 128], F32)
        ones_bf = gsb("ones_bf", [128, 128], BF16)
        modT = gsb("modT", [128, 192], F32)
        gs1 = gsb("gs1", [128, 32], F32)
        gs2 = gsb("gs2", [128, 32], F32)
        fg = gsb("fg", [128, 32], F32)
        kvg = gsb("kvg", [128, 4], F32)
        qg = gsb("qg", [128, 8], F32)
        posf = gsb("posf", [128, 32], F32)
        chkf = gsb("chkf", [128, 32], F32)
        cosT = gsb("cosT", [128, 32, 32], F32)
        sinT = gsb("sinT", [128, 32, 32], F32)
        r2b = gsb("r2b", [128, 2048], F32)
        rstd3 = gsb("rstd3", [128, 16], F32)
        tch = gsb("tch", [128, 8], F32)
        B_const = Buf()
        B_mod = Buf()
        B_r2b = Buf()
        B_r3 = Buf()
        banks = [gst.enter_context(nc.psum_tensor("bank%d" % i, [128, 512], F32)) for i in range(8)]
        BK = [Buf() for _ in range(8)]
        sh1 = modT[:, 0:32]
        gate1 = modT[:, 64:96]
        sh2 = modT[:, 96:128]
        gate2 = modT[:, 160:192]

        touch = {
            "act": lambda e: e.activation(out=tch[0:1, 0:1], in_=tch[0:1, 1:2], func=AF.Copy),
            "dve": lambda e: e.tensor_copy(out=tch[0:1, 2:3], in_=tch[0:1, 3:4]),
            "pool": lambda e: e.memset(tch[0:1, 4:5], 0.0),
            "sp": lambda e: e.dma_start(out=tdr[0:1, 0:2], in_=tch[0:1, 6:8]),
        }

        def rsqrt_ops(dst, src, scale, reads, writes):
            P.dve(lambda e: e.tensor_scalar(out=dst, in0=src, scalar1=scale, scalar2=EPS, op0=ALU.mult, op1=ALU.add),
                  reads=reads, writes=writes)
            P.act(lambda e: e.activation(out=dst, in_=dst, func=AF.Sqrt), reads=writes, writes=writes)
            P.dve(lambda e: e.reciprocal(out=dst, in_=dst), reads=writes, writes=writes)

        with ExitStack() as st:
            def sb(name, shape, dt):
                return st.enter_context(sbt(name, list(shape), dt))

            io_i = sb("io_i", [128, 128], I32)
            P.pool(lambda e: e.iota(io_i[:], pattern=[[1, 128]], base=0, channel_multiplier=-1), writes=[B_const])
            P.dve(lambda e: e.tensor_copy(out=ident[:], in_=io_i[:]), reads=[B_const], writes=[B_const])
            P.dve(lambda e: e.tensor_scalar(out=ident[:], in0=ident[:], scalar1=0.0, scalar2=None, op0=ALU.is_equal),
                  reads=[B_const], writes=[B_const])
            P.pool(lambda e: e.memset(ones_bf[:], 1.0), writes=[B_const])
            P.pool(lambda e: e.memset(tch[:], 0.0), writes=[B_const])
            Bp = Buf()
            ct_sb = sb("ct_sb", [128, 32], F32)
            badaT = sb("badaT", [128, 192], F32)
            g1s = sb("g1s", [128, 32], F32)
            g2s = sb("g2s", [128, 32], F32)
            posc_i = sb("posc_i", [128, 32], I32)
            chk_i = sb("chk_i", [128, 32], I32)
            for (dst, src) in ((ct_sb, cT), (badaT, b_adaT), (g1s, g1T), (g2s, g2T), (fg, fgT), (kvg, kvgT),
                               (qg, qgT), (posc_i, posc)):
                P.dma("sp", dst[:], src, writes=[Bp])
            P.dve(lambda e: e.tensor_copy(out=posf[:], in_=posc_i[:]), reads=[Bp], writes=[B_const])
            P.dve(lambda e: e.tensor_scalar(out=chk_i[:], in0=posc_i[:], scalar1=6, scalar2=None,
                                            op0=ALU.arith_shift_right), reads=[Bp], writes=[Bp])
            P.dve(lambda e: e.tensor_copy(out=chkf[:], in_=chk_i[:]), reads=[Bp], writes=[B_const])
            inv_i = sb("inv_i", [128, 32], I32)
            invf = sb("invf", [128, 32], F32)
            for i_ in range(32):
                P.pool(lambda e, i_=i_: e.memset(invf[:, i_:i_ + 1], float(np.float32(10000.0) ** np.float32(-2.0 * i_ / 64.0))),
                       writes=[Bp])
            rr = sb("rr", [128, 32, 32], F32)
            ri2 = [sb("ri%d" % i_, [128, 32, 32], I32) for i_ in range(2)]
            rf2 = [sb("rf%d" % i_, [128, 32, 32], F32) for i_ in range(2)]
            Br = Buf()
            for blk in range(32):
                P.dve(lambda e, blk=blk: e.tensor_scalar(out=rr[:, blk, :], in0=invf[:], scalar1=posf[:, blk:blk + 1],
                                                         scalar2=1.0 / (2 * math.pi), op0=ALU.mult, op1=ALU.mult),
                      reads=[Bp, B_const], writes=[Br])
            Bt = Buf()
            for (tab, off) in ((cosT, 0.25), (sinT, 0.0)):
                rrf = rr[:].rearrange("p a b -> p (a b)")
                rif = ri2[0 if off else 1][:].rearrange("p a b -> p (a b)")
                rff = rf2[0 if off else 1][:].rearrange("p a b -> p (a b)")
                tabf = tab[:].rearrange("p a b -> p (a b)")
                P.dve(lambda e, off=off, rff=rff, rrf=rrf: e.tensor_scalar(out=rff, in0=rrf, scalar1=off, scalar2=None,
                                                                           op0=ALU.add), reads=[Br], writes=[Bt])
                P.dve(lambda e, rff=rff, rif=rif: e.tensor_copy(out=rif, in_=rff), reads=[Bt], writes=[Bt])
                P.dve(lambda e, tabf=tabf, rif=rif: e.tensor_copy(out=tabf, in_=rif), reads=[Bt], writes=[B_const])
                P.dve(lambda e, rff=rff, tabf=tabf: e.tensor_tensor(out=rff, in0=rff, in1=tabf, op=ALU.subtract),
                      reads=[Bt, B_const], writes=[Bt])
                P.dve(lambda e, rff=rff, tabf=tabf: e.tensor_scalar(out=tabf, in0=rff, scalar1=0.5, scalar2=None,
                                                                    op0=ALU.is_gt), reads=[Bt], writes=[B_const])
                P.dve(lambda e, rff=rff, tabf=tabf: e.tensor_tensor(out=rff, in0=rff, in1=tabf, op=ALU.subtract),
                      reads=[Bt, B_const], writes=[Bt])
                P.dve(lambda e, rff=rff, tabf=tabf: e.tensor_scalar(out=tabf, in0=rff, scalar1=-0.5, scalar2=None,
                                                                    op0=ALU.is_lt), reads=[Bt], writes=[B_const])
                P.dve(lambda e, rff=rff, tabf=tabf: e.tensor_tensor(out=rff, in0=rff, in1=tabf, op=ALU.add),
                      reads=[Bt, B_const], writes=[Bt])
                P.act(lambda e, rff=rff, tabf=tabf: e.activation(out=tabf, in_=rff, func=AF.Sin, scale=2 * math.pi),
                      reads=[Bt], writes=[B_const])
            if "dbgtab" in DBG_OUT:
                dbgtab = scr("dbgtab", [128, 3072], F32)
                P.dma("sp", dbgtab[:, 0:1024], cosT[:].rearrange("p a b -> p (a b)"), reads=[B_const, Bt])
                P.dma("sp", dbgtab[:, 1024:2048], sinT[:].rearrange("p a b -> p (a b)"), reads=[B_const, Bt])
                P.dma("sp", dbgtab[:, 2048:3072], rr[:].rearrange("p a b -> p (a b)"), reads=[Br, Bt])
            scT = sb("scT", [128, 32], BF16)
            P.act(lambda e: e.activation(out=scT[:], in_=ct_sb[:], func=AF.Silu), reads=[Bp], writes=[Bp])
            wts = [sb("wa%d" % i, [128, 32, 128], BF16) for i in range(3)]
            Bw = [Buf() for _ in range(3)]
            war = w_ada.rearrange("(k p) e -> p k e", p=128)
            for ec in range(192):
                s = ec % 3
                P.dma("pool", wts[s][:], war[:, :, ec * 128:(ec + 1) * 128], writes=[Bw[s]])
                for k in range(32):
                    P.pe(lambda e, s=s, k=k, ec=ec: e.matmul(banks[0][:, ec:ec + 1], lhsT=wts[s][:, k, :],
                                                             rhs=scT[:, k:k + 1], start=(k == 0), stop=(k == 31)),
                         reads=[Bw[s], Bp], writes=[BK[0]])
            P.dve(lambda e: e.tensor_tensor(out=modT[:], in0=banks[0][:, 0:192], in1=badaT[:], op=ALU.add),
                  reads=[BK[0], Bp], writes=[B_mod])
            P.dve(lambda e: e.scalar_tensor_tensor(out=gs1[:], in0=modT[:, 32:64], scalar=1.0, in1=g1s[:],
                                                   op0=ALU.add, op1=ALU.mult), reads=[B_mod, Bp], writes=[B_mod])
            P.dve(lambda e: e.scalar_tensor_tensor(out=gs2[:], in0=modT[:, 128:160], scalar=1.0, in1=g2s[:],
                                                   op0=ALU.add, op1=ALU.mult), reads=[B_mod, Bp], writes=[B_mod])
            P.end_stage(touch)

        with ExitStack() as st:
            def sb(name, shape, dt):
                return st.enter_context(sbt(name, list(shape), dt))

            xb = [sb("xb%d" % i, [128, 4096], F32) for i in range(2)]
            Bx = [Buf() for _ in range(2)]
            junk = sb("junk", [128, 4096], BF16)
            Bj = Buf()
            hst = [sb("hst%d" % i, [128, 32, 512], BF16) for i in range(2)]
            Bh = [Buf() for _ in range(2)]
            xst = [sb("xst%d" % i, [128, 32, 128], F32) for i in range(2)]
            Bxs = [Buf() for _ in range(2)]
            ssq = sb("ssq", [128, 32], F32)
            Bs = Buf()
            h1Tr = h1T.rearrange("(k p) t -> p k t", p=128)
            xTr = xT.rearrange("(k p) t -> p k t", p=128)
            bi = 0
            for blk in range(32):
                s = blk % 2
                x_ = xb[s]
                P.dma("sp", x_[:], xc[blk * 128:(blk + 1) * 128, :], writes=[Bx[s]])
                if blk < 16:
                    xs = xst[blk % 2]
                    for k in range(32):
                        b = bi % 2
                        bi_k = k % 4
                        P.pe(lambda e, x_=x_, k=k, b=b, bi_k=bi_k: e.transpose(
                            banks[b][:, bi_k * 128:(bi_k + 1) * 128], x_[:, k * 128:(k + 1) * 128], ident[:]),
                            reads=[Bx[s], B_const], writes=[BK[b]])
                        if bi_k == 3:
                            P.dve(lambda e, xs=xs, k=k, b=b: e.tensor_copy(
                                out=xs[:, k - 3:k + 1, :], in_=banks[b][:].rearrange("p (a t) -> p a t", a=4)),
                                reads=[BK[b]], writes=[Bxs[blk % 2]])
                            bi += 1
                    P.dma("sp", xTr[:, :, blk * 128:(blk + 1) * 128], xs[:], reads=[Bxs[blk % 2]])
                P.act(lambda e, x_=x_, blk=blk: e.activation(out=junk[:], in_=x_[:], func=AF.Square,
                                                             accum_out=ssq[:, blk:blk + 1]),
                      reads=[Bx[s]], writes=[Bj, Bs])
                rsqrt_ops(ssq[:, blk:blk + 1], ssq[:, blk:blk + 1], 1.0 / D, [Bs], [Bs])
                P.dve(lambda e, x_=x_, blk=blk: e.tensor_scalar(out=x_[:], in0=x_[:], scalar1=ssq[:, blk:blk + 1],
                                                                scalar2=None, op0=ALU.mult),
                      reads=[Bx[s], Bs], writes=[Bx[s]])
                hs = hst[(blk // 4) % 2]
                Bhs = Bh[(blk // 4) % 2]
                tb = blk % 4
                for k in range(32):
                    b = 2 + (bi % 2)
                    bi_k = k % 4
                    P.pe(lambda e, x_=x_, k=k, b=b, bi_k=bi_k: e.transpose(
                        banks[b][:, bi_k * 128:(bi_k + 1) * 128], x_[:, k * 128:(k + 1) * 128], ident[:]),
                        reads=[Bx[s], B_const], writes=[BK[b]])
                    if bi_k == 3:
                        for kk in range(k - 3, k + 1):
                            P.act(lambda e, hs=hs, kk=kk, b=b, tb=tb: e.activation(
                                out=hs[:, kk, tb * 128:(tb + 1) * 128], in_=banks[b][:, (kk % 4) * 128:(kk % 4 + 1) * 128],
                                func=AF.Identity, scale=gs1[:, kk:kk + 1], bias=sh1[:, kk:kk + 1]),
                                reads=[BK[b], B_mod], writes=[Bhs])
                        bi += 1
                if tb == 3:
                    t0 = (blk // 4) * 512
                    P.dma("sp", h1Tr[:, :, t0:t0 + 512], hs[:], reads=[Bhs])
            P.end_stage(touch)

        def run_gemm(st, XT, KC, n_tt, jobs, xbufs=1, pre_tt=None):
            xts = [st.enter_context(sbt("xt%d" % i, [128, KC, 512], BF16)) for i in range(xbufs)]
            Bxt = [Buf() for _ in range(xbufs)]
            XTr = XT.rearrange("(k p) t -> p k t", p=128)
            for tt in range(n_tt):
                s = tt % xbufs
                for k0 in range(0, KC, 32):
                    k1 = min(KC, k0 + 32)
                    P.dma("sp", xts[s][:, k0:k1, :], XTr[:, k0:k1, tt * 512:(tt + 1) * 512], writes=[Bxt[s]])
                if pre_tt is not None:
                    pre_tt(tt)
                for job in jobs:
                    job(xts[s], Bxt[s], tt)

        class WStream:
            def __init__(self, st, n=3):
                self.t = [st.enter_context(sbt("ws%d" % i, [128, 32, 128], BF16)) for i in range(n)]
                self.B = [Buf() for _ in range(n)]
                self.i = 0

            def load(self, W, r0, nk, c0, ncol):
                s = self.i % len(self.t)
                self.i += 1
                src = W[r0:r0 + nk * 128, c0:c0 + ncol].rearrange("(k p) e -> p k e", p=128)
                P.dma("pool", self.t[s][:, 0:nk, 0:ncol], src, writes=[self.B[s]])
                return self.t[s], self.B[s]

        gb = [0]

        def gbank():
            gb[0] += 1
            return gb[0] % 2

        def fm_job(ws, W, KC, c0, ncol, epi):
            def job(xt, Bxt, tt):
                b = gbank()
                for k0 in range(0, KC, 32):
                    nk = min(32, KC - k0)
                    wt, Bw = ws.load(W, k0 * 128, nk, c0, ncol)
                    for k in range(nk):
                        P.pe(lambda e, wt=wt, k=k, k0=k0, b=b: e.matmul(
                            banks[b][0:ncol, :], lhsT=wt[:, k, 0:ncol], rhs=xt[:, k0 + k, :],
                            start=(k0 + k == 0), stop=(k0 + k == KC - 1)), reads=[Bw, Bxt], writes=[BK[b]])
                epi(tt, b)
            return job

        with ExitStack() as st:
            def sb(name, shape, dt):
                return st.enter_context(sbt(name, list(shape), dt))

            ws = WStream(st)
            ost = [sb("ost%d" % i, [128, 512], BF16) for i in range(3)]
            Bo = [Buf() for _ in range(3)]
            oi = [0]

            def copy_out(dst_fn, scale=1.0):
                def epi(tt, b):
                    s = oi[0] % 3
                    oi[0] += 1
                    P.act(lambda e, s=s, b=b: e.activation(out=ost[s][:], in_=banks[b][:], func=AF.Copy, scale=scale),
                          reads=[BK[b]], writes=[Bo[s]])
                    P.dma("sp", dst_fn(tt), ost[s][:], reads=[Bo[s]])
                return epi

            ckv = sb("ckv", [128, 4, 512], F32)
            sqb = sb("sqb", [128, 4, 512], BF16)
            rb = sb("rb", [128, 512], F32)
            kvn = sb("kvn", [128, 4, 512], BF16)
            Bc = Buf()
            Bsq = Buf()
            Brb = Buf()
            Bkvn = Buf()
            kvnTr = kvnT.rearrange("(k p) t -> p k t", p=128)

            def ckv_epi(j):
                def epi(tt, b):
                    P.dve(lambda e, b=b: e.tensor_copy(out=ckv[:, j, :], in_=banks[b][:]), reads=[BK[b]], writes=[Bc])
                    P.act(lambda e, b=b: e.activation(out=sqb[:, j, :], in_=ckv[:, j, :], func=AF.Square),
                          reads=[Bc], writes=[Bsq])
                    if j == 3:
                        for jj in range(4):
                            P.pe(lambda e, jj=jj: e.matmul(banks[4][:], lhsT=ones_bf[:], rhs=sqb[:, jj, :],
                                                           start=(jj == 0), stop=(jj == 3)),
                                 reads=[Bsq, B_const], writes=[BK[4]])
                        rsqrt_ops(rb[:], banks[4][:], 1.0 / 512, [BK[4]], [Brb])
                        for jj in range(4):
                            P.dve(lambda e, jj=jj: e.scalar_tensor_tensor(
                                out=kvn[:, jj, :], in0=ckv[:, jj, :], scalar=kvg[:, jj:jj + 1], in1=rb[:],
                                op0=ALU.mult, op1=ALU.mult), reads=[Bc, Brb, Bp0], writes=[Bkvn])
                        P.dma("sp", kvnTr[:, :, tt * 512:(tt + 1) * 512], kvn[:], reads=[Bkvn])
                return epi

            Bp0 = B_const
            wtm = sb("wtm", [128, 32, 192], BF16)
            Bwtm = Buf()
            w_in_r = w_in.rearrange("(k p) e -> p k e", p=128)
            P.dma("pool", wtm[:, :, 0:128], w_in_r[:, :, 2176:2304], writes=[Bwtm])
            P.dma("pool", wtm[:, :, 128:192], w_in_r[:, :, 8096:8160], writes=[Bwtm])
            vst = [sb("vst%d" % i, [128, 128], BF16) for i in range(2)]
            Bv = [Buf() for _ in range(2)]
            krf = sb("krf", [128, 64], F32)
            kro = sb("kro", [128, 64], F32)
            tmp1 = sb("tmp1", [128, 32], F32)
            Bkr = Buf()
            krst = sb("krst", [64, 512], BF16)
            Bkrst = Buf()

            def rope_tok(e_list, src, dst, blk, nh, tmp):
                c = cosT[:, blk, :].unsqueeze(1).to_broadcast([128, nh, 32])
                s_ = sinT[:, blk, :].unsqueeze(1).to_broadcast([128, nh, 32])
                x1 = src[:, :, 0:32]
                x2 = src[:, :, 32:64]
                ops = [
                    (dst[:, :, 0:32], x1, c, ALU.mult), (tmp, x2, s_, ALU.mult),
                    (dst[:, :, 0:32], dst[:, :, 0:32], tmp, ALU.subtract),
                    (dst[:, :, 32:64], x1, s_, ALU.mult), (tmp, x2, c, ALU.mult),
                    (dst[:, :, 32:64], dst[:, :, 32:64], tmp, ALU.add)]
                for (o, a, b_, op) in ops:
                    P.dve(lambda e, o=o, a=a, b_=b_, op=op: e.tensor_tensor(out=o, in0=a, in1=b_, op=op),
                          reads=e_list[0], writes=e_list[1])

            def tm_job_k(xt, Bxt, tt):
                for tb in range(4):
                    blk = tt * 4 + tb
                    b = 2 + (tb % 2)
                    for k in range(32):
                        P.pe(lambda e, k=k, b=b, tb=tb: e.matmul(banks[b][:, 0:192], lhsT=xt[:, k, tb * 128:(tb + 1) * 128],
                                                                 rhs=wtm[:, k, :], start=(k == 0), stop=(k == 31)),
                             reads=[Bxt, Bwtm], writes=[BK[b]])
                    s = blk % 2
                    P.act(lambda e, s=s, b=b: e.activation(out=vst[s][:], in_=banks[b][:, 0:128], func=AF.Copy),
                          reads=[BK[b]], writes=[Bv[s]])
                    P.dma("sp", va[blk * 128:(blk + 1) * 128, :], vst[s][:], reads=[Bv[s]])
                    P.act(lambda e, b=b: e.activation(out=krf[:], in_=banks[b][:, 128:192], func=AF.Copy), reads=[BK[b]],
                          writes=[Bkr])
                    rope_tok(([Bkr, B_const], [Bkr]), krf[:].unsqueeze(1), kro[:].unsqueeze(1), blk, 1,
                             tmp1[:].unsqueeze(1))
                    P.pe(lambda e: e.transpose(banks[5][0:64, 0:128], kro[:], ident[:]), reads=[Bkr, B_const],
                         writes=[BK[5]])
                    P.act(lambda e, tb=tb: e.activation(out=krst[:, tb * 128:(tb + 1) * 128], in_=banks[5][0:64, 0:128],
                                                        func=AF.Copy), reads=[BK[5]], writes=[Bkrst])
                P.dma("sp", krT[:, tt * 512:(tt + 1) * 512], krst[:], reads=[Bkrst])

            jobs = [fm_job(ws, w_in, 32, 2048, 128, copy_out(lambda tt: kaT[:, tt * 512:(tt + 1) * 512])),
                    fm_job(ws, w_in, 32, 6400, 128, copy_out(lambda tt: kiT[:, tt * 512:(tt + 1) * 512]))]
            for j in range(4):
                jobs.append(fm_job(ws, w_in, 32, 7584 + 128 * j, 128, ckv_epi(j)))
            jobs.append(tm_job_k)
            _sub = int(os.environ.get("KSUB", "255"))
            jobs = [jb for n_, jb in enumerate(jobs) if (_sub >> n_) & 1]
            run_gemm(st, h1T, 32, 8, jobs, xbufs=2)
            P.end_stage(touch)

        with ExitStack() as st:
            def sb(name, shape, dt):
                return st.enter_context(sbt(name, list(shape), dt))

            ws = WStream(st)
            ost = [sb("ost%d" % i, [128, 512], BF16) for i in range(3)]
            Bo = [Buf() for _ in range(3)]
            oi = [0]

            def kn_epi(h):
                def epi(tt, b):
                    s = oi[0] % 3
                    oi[0] += 1
                    P.act(lambda e, s=s, b=b: e.activation(out=ost[s][:], in_=banks[b][:], func=AF.Copy),
                          reads=[BK[b]], writes=[Bo[s]])
                    P.dma("sp", knT[h * 128:(h + 1) * 128, tt * 512:(tt + 1) * 512], ost[s][:], reads=[Bo[s]])
                return epi

            wuv = sb("wuv", [128, 4, 2048], BF16)
            Bwuv = Buf()
            P.dma("pool", wuv[:], w_uv.rearrange("(k p) e -> p k e", p=128), writes=[Bwuv])
            vbst = [sb("vbst%d" % i, [128, 2048], BF16) for i in range(2)]
            Bvb = [Buf() for _ in range(2)]

            def tm_job_vb(xt, Bxt, tt):
                for tb in range(4):
                    blk = tt * 4 + tb
                    s = blk % 2
                    for eg in range(4):
                        b = 2 + (eg % 2)
                        for k in range(4):
                            P.pe(lambda e, k=k, b=b, tb=tb, eg=eg: e.matmul(
                                banks[b][:], lhsT=xt[:, k, tb * 128:(tb + 1) * 128],
                                rhs=wuv[:, k, eg * 512:(eg + 1) * 512], start=(k == 0), stop=(k == 3)),
                                reads=[Bxt, Bwuv], writes=[BK[b]])
                        P.act(lambda e, s=s, b=b, eg=eg: e.activation(out=vbst[s][:, eg * 512:(eg + 1) * 512],
                                                                      in_=banks[b][:], func=AF.Copy),
                              reads=[BK[b]], writes=[Bvb[s]])
                    P.dma("sp", vb[blk * 128:(blk + 1) * 128, :], vbst[s][:], reads=[Bvb[s]])

            jobs = [fm_job(ws, w_uk, 4, h * 128, 128, kn_epi(h)) for h in range(16)]
            jobs.append(tm_job_vb)
            run_gemm(st, kvnT, 4, 8, jobs, xbufs=2)
            P.end_stage(touch)

        with ExitStack() as st:
            def sb(name, shape, dt):
                return st.enter_context(sbt(name, list(shape), dt))

            ws = WStream(st)
            ost = [sb("ost%d" % i, [128, 512], BF16) for i in range(3)]
            Bo = [Buf() for _ in range(3)]
            oi = [0]

            def copy_out(dst_fn, scale=1.0):
                def epi(tt, b):
                    s = oi[0] % 3
                    oi[0] += 1
                    P.act(lambda e, s=s, b=b: e.activation(out=ost[s][:], in_=banks[b][:], func=AF.Copy, scale=scale),
                          reads=[BK[b]], writes=[Bo[s]])
                    P.dma("sp", dst_fn(tt), ost[s][:], reads=[Bo[s]])
                return epi

            cq = sb("cq", [128, 8, 512], F32)
            sqb = sb("sqb", [128, 8, 512], BF16)
            rb = sb("rb", [128, 512], F32)
            cqn = sb("cqn", [128, 8, 512], BF16)
            Bc, Bsq, Brb, Bcqn = Buf(), Buf(), Buf(), Buf()
            cqnTr = cqnT.rearrange("(k p) t -> p k t", p=128)

            def cq_epi(j):
                def epi(tt, b):
                    P.dve(lambda e, b=b: e.tensor_copy(out=cq[:, j, :], in_=banks[b][:]), reads=[BK[b]], writes=[Bc])
                    P.act(lambda e, b=b: e.activation(out=sqb[:, j, :], in_=cq[:, j, :], func=AF.Square),
                          reads=[Bc], writes=[Bsq])
                    if j == 7:
                        for jj in range(8):
                            P.pe(lambda e, jj=jj: e.matmul(banks[4][:], lhsT=ones_bf[:], rhs=sqb[:, jj, :],
                                                           start=(jj == 0), stop=(jj == 7)),
                                 reads=[Bsq, B_const], writes=[BK[4]])
                        rsqrt_ops(rb[:], banks[4][:], 1.0 / 1024, [BK[4]], [Brb])
                        for jj in range(8):
                            P.dve(lambda e, jj=jj: e.scalar_tensor_tensor(
                                out=cqn[:, jj, :], in0=cq[:, jj, :], scalar=qg[:, jj:jj + 1], in1=rb[:],
                                op0=ALU.mult, op1=ALU.mult), reads=[Bc, Brb, B_const], writes=[Bcqn])
                        P.dma("sp", cqnTr[:, :, tt * 512:(tt + 1) * 512], cqn[:], reads=[Bcqn])
                return epi

            wist = sb("wist", [32, 512], F32)
            Bwi = Buf()
            IDX_SCALE = (128 ** -0.5) * (32 ** -0.5)

            def wi_epi(tt, b):
                P.act(lambda e, b=b: e.activation(out=wist[:], in_=banks[b][0:32, :], func=AF.Copy, scale=IDX_SCALE),
                      reads=[BK[b]], writes=[Bwi])
                dst = wi3[tt * 4 * 4096:(tt + 1) * 4 * 4096].rearrange("(bg f h) -> h bg f", h=32, f=4)
                P.dma("sp", dst, wist[:].rearrange("h (bg f) -> h bg f", f=4), reads=[Bwi], slow=True)

            jobs = []
            for h in range(16):
                jobs.append(fm_job(ws, w_in, 32, h * 128, 128,
                                   copy_out(lambda tt, h=h: qaT[h * 128:(h + 1) * 128, tt * 512:(tt + 1) * 512],
                                            scale=128 ** -0.5)))
            for h in range(32):
                jobs.append(fm_job(ws, w_in, 32, 2304 + h * 128, 128,
                                   copy_out(lambda tt, h=h: qiT[h * 128:(h + 1) * 128, tt * 512:(tt + 1) * 512])))
            jobs.append(fm_job(ws, w_in, 32, 6528, 32, wi_epi))
            for j in range(8):
                jobs.append(fm_job(ws, w_in, 32, 6560 + 128 * j, 128, cq_epi(j)))
            run_gemm(st, h1T, 32, 4, jobs, xbufs=2)
            P.end_stage(touch)

        with ExitStack() as st:
            def sb(name, shape, dt):
                return st.enter_context(sbt(name, list(shape), dt))

            QS = 192 ** -0.5
            wq = sb("wq", [128, 8, 3072], BF16)
            Bwq = Buf()
            wqr = w_uq.rearrange("(k p) e -> p k e", p=128)
            for k in range(8):
                P.dma("pool", wq[:, k, :], wqr[:, k, :], writes=[Bwq])
            ost = [sb("ost%d" % i, [128, 512], BF16) for i in range(3)]
            Bo = [Buf() for _ in range(3)]
            oi = [0]
            qrf = sb("qrf", [128, 8, 64], F32)
            qro = sb("qro", [128, 8, 64], F32)
            tmp8 = sb("tmp8", [128, 8, 32], F32)
            Bqr = Buf()
            qrst = sb("qrst", [64, 16, 512], BF16)
            Bqrst = Buf()
            wqrp = sb("wqrp", [128, 8, 16, 64], BF16)
            for k in range(8):
                P.dma("pool", wqrp[:, k, :, :], wqr[:, k, :].rearrange("p (h c) -> p h c", c=192)[:, :, 128:192],
                      writes=[Bwq])

            def qup_job(xt, Bxt, tt):
                for h in range(16):
                    b = gbank()
                    for k in range(8):
                        P.pe(lambda e, k=k, b=b, h=h: e.matmul(banks[b][:], lhsT=wq[:, k, h * 192:h * 192 + 128],
                                                               rhs=xt[:, k, :], start=(k == 0), stop=(k == 7)),
                             reads=[Bxt, Bwq], writes=[BK[b]])
                    s = oi[0] % 3
                    oi[0] += 1
                    P.act(lambda e, s=s, b=b: e.activation(out=ost[s][:], in_=banks[b][:], func=AF.Copy, scale=QS),
                          reads=[BK[b]], writes=[Bo[s]])
                    P.dma("sp", qnT[h * 128:(h + 1) * 128, tt * 512:(tt + 1) * 512], ost[s][:], reads=[Bo[s]])
                for tb in range(4):
                    blk = tt * 4 + tb
                    for hg in range(2):
                        b = 2 + hg
                        for k in range(8):
                            P.pe(lambda e, k=k, b=b, tb=tb, hg=hg: e.matmul(
                                banks[b][:],
                                lhsT=xt[:, k, tb * 128:(tb + 1) * 128], rhs=wqrp[:, k, hg * 8:(hg + 1) * 8, :],
                                start=(k == 0), stop=(k == 7)), reads=[Bxt, Bwq], writes=[BK[b]])
                        P.act(lambda e, b=b: e.activation(out=qrf[:].rearrange("p h c -> p (h c)"), in_=banks[b][:],
                                                          func=AF.Copy, scale=QS), reads=[BK[b]], writes=[Bqr])
                        rope_tok(([Bqr, B_const], [Bqr]), qrf[:], qro[:], blk, 8, tmp8[:])
                        for hh in range(8):
                            h = hg * 8 + hh
                            pb = 4 + (hh % 2)
                            P.pe(lambda e, hh=hh, pb=pb: e.transpose(banks[pb][0:64, 0:128], qro[:, hh, :], ident[:]),
                                 reads=[Bqr, B_const], writes=[BK[pb]])
                            P.act(lambda e, h=h, pb=pb, tb=tb: e.activation(
                                out=qrst[:, h, tb * 128:(tb + 1) * 128], in_=banks[pb][0:64, 0:128], func=AF.Copy),
                                reads=[BK[pb]], writes=[Bqrst])
                P.dma("sp", qrT.rearrange("(h c) t -> c h t", c=64)[:, :, tt * 512:(tt + 1) * 512], qrst[:],
                      reads=[Bqrst])

            run_gemm(st, cqnT, 8, 4, [qup_job], xbufs=2)
            P.end_stage(touch)

        slopes = [2.0 ** (-8.0 * (h + 1) / 16.0) for h in range(16)]

        with ExitStack() as st:
            def sb(name, shape, dt):
                return st.enter_context(sbt(name, list(shape), dt))

            kis = sb("kis", [128, 4096], BF16)
            kas = sb("kas", [128, 4096], BF16)
            vas = sb("vas", [128, 32, 128], BF16)
            Bk = Buf()
            P.dma("sp", kis[:], kiT, writes=[Bk])
            P.dma("sp", kas[:], kaT, writes=[Bk])
            P.dma("sp", vas[:], va.rearrange("(b p) d -> p b d", p=128), writes=[Bk])
            chkb = sb("chkb", [128, 4096], F32)
            posb = sb("posb", [128, 2048], F32)
            pb_i = sb("pb_i", [128, 4096], I32)
            Bpb = Buf()
            P.dma("sp", pb_i[:], posr.partition_broadcast(128)[:, 0, :], writes=[Bpb])
            P.dve(lambda e: e.tensor_copy(out=posb[:], in_=pb_i[:, 0:2048]), reads=[Bpb], writes=[Bk])
            P.dve(lambda e: e.tensor_scalar(out=pb_i[:], in0=pb_i[:], scalar1=6, scalar2=None,
                                            op0=ALU.arith_shift_right), reads=[Bpb, Bk], writes=[Bpb])
            P.dve(lambda e: e.tensor_copy(out=chkb[:], in_=pb_i[:]), reads=[Bpb], writes=[Bk])
            d4i = sb("d4i", [128, 4], I32)
            d4 = sb("d4", [128, 4], F32)
            d4b = sb("d4b", [128, 4], F32)
            P.pool(lambda e: e.iota(d4i[:], pattern=[[-32, 4]], base=0, channel_multiplier=1), writes=[Bk])
            P.dve(lambda e: e.tensor_copy(out=d4[:], in_=d4i[:]), reads=[Bk], writes=[Bk])
            P.dve(lambda e: e.tensor_scalar(out=d4b[:], in0=d4[:], scalar1=31.0, scalar2=None, op0=ALU.is_le),
                  reads=[Bk], writes=[Bk])
            P.dve(lambda e: e.tensor_scalar(out=d4[:], in0=d4[:], scalar1=0.0, scalar2=None, op0=ALU.is_ge),
                  reads=[Bk], writes=[Bk])
            P.dve(lambda e: e.tensor_tensor(out=d4[:], in0=d4[:], in1=d4b[:], op=ALU.mult), reads=[Bk], writes=[Bk])
            wbd = sb("wbd", [128, 32, 128], BF16)
            Bwbd = Buf()
            P.pool(lambda e: e.memset(wbd[:], 0.0), writes=[Bwbd])
            WT = sb("WT", [128, 32], F32)
            BWT = Buf()
            qib = [sb("qib%d" % i, [128, 128, 32], BF16) for i in range(2)]
            Bqi = [Buf() for _ in range(2)]
            qil = sb("qil", [128, 32, 128], BF16)
            Bqil = Buf()
            qab = [sb("qab%d" % i, [128, 16, 128], BF16) for i in range(2)]
            Bqa = [Buf() for _ in range(2)]
            Rt = [sb("Rt%d" % i, [128, 512], BF16) for i in range(4)]
            BR = [Buf() for _ in range(4)]
            Isc = sb("Isc", [128, 4096], F32)
            BI = Buf()
            pen = sb("pen", [128, 512], F32)
            Bpen = Buf()
            jk = sb("jk", [128, 4096], BF16)
            Bjk = Buf()
            sm = sb("sm", [128, 16], F32)
            Bsm = Buf()
            DmT = sb("DmT", [128, 32, 128], F32)
            BDm = Buf()
            mtmp = sb("mtmp", [128, 128], F32)
            Bmt = Buf()
            Zt = [sb("Zt%d" % i, [128, 512], F32) for i in range(2)]
            BZ = [[Buf() for _ in range(4)] for _ in range(2)]
            Pt = [sb("Pt%d" % i, [128, 512], BF16) for i in range(2)]
            BP = [Buf() for _ in range(2)]
            rden = sb("rden", [128, 512], F32)
            Brd = Buf()
            ostA = [sb("ostA%d" % i, [128, 4, 128], BF16) for i in range(2)]
            BoA = [Buf() for _ in range(2)]
            qiTr = qiT.rearrange("(h d) t -> d h t", d=128)
            qaTr = qaT.rearrange("(h d) t -> d h t", d=128)
            mixTr = mixT.rearrange("(h d) t -> d h t", d=128)
            ri_ = [0]
            for j in range(16):
                i = j // 4
                nkb = 4 * (i + 1)
                kbl = list(range(0, nkb)) + list(range(16, 16 + nkb))
                kgl = [kb for kb in kbl if kb % 4 == 0]
                nk = len(kbl) * 128
                qi_ = qib[j % 2]
                qa_ = qab[j % 2]
                P.dma("sp", qil[:], qiTr[:, :, j * 128:(j + 1) * 128], writes=[Bqil])
                P.pool(lambda e, qi_=qi_: e.tensor_copy(out=qi_[:], in_=qil[:].rearrange("p h t -> p t h")),
                       reads=[Bqil], writes=[Bqi[j % 2]])
                P.dma("sp", qa_[:], qaTr[:, :, j * 128:(j + 1) * 128], writes=[Bqa[j % 2]])
                P.dma("sp", WT[:], wi3[j * 4096:(j + 1) * 4096].rearrange("(g p) -> p g", p=128), writes=[BWT],
                      slow=True)
                for c4 in range(4):
                    dst = wbd[:].rearrange("p g c -> p (g c)")[:, c4:4096:132]
                    P.dve(lambda e, dst=dst, c4=c4: e.tensor_scalar(out=dst, in0=WT[:], scalar1=d4[:, c4:c4 + 1],
                                                                    scalar2=None, op0=ALU.mult),
                          reads=[BWT, Bk], writes=[Bwbd])
                for gi, kb0 in enumerate(kgl):
                    for g in range(32):
                        lb = 2 + (g % 2)
                        P.pe(lambda e, g=g, lb=lb, kb0=kb0, qi_=qi_: e.matmul(
                            banks[lb][:], lhsT=qi_[:, 4 * g:4 * g + 4, :], rhs=kis[:, kb0 * 128:kb0 * 128 + 512],
                            start=True, stop=True), reads=[Bqi[j % 2], Bk], writes=[BK[lb]])
                        r = ri_[0] % 4
                        ri_[0] += 1
                        if r % 2 == 0:
                            P.act(lambda e, r=r, lb=lb: e.activation(out=Rt[r][:], in_=banks[lb][:], func=AF.Relu),
                                  reads=[BK[lb]], writes=[BR[r]])
                        else:
                            P.dve(lambda e, r=r, lb=lb: e.tensor_scalar(out=Rt[r][:], in0=banks[lb][:], scalar1=0.0,
                                                                        scalar2=None, op0=ALU.max),
                                  reads=[BK[lb]], writes=[BR[r]])
                        P.pe(lambda e, g=g, r=r: e.matmul(banks[4][:], lhsT=wbd[:, g, :], rhs=Rt[r][:],
                                                          start=(g == 0), stop=(g == 31)),
                             reads=[Bwbd, BR[r]], writes=[BK[4]])
                    P.dve(lambda e, kb0=kb0, j=j: e.tensor_scalar(
                        out=pen[:], in0=chkb[:, kb0 * 128:kb0 * 128 + 512], scalar1=chkf[:, j:j + 1], scalar2=-1e30,
                        op0=ALU.is_gt, op1=ALU.mult), reads=[Bk, B_const], writes=[Bpen])
                    P.dve(lambda e, gi=gi: e.tensor_tensor(out=Isc[:, gi * 512:(gi + 1) * 512], in0=banks[4][:],
                                                           in1=pen[:], op=ALU.add),
                          reads=[BK[4], Bpen], writes=[BI])
                Iv = Isc[:, 0:nk]
                P.dve(lambda e, Iv=Iv: e.reduce_max(out=sm[:, 0:1], in_=Iv, axis=AX.X), reads=[BI], writes=[Bsm])
                P.dve(lambda e: e.tensor_scalar(out=sm[:, 0:1], in0=sm[:, 0:1], scalar1=-7.5, scalar2=None,
                                                op0=ALU.add), reads=[Bsm], writes=[Bsm])
                wdt = 8.5
                for it in range(NBIS):
                    P.dve(lambda e, Iv=Iv, nk=nk: e.tensor_scalar(out=jk[:, 0:nk], in0=Iv, scalar1=sm[:, 0:1],
                                                                  scalar2=None, op0=ALU.is_ge, op1=ALU.add,
                                                                  accum_out=sm[:, 2:3]),
                          reads=[BI, Bsm], writes=[Bjk, Bsm])
                    nw = wdt * 0.5 if it < NBIS - 1 else wdt
                    mul = 2.0 * nw if it < NBIS - 1 else wdt
                    P.dve(lambda e, mul=mul: e.tensor_scalar(out=sm[:, 3:4], in0=sm[:, 2:3], scalar1=255.5, scalar2=mul,
                                                             op0=ALU.is_ge, op1=ALU.mult), reads=[Bsm], writes=[Bsm])
                    P.dve(lambda e, nw=nw: e.scalar_tensor_tensor(out=sm[:, 0:1], in0=sm[:, 0:1], scalar=-nw,
                                                                  in1=sm[:, 3:4], op0=ALU.add, op1=ALU.add),
                          reads=[Bsm], writes=[Bsm])
                    wdt = nw
                P.dve(lambda e, Iv=Iv, nk=nk: e.tensor_scalar(out=jk[:, 0:nk], in0=Iv, scalar1=-1e29, scalar2=None,
                                                              op0=ALU.is_ge, op1=ALU.add, accum_out=sm[:, 4:5]),
                      reads=[BI, Bsm], writes=[Bjk, Bsm])
                P.dve(lambda e: e.tensor_scalar(out=sm[:, 5:6], in0=sm[:, 4:5], scalar1=256.5, scalar2=None,
                                                op0=ALU.is_gt), reads=[Bsm], writes=[Bsm])
                P.dve(lambda e: e.tensor_tensor(out=sm[:, 6:7], in0=sm[:, 0:1], in1=sm[:, 5:6], op=ALU.mult),
                      reads=[Bsm], writes=[Bsm])
                P.dve(lambda e: e.tensor_scalar(out=sm[:, 7:8], in0=sm[:, 5:6], scalar1=-1.0, scalar2=1e29,
                                                op0=ALU.add, op1=ALU.mult), reads=[Bsm], writes=[Bsm])
                P.dve(lambda e: e.tensor_tensor(out=sm[:, 0:1], in0=sm[:, 6:7], in1=sm[:, 7:8], op=ALU.add),
                      reads=[Bsm], writes=[Bsm])
                P.dve(lambda e, Iv=Iv: e.tensor_scalar(out=Iv, in0=Iv, scalar1=sm[:, 0:1], scalar2=None, op0=ALU.is_ge),
                      reads=[BI, Bsm], writes=[BI])
                for kbi, kb in enumerate(kbl):
                    tbk = 5
                    P.pe(lambda e, kbi=kbi: e.transpose(banks[5][:, 0:128], Isc[:, kbi * 128:(kbi + 1) * 128], ident[:]),
                         reads=[BI, B_const], writes=[BK[5]])
                    P.dve(lambda e: e.tensor_scalar(out=mtmp[:], in0=banks[5][:, 0:128], scalar1=-1e6, scalar2=1e6,
                                                    op0=ALU.mult, op1=ALU.add), reads=[BK[5]], writes=[Bmt])
                    P.dve(lambda e, kbi=kbi, kb=kb, j=j: e.tensor_scalar(
                        out=DmT[:, kbi, :], in0=posb[:, j * 128:(j + 1) * 128], scalar1=posf[:, kb:kb + 1], scalar2=None,
                        op0=ALU.subtract), reads=[Bk, B_const], writes=[BDm])
                    P.dve(lambda e, kbi=kbi: e.scalar_tensor_tensor(
                        out=DmT[:, kbi, :], in0=DmT[:, kbi, :], scalar=-1.0, in1=DmT[:, kbi, :], op0=ALU.mult,
                        op1=ALU.max), reads=[BDm], writes=[BDm])
                    P.dve(lambda e, kbi=kbi: e.tensor_tensor(out=DmT[:, kbi, :], in0=DmT[:, kbi, :], in1=mtmp[:],
                                                             op=ALU.add), reads=[BDm, Bmt], writes=[BDm])
                for hg in range(4):
                    ob, db = 6, 7
                    for kbi, kb in enumerate(kbl):
                        sbk = kbi % 2
                        P.pe(lambda e, sbk=sbk, kb=kb, hg=hg, qa_=qa_: e.matmul(
                            banks[sbk][:], lhsT=kas[:, kb * 128:(kb + 1) * 128],
                            rhs=qa_[:, 4 * hg:4 * hg + 4, :], start=True, stop=True),
                            reads=[Bk, Bqa[j % 2]], writes=[BK[sbk]])
                        z = Zt[kbi % 2]
                        for hh in range(4):
                            h = 4 * hg + hh
                            P.dve(lambda e, z=z, hh=hh, h=h, kbi=kbi, sbk=sbk: e.scalar_tensor_tensor(
                                out=z[:, hh * 128:(hh + 1) * 128], in0=DmT[:, kbi, :], scalar=-slopes[h],
                                in1=banks[sbk][:, hh * 128:(hh + 1) * 128], op0=ALU.mult, op1=ALU.add),
                                reads=[BDm, BK[sbk]], writes=[BZ[kbi % 2][hh]])
                        p_ = Pt[kbi % 2]
                        P.act(lambda e, z=z, p_=p_: e.activation(out=p_[:], in_=z[:], func=AF.Exp),
                              reads=BZ[kbi % 2], writes=[BP[kbi % 2]])
                        P.pe(lambda e, p_=p_, kb=kb, kbi=kbi, nl=len(kbl): e.matmul(banks[ob][:], lhsT=vas[:, kb, :], rhs=p_[:],
                                                                       start=(kbi == 0), stop=(kbi == nl - 1)),
                             reads=[Bk, BP[kbi % 2]], writes=[BK[ob]])
                        P.pe(lambda e, p_=p_, kbi=kbi, nl=len(kbl): e.matmul(banks[db][:], lhsT=ones_bf[:], rhs=p_[:],
                                                                start=(kbi == 0), stop=(kbi == nl - 1)),
                             reads=[B_const, BP[kbi % 2]], writes=[BK[db]])
                    P.dve(lambda e: e.reciprocal(out=rden[:], in_=banks[db][:]), reads=[BK[db]], writes=[Brd])
                    oa = ostA[hg % 2]
                    P.dve(lambda e, oa=oa: e.tensor_tensor(out=oa[:].rearrange("p h t -> p (h t)"), in0=banks[ob][:],
                                                           in1=rden[:], op=ALU.mult),
                          reads=[BK[ob], Brd], writes=[BoA[hg % 2]])
                    P.dma("sp", mixTr[:, 4 * hg:4 * hg + 4, j * 128:(j + 1) * 128], oa[:], reads=[BoA[hg % 2]])
            P.end_stage(touch)

        with ExitStack() as st:
            def sb(name, shape, dt):
                return st.enter_context(sbt(name, list(shape), dt))

            krs = sb("krs", [64, 4096], BF16)
            Bk = Buf()
            P.dma("sp", krs[:], krT, writes=[Bk])
            ctb = sb("ctb", [128, 2048], F32)
            pb_i = sb("pb_i", [128, 2048], I32)
            P.dma("sp", pb_i[:], posr[:, 0:2048].partition_broadcast(128)[:, 0, :], writes=[Bk])
            P.dve(lambda e: e.tensor_scalar(out=pb_i[:], in0=pb_i[:], scalar1=6, scalar2=None,
                                            op0=ALU.arith_shift_right), reads=[Bk], writes=[Bk])
            P.dve(lambda e: e.tensor_copy(out=ctb[:], in_=pb_i[:]), reads=[Bk], writes=[Bk])
            cm = sb("cm", [128, 8, 512], BF16)
            Bcm = Buf()
            kn = [sb("kn%d" % i, [128, 4096], BF16) for i in range(2)]
            vbh = [sb("vbh%d" % i, [128, 32, 128], BF16) for i in range(2)]
            Bkn = [Buf() for _ in range(2)]
            qn = [sb("qn%d" % i, [128, 512], BF16) for i in range(2)]
            qr = [sb("qr%d" % i, [64, 512], BF16) for i in range(2)]
            Bq = [Buf() for _ in range(2)]
            Pt = [sb("PtB%d" % i, [128, 512], BF16) for i in range(3)]
            BP = [Buf() for _ in range(3)]
            rden = sb("rdenB", [128, 512], F32)
            Brd = Buf()
            ostB = [sb("ostB%d" % i, [128, 512], BF16) for i in range(2)]
            BoB = [Buf() for _ in range(2)]
            vbr = vb.rearrange("(b p) e -> p b e", p=128)
            hi_ = 0
            pi_ = 0
            for i in range(4):
                nkb = 4 * (i + 1)
                kbl = list(range(0, nkb)) + list(range(16, 16 + nkb))
                band = list(range(4 * i, 4 * i + 4)) + list(range(16 + 4 * i, 16 + 4 * i + 4))
                for bi_, kb in enumerate(band):
                    P.dve(lambda e, bi_=bi_, kb=kb, i=i: e.tensor_scalar(
                        out=cm[:, bi_, :], in0=ctb[:, i * 512:(i + 1) * 512], scalar1=chkf[:, kb:kb + 1], scalar2=None,
                        op0=ALU.is_ge), reads=[Bk, B_const], writes=[Bcm])
                for h in range(16):
                    s = hi_ % 2
                    hi_ += 1
                    P.dma("sp", kn[s][:, 0:nkb * 128], knT[h * 128:(h + 1) * 128, 0:nkb * 128], writes=[Bkn[s]])
                    P.dma("sp", kn[s][:, 2048:2048 + nkb * 128], knT[h * 128:(h + 1) * 128, 2048:2048 + nkb * 128],
                          writes=[Bkn[s]])
                    P.dma("sp", vbh[s][:, 0:nkb, :], vbr[:, 0:nkb, h * 128:(h + 1) * 128], writes=[Bkn[s]])
                    P.dma("sp", vbh[s][:, 16:16 + nkb, :], vbr[:, 16:16 + nkb, h * 128:(h + 1) * 128], writes=[Bkn[s]])
                    P.dma("sp", qn[s][:], qnT[h * 128:(h + 1) * 128, i * 512:(i + 1) * 512], writes=[Bq[s]])
                    P.dma("sp", qr[s][:], qrT[h * 64:(h + 1) * 64, i * 512:(i + 1) * 512], writes=[Bq[s]])
                    ob = 4 + (h % 2)
                    db = 6 + (h % 2)
                    for kbi, kb in enumerate(kbl):
                        sbk = kbi % 2
                        P.pe(lambda e, sbk=sbk, kb=kb, s=s: e.matmul(banks[sbk][:], lhsT=kn[s][:, kb * 128:(kb + 1) * 128],
                                                                     rhs=qn[s][:], start=True, stop=False),
                             reads=[Bkn[s], Bq[s]], writes=[BK[sbk]])
                        P.pe(lambda e, sbk=sbk, kb=kb, s=s: e.matmul(banks[sbk][:], lhsT=krs[:, kb * 128:(kb + 1) * 128],
                                                                     rhs=qr[s][:], start=False, stop=True),
                             reads=[Bk, Bq[s]], writes=[BK[sbk]])
                        pp = pi_ % 3
                        pi_ += 1
                        p_ = Pt[pp]
                        P.act(lambda e, p_=p_, sbk=sbk: e.activation(out=p_[:], in_=banks[sbk][:], func=AF.Exp),
                              reads=[BK[sbk]], writes=[BP[pp]])
                        if kb in band:
                            bi_ = band.index(kb)
                            P.dve(lambda e, p_=p_, bi_=bi_: e.tensor_tensor(out=p_[:], in0=p_[:], in1=cm[:, bi_, :],
                                                                            op=ALU.mult),
                                  reads=[BP[pp], Bcm], writes=[BP[pp]])
                        P.pe(lambda e, p_=p_, kb=kb, kbi=kbi, s=s, ob=ob, nl=len(kbl): e.matmul(
                            banks[ob][:], lhsT=vbh[s][:, kb, :], rhs=p_[:], start=(kbi == 0),
                            stop=(kbi == nl - 1)), reads=[Bkn[s], BP[pp]], writes=[BK[ob]])
                        P.pe(lambda e, p_=p_, kbi=kbi, db=db, nl=len(kbl): e.matmul(
                            banks[db][:], lhsT=ones_bf[:], rhs=p_[:], start=(kbi == 0), stop=(kbi == nl - 1)),
                            reads=[B_const, BP[pp]], writes=[BK[db]])
                    P.dve(lambda e, db=db: e.reciprocal(out=rden[:], in_=banks[db][:]), reads=[BK[db]], writes=[Brd])
                    o_ = ostB[h % 2]
                    P.dve(lambda e, o_=o_, ob=ob: e.tensor_tensor(out=o_[:], in0=banks[ob][:], in1=rden[:], op=ALU.mult),
                          reads=[BK[ob], Brd], writes=[BoB[h % 2]])
                    P.dma("sp", mixT[(16 + h) * 128:(17 + h) * 128, i * 512:(i + 1) * 512], o_[:], reads=[BoB[h % 2]])
            P.end_stage(touch)

        with ExitStack() as st:
            def sb(name, shape, dt):
                return st.enter_context(sbt(name, list(shape), dt))

            ws = WStream(st)
            xch = [sb("xch%d" % i, [128, 512], F32) for i in range(2)]
            Bxc = [Buf() for _ in range(2)]
            sq = [sb("sq%d" % i, [128, 512], BF16) for i in range(2)]
            Bsq = [Buf() for _ in range(2)]
            ci = [0]

            def wo_epi(ec):
                def epi(tt, b):
                    s = ci[0] % 2
                    ci[0] += 1
                    P.dma("sp", xch[s][:], xT[ec * 128:(ec + 1) * 128, tt * 512:(tt + 1) * 512], writes=[Bxc[s]])
                    P.dve(lambda e, s=s, b=b: e.scalar_tensor_tensor(
                        out=xch[s][:], in0=banks[b][:], scalar=gate1[:, ec:ec + 1], in1=xch[s][:], op0=ALU.mult,
                        op1=ALU.add), reads=[BK[b], B_mod, Bxc[s]], writes=[Bxc[s]])
                    P.dma("sp", x2T[ec * 128:(ec + 1) * 128, tt * 512:(tt + 1) * 512], xch[s][:], reads=[Bxc[s]])
                    P.act(lambda e, s=s: e.activation(out=sq[s][:], in_=xch[s][:], func=AF.Square), reads=[Bxc[s]],
                          writes=[Bsq[s]])
                    P.pe(lambda e, s=s: e.matmul(banks[4][:], lhsT=ones_bf[:], rhs=sq[s][:], start=(ec == 0),
                                                 stop=(ec == 31)), reads=[B_const, Bsq[s]], writes=[BK[4]])
                    if ec == 31:
                        rsqrt_ops(r2b[:, tt * 512:(tt + 1) * 512], banks[4][:], 1.0 / D, [BK[4]], [B_r2b])
                return epi

            jobs = [fm_job(ws, w_o, 32, ec * 128, 128, wo_epi(ec)) for ec in range(32)]
            run_gemm(st, mixT, 32, 4, jobs, xbufs=2)
            P.end_stage(touch)

        with ExitStack() as st:
            def sb(name, shape, dt):
                return st.enter_context(sbt(name, list(shape), dt))

            xch = [sb("xch%d" % i, [128, 512], F32) for i in range(3)]
            Bxc = [Buf() for _ in range(3)]
            hch = [sb("hch%d" % i, [128, 512], BF16) for i in range(3)]
            Bhc = [Buf() for _ in range(3)]
            n = 0
            for tt in range(4):
                for k in range(32):
                    s = n % 3
                    n += 1
                    P.dma("sp", xch[s][:], x2T[k * 128:(k + 1) * 128, tt * 512:(tt + 1) * 512], writes=[Bxc[s]])
                    P.dve(lambda e, s=s, k=k, tt=tt: e.scalar_tensor_tensor(
                        out=xch[s][:], in0=xch[s][:], scalar=gs2[:, k:k + 1], in1=r2b[:, tt * 512:(tt + 1) * 512],
                        op0=ALU.mult, op1=ALU.mult), reads=[Bxc[s], B_mod, B_r2b], writes=[Bxc[s]])
                    P.act(lambda e, s=s, k=k: e.activation(out=hch[s][:], in_=xch[s][:], func=AF.Identity,
                                                           bias=sh2[:, k:k + 1], scale=1.0),
                          reads=[Bxc[s], B_mod], writes=[Bhc[s]])
                    P.dma("sp", h2T[k * 128:(k + 1) * 128, tt * 512:(tt + 1) * 512], hch[s][:], reads=[Bhc[s]])
            P.end_stage(touch)

        with ExitStack() as st:
            def sb(name, shape, dt):
                return st.enter_context(sbt(name, list(shape), dt))

            ws = WStream(st)
            u = [sb("u%d" % i, [128, 512], BF16) for i in range(3)]
            Bu = [Buf() for _ in range(3)]
            ci = [0]

            def m1_epi(fc):
                def epi(tt, b):
                    s = ci[0] % 3
                    ci[0] += 1
                    P.act(lambda e, s=s, b=b: e.activation(out=u[s][:], in_=banks[b][:], func=AF.Relu),
                          reads=[BK[b]], writes=[Bu[s]])
                    P.dve(lambda e, s=s: e.tensor_tensor(out=u[s][:], in0=u[s][:], in1=u[s][:], op=ALU.mult),
                          reads=[Bu[s]], writes=[Bu[s]])
                    P.dma("sp", hidT[fc * 128:(fc + 1) * 128, tt * 512:(tt + 1) * 512], u[s][:], reads=[Bu[s]])
                return epi

            jobs = [fm_job(ws, w1, 32, fc * 128, 128, m1_epi(fc)) for fc in range(128)]
            run_gemm(st, h2T, 32, 4, jobs, xbufs=2)
            P.end_stage(touch)

        with ExitStack() as st:
            def sb(name, shape, dt):
                return st.enter_context(sbt(name, list(shape), dt))

            ws = WStream(st)
            xch = [sb("xch%d" % i, [128, 512], F32) for i in range(2)]
            Bxc = [Buf() for _ in range(2)]
            sq = [sb("sq%d" % i, [128, 512], BF16) for i in range(2)]
            Bsq = [Buf() for _ in range(2)]
            sscol = sb("sscol", [128, 16], F32)
            Bss = Buf()
            P.pool(lambda e: e.memset(sscol[:], 0.0), writes=[Bss])
            ci = [0]

            def m2_epi(ec):
                def epi(tt, b):
                    s = ci[0] % 2
                    ci[0] += 1
                    P.dma("sp", xch[s][:], x2T[ec * 128:(ec + 1) * 128, tt * 512:(tt + 1) * 512], writes=[Bxc[s]])
                    P.dve(lambda e, s=s, b=b: e.scalar_tensor_tensor(
                        out=xch[s][:], in0=banks[b][:], scalar=gate2[:, ec:ec + 1], in1=xch[s][:], op0=ALU.mult,
                        op1=ALU.add), reads=[BK[b], B_mod, Bxc[s]], writes=[Bxc[s]])
                    P.act(lambda e, s=s: e.activation(out=sq[s][:], in_=xch[s][:], func=AF.Square), reads=[Bxc[s]],
                          writes=[Bsq[s]])
                    for tb in range(4):
                        P.pe(lambda e, s=s, tb=tb: e.matmul(banks[4][:, tb:tb + 1], lhsT=sq[s][:, tb * 128:(tb + 1) * 128],
                                                            rhs=ones_bf[:, 0:1], start=True, stop=True),
                             reads=[B_const, Bsq[s]], writes=[BK[4]])
                    P.dve(lambda e, tt=tt: e.tensor_tensor(out=sscol[:, tt * 4:tt * 4 + 4], in0=sscol[:, tt * 4:tt * 4 + 4],
                                                           in1=banks[4][:, 0:4], op=ALU.add),
                          reads=[BK[4], Bss], writes=[Bss])
                    P.dve(lambda e, s=s: e.tensor_scalar(out=xch[s][:], in0=xch[s][:], scalar1=fg[:, ec:ec + 1],
                                                         scalar2=None, op0=ALU.mult),
                          reads=[Bxc[s], Bsq[s], B_const], writes=[Bxc[s]])
                    P.dma("sp", x3T[ec * 128:(ec + 1) * 128, tt * 512:(tt + 1) * 512], xch[s][:], reads=[Bxc[s]])
                    if ec == 31 and tt == 3:
                        rsqrt_ops(rstd3[:], sscol[:], 1.0 / D, [Bss], [B_r3])
                return epi

            jobs = [fm_job(ws, w2, 128, ec * 128, 128, m2_epi(ec)) for ec in range(32)]
            run_gemm(st, hidT, 128, 4, jobs, xbufs=1)
            P.end_stage(touch)

        with ExitStack() as st:
            def sb(name, shape, dt):
                return st.enter_context(sbt(name, list(shape), dt))

            x3s = [sb("x3s%d" % i, [128, 32, 128], F32) for i in range(2)]
            Bx3 = [Buf() for _ in range(2)]
            yst = [sb("yst%d" % i, [128, 4096], F32) for i in range(2)]
            By = [Buf() for _ in range(2)]
            x3Tr = x3T.rearrange("(k p) t -> p k t", p=128)
            for blk in range(16):
                s = blk % 2
                P.dma("sp", x3s[s][:], x3Tr[:, :, blk * 128:(blk + 1) * 128], writes=[Bx3[s]])
                for k in range(32):
                    b = (k // 4) % 2
                    P.pe(lambda e, s=s, k=k, b=b: e.transpose(banks[b][:, (k % 4) * 128:(k % 4 + 1) * 128], x3s[s][:, k, :],
                                                              ident[:]), reads=[Bx3[s], B_const], writes=[BK[b]])
                    if k % 4 == 3:
                        P.act(lambda e, s=s, k=k, b=b, blk=blk: e.activation(
                            out=yst[s][:, (k - 3) * 128:(k + 1) * 128], in_=banks[b][:], func=AF.Identity,
                            scale=rstd3[:, blk:blk + 1]), reads=[BK[b], B_r3], writes=[By[s]])
                P.dma("sp", y[blk * 128:(blk + 1) * 128, :], yst[s][:], reads=[By[s]])
            P.end_stage(touch)
    return nc


_NC_CACHE = {}


def kernel(x, c, positions, w_ada, b_ada, ln1_g, w_in, q_norm_g, kv_norm_g, w_uq, w_uk, w_uv, w_o, ln2_g,
           w_mlp_in, w_mlp_out, final_g):
    x = np.asarray(x)
    positions = np.asarray(positions)
    B, S, _ = x.shape

    def colT(v, k):
        return np.ascontiguousarray(np.asarray(v, dtype=np.float32).reshape(k, 128).T)

    shared = {
        "w_ada": np.ascontiguousarray(np.asarray(w_ada)[0]), "b_adaT": colT(np.asarray(b_ada)[0], 192),
        "g1T": colT(np.asarray(ln1_g)[0], 32), "w_in": np.ascontiguousarray(np.asarray(w_in)[0]),
        "qgT": colT(np.asarray(q_norm_g)[0], 8), "kvgT": colT(np.asarray(kv_norm_g)[0], 4),
        "w_uq": np.ascontiguousarray(np.asarray(w_uq)[0]), "w_uk": np.ascontiguousarray(np.asarray(w_uk)[0]),
        "w_uv": np.ascontiguousarray(np.asarray(w_uv)[0]), "w_o": np.ascontiguousarray(np.asarray(w_o)[0]),
        "g2T": colT(np.asarray(ln2_g)[0], 32), "w1": np.ascontiguousarray(np.asarray(w_mlp_in)[0]),
        "w2": np.ascontiguousarray(np.asarray(w_mlp_out)[0]), "fgT": colT(np.asarray(final_g), 32),
    }
    in_maps = []
    perms = []
    for core in range(8):
        b, p = core // 2, core % 2
        own = [2 * j + p for j in range(16)]
        oth = [2 * j + 1 - p for j in range(16)]
        blocks = own + oth
        idx = np.concatenate([np.arange(g * 128, (g + 1) * 128) for g in blocks])
        perms.append((b, own))
        m = dict(shared)
        m["xc"] = np.ascontiguousarray(x[b][idx])
        pp = np.ascontiguousarray(positions[b][idx].astype(np.int32))
        m["posr"] = pp.reshape(1, 4096)
        m["posc"] = np.ascontiguousarray(pp.reshape(32, 128).T)
        m["cT"] = colT(np.asarray(c)[b], 32)
        in_maps.append(m)
    if "nc" not in _NC_CACHE:
        _NC_CACHE["nc"] = build_nc()
    res = run_bass_kernel_spmd(_NC_CACHE["nc"], in_maps, core_ids=list(range(8)))
    out = np.empty((B, S, D), dtype=np.float32)
    for core in range(8):
        b, own = perms[core]
        yv = np.asarray(res.results[core]["y"]).reshape(16, 128, D)
        for j, g in enumerate(own):
            out[b, g * 128:(g + 1) * 128, :] = yv[j]
    return out
```

```python
import math
import os
from contextlib import ExitStack
import numpy as np
import concourse.bass as bass
import concourse.mybir as mybir
from concourse.bass_utils import run_bass_kernel_spmd

F32 = mybir.dt.float32
BF16 = mybir.dt.bfloat16
I32 = mybir.dt.int32
AF = mybir.ActivationFunctionType
ALU = mybir.AluOpType
AX = mybir.AxisListType

ENGS = ("pe", "act", "dve", "pool", "sp")
SAME_ENGINE_SYNC = os.environ.get("KSES", "1") == "1"
D = 4096
EPS = 1e-6
NBIS = 20


import os
MAXSTAGE = int(os.environ.get("KSTAGE", "99"))
DBG_OUT = set(filter(None, os.environ.get("KDBG", "").split(",")))


class StopBuild(Exception):
    pass


class Buf:
    __slots__ = ("writers", "readers")

    def __init__(self):
        self.writers = []
        self.readers = []


class Op:
    __slots__ = ("eng", "fn", "deps", "is_dma", "seq", "signals", "dsem", "dtarget", "emitted", "touch")

    def __init__(self, eng, fn, is_dma):
        self.eng = eng
        self.fn = fn
        self.deps = []
        self.is_dma = is_dma
        self.seq = None
        self.signals = False
        self.dsem = None
        self.dtarget = None
        self.emitted = False
        self.touch = False


class Prog:
    def __init__(self, nc, stack, n_dma_sems=10):
        self.nc = nc
        self.pending = {e: [] for e in ENGS}
        self.stage_deps = {e: [] for e in ENGS}
        self.csem = {e: stack.enter_context(nc.semaphore("c_" + e)) for e in ENGS}
        self.dsems = {e: [stack.enter_context(nc.semaphore("d_%s_%d" % (e, i))) for i in range(n_dma_sems)]
                      for e in ("sp", "pool")}
        self.cnt = {e: 0 for e in ENGS}
        self.dma_k = {e: 0 for e in self.dsems}
        self.dma_uses = {e: [0] * n_dma_sems for e in self.dsems}
        self.waited = {e: {} for e in ENGS}
        self.stage_no = 0

    def add(self, eng, fn, reads=(), writes=(), dma=False, extra_deps=()):
        op = Op(eng, fn, dma)
        deps = []
        for b in reads:
            deps.extend(b.writers)
        for b in writes:
            deps.extend(b.writers)
            deps.extend(b.readers)
        deps.extend(extra_deps)
        if self.stage_deps[eng]:
            deps.extend(self.stage_deps[eng])
            self.stage_deps[eng] = []
        seen = set()
        for d in deps:
            if d is op or id(d) in seen or (d.emitted and not d.touch):
                continue
            seen.add(id(d))
            op.deps.append(d)
            if not d.is_dma and not (d.eng == eng and (eng == "pe" or not SAME_ENGINE_SYNC)):
                d.signals = True
        for b in reads:
            b.readers.append(op)
            if len(b.readers) > 48:
                b.readers = b.readers[-48:]
        for b in writes:
            if b.readers:
                b.readers = []
                b.writers = [op]
            else:
                b.writers.append(op)
                if len(b.writers) > 48:
                    b.writers = b.writers[-48:]
        self.pending[eng].append(op)
        return op

    def pe(self, fn, reads=(), writes=(), **kw):
        return self.add("pe", fn, reads, writes, **kw)

    def act(self, fn, reads=(), writes=(), **kw):
        return self.add("act", fn, reads, writes, **kw)

    def dve(self, fn, reads=(), writes=(), **kw):
        return self.add("dve", fn, reads, writes, **kw)

    def pool(self, fn, reads=(), writes=(), **kw):
        return self.add("pool", fn, reads, writes, **kw)

    def dma(self, q, out, in_, reads=(), writes=(), slow=False, **kw):
        if slow:
            fn = lambda e: e.dma_start(out=out, in_=in_, allow_slow_non_contiguous=True)
        else:
            fn = lambda e: e.dma_start(out=out, in_=in_)
        return self.add(q, fn, reads, writes, dma=True, **kw)

    def end_stage(self, touch):
        nc = self.nc
        tops = []
        for e in ("act", "dve", "pool", "sp"):
            t = Op(e, touch[e], e == "sp")
            t.signals = True
            t.touch = True
            self.pending[e].append(("T", t))
            tops.append(t)
        for e in ENGS:
            for item in self.pending[e]:
                op = item[1] if isinstance(item, tuple) else item
                if op.is_dma:
                    i = self.dma_k[e] % len(self.dsems[e])
                    self.dma_k[e] += 1
                    self.dma_uses[e][i] += 1
                    op.dsem = (e, i)
                    op.dtarget = 16 * self.dma_uses[e][i]
                elif op.signals:
                    self.cnt[e] += 1
                    op.seq = self.cnt[e]
        with nc.Block() as block:
            engmap = {"pe": block.tensor, "act": block.scalar, "dve": block.vector, "pool": block.gpsimd,
                      "sp": block.sync}

            def make(ename):
                items = self.pending[ename]
                waited = self.waited[ename]

                def body(eng):
                    def wait(key, sem, val):
                        if waited.get(key, 0) >= val:
                            return
                        waited[key] = val
                        eng.wait_ge(sem, val)

                    def wait_all_dma():
                        if ename in self.dsems:
                            for i, s in enumerate(self.dsems[ename]):
                                tot = 16 * self.dma_uses[ename][i]
                                if tot:
                                    wait(("d", ename, i), s, tot)

                    for item in items:
                        if isinstance(item, tuple):
                            op = item[1]
                            if ename in self.dsems:
                                for i, s in enumerate(self.dsems[ename]):
                                    tot = 16 * self.dma_uses[ename][i]
                                    if op.is_dma and op.dsem == (ename, i):
                                        tot -= 16
                                    if tot:
                                        wait(("d", ename, i), s, tot)
                        else:
                            op = item
                        if op.is_dma and op.dtarget > 16:
                            wait(("d",) + op.dsem, self.dsems[op.dsem[0]][op.dsem[1]], op.dtarget - 16)
                        for d in op.deps:
                            if d.is_dma:
                                wait(("d",) + d.dsem, self.dsems[d.dsem[0]][d.dsem[1]], d.dtarget)
                            else:
                                if d.eng == ename and (ename == "pe" or not SAME_ENGINE_SYNC):
                                    continue
                                wait(("c", d.eng), self.csem[d.eng], d.seq)
                        ins = op.fn(eng)
                        if op.is_dma:
                            ins.then_inc(self.dsems[op.dsem[0]][op.dsem[1]], 16)
                        elif op.signals:
                            ins.then_inc(self.csem[ename], 1)
                    wait_all_dma()

                return body

            for e in ENGS:
                if self.pending[e]:
                    engmap[e](make(e))
        for e in ENGS:
            for item in self.pending[e]:
                (item[1] if isinstance(item, tuple) else item).emitted = True
        self.pending = {e: [] for e in ENGS}
        for e in ENGS:
            self.stage_deps[e] = list(tops)
        self.stage_no += 1
        if self.stage_no > MAXSTAGE:
            raise StopBuild()


def build_nc():
    nc = bass.Bass("TRN2", target_bir_lowering=False)

    def inp(name, shape, dt=F32):
        return nc.dram_tensor(name, list(shape), dt, kind="ExternalInput").ap()

    _uc = [0]

    def sbt(name, shape, dt):
        _uc[0] += 1
        return nc.sbuf_tensor("%s_u%d" % (name, _uc[0]), shape, dt)

    def scr(name, shape, dt):
        return nc.dram_tensor(name, list(shape), dt, kind=("ExternalOutput" if name in DBG_OUT else "Internal")).ap()

    xc = inp("xc", [4096, 4096])
    posr = inp("posr", [1, 4096], I32)
    posc = inp("posc", [128, 32], I32)
    cT = inp("cT", [128, 32])
    w_ada = inp("w_ada", [4096, 24576])
    b_adaT = inp("b_adaT", [128, 192])
    g1T = inp("g1T", [128, 32])
    w_in = inp("w_in", [4096, 8160])
    qgT = inp("qgT", [128, 8])
    kvgT = inp("kvgT", [128, 4])
    w_uq = inp("w_uq", [1024, 3072])
    w_uk = inp("w_uk", [512, 2048])
    w_uv = inp("w_uv", [512, 2048])
    w_o = inp("w_o", [4096, 4096])
    g2T = inp("g2T", [128, 32])
    w1 = inp("w1", [4096, 16384])
    w2 = inp("w2", [16384, 4096])
    fgT = inp("fgT", [128, 32])
    y = nc.dram_tensor("y", [2048, 4096], F32, kind="ExternalOutput").ap()

    h1T = scr("h1T", [4096, 4096], BF16)
    xT = scr("xT", [4096, 2048], F32)
    kaT = scr("kaT", [128, 4096], BF16)
    va = scr("va", [4096, 128], BF16)
    kiT = scr("kiT", [128, 4096], BF16)
    krT = scr("krT", [64, 4096], BF16)
    kvnT = scr("kvnT", [512, 4096], BF16)
    knT = scr("knT", [2048, 4096], BF16)
    vb = scr("vb", [4096, 2048], BF16)
    qaT = scr("qaT", [2048, 2048], BF16)
    qiT = scr("qiT", [4096, 2048], BF16)
    wi3 = scr("wi3", [16 * 32 * 128], F32)
    cqnT = scr("cqnT", [1024, 2048], BF16)
    qnT = scr("qnT", [2048, 2048], BF16)
    qrT = scr("qrT", [16 * 64, 2048], BF16)
    mixT = scr("mixT", [4096, 2048], BF16)
    x2T = scr("x2T", [4096, 2048], F32)
    h2T = scr("h2T", [4096, 2048], BF16)
    hidT = scr("hidT", [16384, 2048], BF16)
    x3T = scr("x3T", [4096, 2048], F32)
    tdr = scr("tdr", [1, 64], F32)

    try:
        _build_body(nc, locals())
    except StopBuild:
        pass
    return nc


def _build_body(nc, L):
    globals().update({k: v for k, v in L.items() if k not in ("nc",)})
    with ExitStack() as gst:
        P = Prog(nc, gst)

        def gsb(name, shape, dt):
            return gst.enter_context(sbt(name, list(shape), dt))

        ident = gsb("ident", [128, 128], F32)
        ones_bf = gsb("ones_bf", [128, 128], BF16)
        modT = gsb("modT", [128, 192], F32)
        gs1 = gsb("gs1", [128, 32], F32)
        gs2 = gsb("gs2", [128, 32], F32)
        fg = gsb("fg", [128, 32], F32)
        kvg = gsb("kvg", [128, 4], F32)
        qg = gsb("qg", [128, 8], F32)
        posf = gsb("posf", [128, 32], F32)
        chkf = gsb("chkf", [128, 32], F32)
        cosT = gsb("cosT", [128, 32, 32], F32)
        sinT = gsb("sinT", [128, 32, 32], F32)
        r2b = gsb("r2b", [128, 2048], F32)
        rstd3 = gsb("rstd3", [128, 16], F32)
        tch = gsb("tch", [128, 8], F32)
        B_const = Buf()
        B_mod = Buf()
        B_r2b = Buf()
        B_r3 = Buf()
        banks = [gst.enter_context(nc.psum_tensor("bank%d" % i, [128, 512], F32)) for i in range(8)]
        BK = [Buf() for _ in range(8)]
        sh1 = modT[:, 0:32]
        gate1 = modT[:, 64:96]
        sh2 = modT[:, 96:128]
        gate2 = modT[:, 160:192]

        touch = {
            "act": lambda e: e.activation(out=tch[0:1, 0:1], in_=tch[0:1, 1:2], func=AF.Copy),
            "dve": lambda e: e.tensor_copy(out=tch[0:1, 2:3], in_=tch[0:1, 3:4]),
            "pool": lambda e: e.memset(tch[0:1, 4:5], 0.0),
            "sp": lambda e: e.dma_start(out=tdr[0:1, 0:2], in_=tch[0:1, 6:8]),
        }

        def rsqrt_ops(dst, src, scale, reads, writes):
            P.dve(lambda e: e.tensor_scalar(out=dst, in0=src, scalar1=scale, scalar2=EPS, op0=ALU.mult, op1=ALU.add),
                  reads=reads, writes=writes)
            P.act(lambda e: e.activation(out=dst, in_=dst, func=AF.Sqrt), reads=writes, writes=writes)
            P.dve(lambda e: e.reciprocal(out=dst, in_=dst), reads=writes, writes=writes)

        with ExitStack() as st:
            def sb(name, shape, dt):
                return st.enter_context(sbt(name, list(shape), dt))

            io_i = sb("io_i", [128, 128], I32)
            P.pool(lambda e: e.iota(io_i[:], pattern=[[1, 128]], base=0, channel_multiplier=-1), writes=[B_const])
            P.dve(lambda e: e.tensor_copy(out=ident[:], in_=io_i[:]), reads=[B_const], writes=[B_const])
            P.dve(lambda e: e.tensor_scalar(out=ident[:], in0=ident[:], scalar1=0.0, scalar2=None, op0=ALU.is_equal),
                  reads=[B_const], writes=[B_const])
            P.pool(lambda e: e.memset(ones_bf[:], 1.0), writes=[B_const])
            P.pool(lambda e: e.memset(tch[:], 0.0), writes=[B_const])
            Bp = Buf()
            ct_sb = sb("ct_sb", [128, 32], F32)
            badaT = sb("badaT", [128, 192], F32)
            g1s = sb("g1s", [128, 32], F32)
            g2s = sb("g2s", [128, 32], F32)
            posc_i = sb("posc_i", [128, 32], I32)
            chk_i = sb("chk_i", [128, 32], I32)
            for (dst, src) in ((ct_sb, cT), (badaT, b_adaT), (g1s, g1T), (g2s, g2T), (fg, fgT), (kvg, kvgT),
                               (qg, qgT), (posc_i, posc)):
                P.dma("sp", dst[:], src, writes=[Bp])
            P.dve(lambda e: e.tensor_copy(out=posf[:], in_=posc_i[:]), reads=[Bp], writes=[B_const])
            P.dve(lambda e: e.tensor_scalar(out=chk_i[:], in0=posc_i[:], scalar1=6, scalar2=None,
                                            op0=ALU.arith_shift_right), reads=[Bp], writes=[Bp])
            P.dve(lambda e: e.tensor_copy(out=chkf[:], in_=chk_i[:]), reads=[Bp], writes=[B_const])
            inv_i = sb("inv_i", [128, 32], I32)
            invf = sb("invf", [128, 32], F32)
            for i_ in range(32):
                P.pool(lambda e, i_=i_: e.memset(invf[:, i_:i_ + 1], float(np.float32(10000.0) ** np.float32(-2.0 * i_ / 64.0))),
                       writes=[Bp])
            rr = sb("rr", [128, 32, 32], F32)
            ri2 = [sb("ri%d" % i_, [128, 32, 32], I32) for i_ in range(2)]
            rf2 = [sb("rf%d" % i_, [128, 32, 32], F32) for i_ in range(2)]
            Br = Buf()
            for blk in range(32):
                P.dve(lambda e, blk=blk: e.tensor_scalar(out=rr[:, blk, :], in0=invf[:], scalar1=posf[:, blk:blk + 1],
                                                         scalar2=1.0 / (2 * math.pi), op0=ALU.mult, op1=ALU.mult),
                      reads=[Bp, B_const], writes=[Br])
            Bt = Buf()
            for (tab, off) in ((cosT, 0.25), (sinT, 0.0)):
                rrf = rr[:].rearrange("p a b -> p (a b)")
                rif = ri2[0 if off else 1][:].rearrange("p a b -> p (a b)")
                rff = rf2[0 if off else 1][:].rearrange("p a b -> p (a b)")
                tabf = tab[:].rearrange("p a b -> p (a b)")
                P.dve(lambda e, off=off, rff=rff, rrf=rrf: e.tensor_scalar(out=rff, in0=rrf, scalar1=off, scalar2=None,
                                                                           op0=ALU.add), reads=[Br], writes=[Bt])
                P.dve(lambda e, rff=rff, rif=rif: e.tensor_copy(out=rif, in_=rff), reads=[Bt], writes=[Bt])
                P.dve(lambda e, tabf=tabf, rif=rif: e.tensor_copy(out=tabf, in_=rif), reads=[Bt], writes=[B_const])
                P.dve(lambda e, rff=rff, tabf=tabf: e.tensor_tensor(out=rff, in0=rff, in1=tabf, op=ALU.subtract),
                      reads=[Bt, B_const], writes=[Bt])
                P.dve(lambda e, rff=rff, tabf=tabf: e.tensor_scalar(out=tabf, in0=rff, scalar1=0.5, scalar2=None,
                                                                    op0=ALU.is_gt), reads=[Bt], writes=[B_const])
                P.dve(lambda e, rff=rff, tabf=tabf: e.tensor_tensor(out=rff, in0=rff, in1=tabf, op=ALU.subtract),
                      reads=[Bt, B_const], writes=[Bt])
                P.dve(lambda e, rff=rff, tabf=tabf: e.tensor_scalar(out=tabf, in0=rff, scalar1=-0.5, scalar2=None,
                                                                    op0=ALU.is_lt), reads=[Bt], writes=[B_const])
                P.dve(lambda e, rff=rff, tabf=tabf: e.tensor_tensor(out=rff, in0=rff, in1=tabf, op=ALU.add),
                      reads=[Bt, B_const], writes=[Bt])
                P.act(lambda e, rff=rff, tabf=tabf: e.activation(out=tabf, in_=rff, func=AF.Sin, scale=2 * math.pi),
                      reads=[Bt], writes=[B_const])
            if "dbgtab" in DBG_OUT:
                dbgtab = scr("dbgtab", [128, 3072], F32)
                P.dma("sp", dbgtab[:, 0:1024], cosT[:].rearrange("p a b -> p (a b)"), reads=[B_const, Bt])
                P.dma("sp", dbgtab[:, 1024:2048], sinT[:].rearrange("p a b -> p (a b)"), reads=[B_const, Bt])
                P.dma("sp", dbgtab[:, 2048:3072], rr[:].rearrange("p a b -> p (a b)"), reads=[Br, Bt])
            scT = sb("scT", [128, 32], BF16)
            P.act(lambda e: e.activation(out=scT[:], in_=ct_sb[:], func=AF.Silu), reads=[Bp], writes=[Bp])
            wts = [sb("wa%d" % i, [128, 32, 128], BF16) for i in range(3)]
            Bw = [Buf() for _ in range(3)]
            war = w_ada.rearrange("(k p) e -> p k e", p=128)
            for ec in range(192):
                s = ec % 3
                P.dma("pool", wts[s][:], war[:, :, ec * 128:(ec + 1) * 128], writes=[Bw[s]])
                for k in range(32):
                    P.pe(lambda e, s=s, k=k, ec=ec: e.matmul(banks[0][:, ec:ec + 1], lhsT=wts[s][:, k, :],
                                                             rhs=scT[:, k:k + 1], start=(k == 0), stop=(k == 31)),
                         reads=[Bw[s], Bp], writes=[BK[0]])
            P.dve(lambda e: e.tensor_tensor(out=modT[:], in0=banks[0][:, 0:192], in1=badaT[:], op=ALU.add),
                  reads=[BK[0], Bp], writes=[B_mod])
            P.dve(lambda e: e.scalar_tensor_tensor(out=gs1[:], in0=modT[:, 32:64], scalar=1.0, in1=g1s[:],
                                                   op0=ALU.add, op1=ALU.mult), reads=[B_mod, Bp], writes=[B_mod])
            P.dve(lambda e: e.scalar_tensor_tensor(out=gs2[:], in0=modT[:, 128:160], scalar=1.0, in1=g2s[:],
                                                   op0=ALU.add, op1=ALU.mult), reads=[B_mod, Bp], writes=[B_mod])
            P.end_stage(touch)

        with ExitStack() as st:
            def sb(name, shape, dt):
                return st.enter_context(sbt(name, list(shape), dt))

            xb = [sb("xb%d" % i, [128, 4096], F32) for i in range(2)]
            Bx = [Buf() for _ in range(2)]
            junk = sb("junk", [128, 4096], BF16)
            Bj = Buf()
            hst = [sb("hst%d" % i, [128, 32, 512], BF16) for i in range(2)]
            Bh = [Buf() for _ in range(2)]
            xst = [sb("xst%d" % i, [128, 32, 128], F32) for i in range(2)]
            Bxs = [Buf() for _ in range(2)]
            ssq = sb("ssq", [128, 32], F32)
            Bs = Buf()
            h1Tr = h1T.rearrange("(k p) t -> p k t", p=128)
            xTr = xT.rearrange("(k p) t -> p k t", p=128)
            bi = 0
            for blk in range(32):
                s = blk % 2
                x_ = xb[s]
                P.dma("sp", x_[:], xc[blk * 128:(blk + 1) * 128, :], writes=[Bx[s]])
                if blk < 16:
                    xs = xst[blk % 2]
                    for k in range(32):
                        b = bi % 2
                        bi_k = k % 4
                        P.pe(lambda e, x_=x_, k=k, b=b, bi_k=bi_k: e.transpose(
                            banks[b][:, bi_k * 128:(bi_k + 1) * 128], x_[:, k * 128:(k + 1) * 128], ident[:]),
                            reads=[Bx[s], B_const], writes=[BK[b]])
                        if bi_k == 3:
                            P.dve(lambda e, xs=xs, k=k, b=b: e.tensor_copy(
                                out=xs[:, k - 3:k + 1, :], in_=banks[b][:].rearrange("p (a t) -> p a t", a=4)),
                                reads=[BK[b]], writes=[Bxs[blk % 2]])
                            bi += 1
                    P.dma("sp", xTr[:, :, blk * 128:(blk + 1) * 128], xs[:], reads=[Bxs[blk % 2]])
                P.act(lambda e, x_=x_, blk=blk: e.activation(out=junk[:], in_=x_[:], func=AF.Square,
                                                             accum_out=ssq[:, blk:blk + 1]),
                      reads=[Bx[s]], writes=[Bj, Bs])
                rsqrt_ops(ssq[:, blk:blk + 1], ssq[:, blk:blk + 1], 1.0 / D, [Bs], [Bs])
                P.dve(lambda e, x_=x_, blk=blk: e.tensor_scalar(out=x_[:], in0=x_[:], scalar1=ssq[:, blk:blk + 1],
                                                                scalar2=None, op0=ALU.mult),
                      reads=[Bx[s], Bs], writes=[Bx[s]])
                hs = hst[(blk // 4) % 2]
                Bhs = Bh[(blk // 4) % 2]
                tb = blk % 4
                for k in range(32):
                    b = 2 + (bi % 2)
                    bi_k = k % 4
                    P.pe(lambda e, x_=x_, k=k, b=b, bi_k=bi_k: e.transpose(
                        banks[b][:, bi_k * 128:(bi_k + 1) * 128], x_[:, k * 128:(k + 1) * 128], ident[:]),
                        reads=[Bx[s], B_const], writes=[BK[b]])
                    if bi_k == 3:
                        for kk in range(k - 3, k + 1):
                            P.act(lambda e, hs=hs, kk=kk, b=b, tb=tb: e.activation(
                                out=hs[:, kk, tb * 128:(tb + 1) * 128], in_=banks[b][:, (kk % 4) * 128:(kk % 4 + 1) * 128],
                                func=AF.Identity, scale=gs1[:, kk:kk + 1], bias=sh1[:, kk:kk + 1]),
                                reads=[BK[b], B_mod], writes=[Bhs])
                        bi += 1
                if tb == 3:
                    t0 = (blk // 4) * 512
                    P.dma("sp", h1Tr[:, :, t0:t0 + 512], hs[:], reads=[Bhs])
            P.end_stage(touch)

        def run_gemm(st, XT, KC, n_tt, jobs, xbufs=1, pre_tt=None):
            xts = [st.enter_context(sbt("xt%d" % i, [128, KC, 512], BF16)) for i in range(xbufs)]
            Bxt = [Buf() for _ in range(xbufs)]
            XTr = XT.rearrange("(k p) t -> p k t", p=128)
            for tt in range(n_tt):
                s = tt % xbufs
                for k0 in range(0, KC, 32):
                    k1 = min(KC, k0 + 32)
                    P.dma("sp", xts[s][:, k0:k1, :], XTr[:, k0:k1, tt * 512:(tt + 1) * 512], writes=[Bxt[s]])
                if pre_tt is not None:
                    pre_tt(tt)
                for job in jobs:
                    job(xts[s], Bxt[s], tt)

        class WStream:
            def __init__(self, st, n=3):
                self.t = [st.enter_context(sbt("ws%d" % i, [128, 32, 128], BF16)) for i in range(n)]
                self.B = [Buf() for _ in range(n)]
                self.i = 0

            def load(self, W, r0, nk, c0, ncol):
                s = self.i % len(self.t)
                self.i += 1
                src = W[r0:r0 + nk * 128, c0:c0 + ncol].rearrange("(k p) e -> p k e", p=128)
                P.dma("pool", self.t[s][:, 0:nk, 0:ncol], src, writes=[self.B[s]])
                return self.t[s], self.B[s]

        gb = [0]

        def gbank():
            gb[0] += 1
            return gb[0] % 2

        def fm_job(ws, W, KC, c0, ncol, epi):
            def job(xt, Bxt, tt):
                b = gbank()
                for k0 in range(0, KC, 32):
                    nk = min(32, KC - k0)
                    wt, Bw = ws.load(W, k0 * 128, nk, c0, ncol)
                    for k in range(nk):
                        P.pe(lambda e, wt=wt, k=k, k0=k0, b=b: e.matmul(
                            banks[b][0:ncol, :], lhsT=wt[:, k, 0:ncol], rhs=xt[:, k0 + k, :],
                            start=(k0 + k == 0), stop=(k0 + k == KC - 1)), reads=[Bw, Bxt], writes=[BK[b]])
                epi(tt, b)
            return job

        with ExitStack() as st:
            def sb(name, shape, dt):
                return st.enter_context(sbt(name, list(shape), dt))

            ws = WStream(st)
            ost = [sb("ost%d" % i, [128, 512], BF16) for i in range(3)]
            Bo = [Buf() for _ in range(3)]
            oi = [0]

            def copy_out(dst_fn, scale=1.0):
                def epi(tt, b):
                    s = oi[0] % 3
                    oi[0] += 1
                    P.act(lambda e, s=s, b=b: e.activation(out=ost[s][:], in_=banks[b][:], func=AF.Copy, scale=scale),
                          reads=[BK[b]], writes=[Bo[s]])
                    P.dma("sp", dst_fn(tt), ost[s][:], reads=[Bo[s]])
                return epi

            ckv = sb("ckv", [128, 4, 512], F32)
            sqb = sb("sqb", [128, 4, 512], BF16)
            rb = sb("rb", [128, 512], F32)
            kvn = sb("kvn", [128, 4, 512], BF16)
            Bc = Buf()
            Bsq = Buf()
            Brb = Buf()
            Bkvn = Buf()
            kvnTr = kvnT.rearrange("(k p) t -> p k t", p=128)

            def ckv_epi(j):
                def epi(tt, b):
                    P.dve(lambda e, b=b: e.tensor_copy(out=ckv[:, j, :], in_=banks[b][:]), reads=[BK[b]], writes=[Bc])
                    P.act(lambda e, b=b: e.activation(out=sqb[:, j, :], in_=ckv[:, j, :], func=AF.Square),
                          reads=[Bc], writes=[Bsq])
                    if j == 3:
                        for jj in range(4):
                            P.pe(lambda e, jj=jj: e.matmul(banks[4][:], lhsT=ones_bf[:], rhs=sqb[:, jj, :],
                                                           start=(jj == 0), stop=(jj == 3)),
                                 reads=[Bsq, B_const], writes=[BK[4]])
                        rsqrt_ops(rb[:], banks[4][:], 1.0 / 512, [BK[4]], [Brb])
                        for jj in range(4):
                            P.dve(lambda e, jj=jj: e.scalar_tensor_tensor(
                                out=kvn[:, jj, :], in0=ckv[:, jj, :], scalar=kvg[:, jj:jj + 1], in1=rb[:],
                                op0=ALU.mult, op1=ALU.mult), reads=[Bc, Brb, Bp0], writes=[Bkvn])
                        P.dma("sp", kvnTr[:, :, tt * 512:(tt + 1) * 512], kvn[:], reads=[Bkvn])
                return epi

            Bp0 = B_const
            wtm = sb("wtm", [128, 32, 192], BF16)
            Bwtm = Buf()
            w_in_r = w_in.rearrange("(k p) e -> p k e", p=128)
            P.dma("pool", wtm[:, :, 0:128], w_in_r[:, :, 2176:2304], writes=[Bwtm])
            P.dma("pool", wtm[:, :, 128:192], w_in_r[:, :, 8096:8160], writes=[Bwtm])
            vst = [sb("vst%d" % i, [128, 128], BF16) for i in range(2)]
            Bv = [Buf() for _ in range(2)]
            krf = sb("krf", [128, 64], F32)
            kro = sb("kro", [128, 64], F32)
            tmp1 = sb("tmp1", [128, 32], F32)
            Bkr = Buf()
            krst = sb("krst", [64, 512], BF16)
            Bkrst = Buf()

            def rope_tok(e_list, src, dst, blk, nh, tmp):
                c = cosT[:, blk, :].unsqueeze(1).to_broadcast([128, nh, 32])
                s_ = sinT[:, blk, :].unsqueeze(1).to_broadcast([128, nh, 32])
                x1 = src[:, :, 0:32]
                x2 = src[:, :, 32:64]
                ops = [
                    (dst[:, :, 0:32], x1, c, ALU.mult), (tmp, x2, s_, ALU.mult),
                    (dst[:, :, 0:32], dst[:, :, 0:32], tmp, ALU.subtract),
                    (dst[:, :, 32:64], x1, s_, ALU.mult), (tmp, x2, c, ALU.mult),
                    (dst[:, :, 32:64], dst[:, :, 32:64], tmp, ALU.add)]
                for (o, a, b_, op) in ops:
                    P.dve(lambda e, o=o, a=a, b_=b_, op=op: e.tensor_tensor(out=o, in0=a, in1=b_, op=op),
                          reads=e_list[0], writes=e_list[1])

            def tm_job_k(xt, Bxt, tt):
                for tb in range(4):
                    blk = tt * 4 + tb
                    b = 2 + (tb % 2)
                    for k in range(32):
                        P.pe(lambda e, k=k, b=b, tb=tb: e.matmul(banks[b][:, 0:192], lhsT=xt[:, k, tb * 128:(tb + 1) * 128],
                                                                 rhs=wtm[:, k, :], start=(k == 0), stop=(k == 31)),
                             reads=[Bxt, Bwtm], writes=[BK[b]])
                    s = blk % 2
                    P.act(lambda e, s=s, b=b: e.activation(out=vst[s][:], in_=banks[b][:, 0:128], func=AF.Copy),
                          reads=[BK[b]], writes=[Bv[s]])
                    P.dma("sp", va[blk * 128:(blk + 1) * 128, :], vst[s][:], reads=[Bv[s]])
                    P.act(lambda e, b=b: e.activation(out=krf[:], in_=banks[b][:, 128:192], func=AF.Copy), reads=[BK[b]],
                          writes=[Bkr])
                    rope_tok(([Bkr, B_const], [Bkr]), krf[:].unsqueeze(1), kro[:].unsqueeze(1), blk, 1,
                             tmp1[:].unsqueeze(1))
                    P.pe(lambda e: e.transpose(banks[5][0:64, 0:128], kro[:], ident[:]), reads=[Bkr, B_const],
                         writes=[BK[5]])
                    P.act(lambda e, tb=tb: e.activation(out=krst[:, tb * 128:(tb + 1) * 128], in_=banks[5][0:64, 0:128],
                                                        func=AF.Copy), reads=[BK[5]], writes=[Bkrst])
                P.dma("sp", krT[:, tt * 512:(tt + 1) * 512], krst[:], reads=[Bkrst])

            jobs = [fm_job(ws, w_in, 32, 2048, 128, copy_out(lambda tt: kaT[:, tt * 512:(tt + 1) * 512])),
                    fm_job(ws, w_in, 32, 6400, 128, copy_out(lambda tt: kiT[:, tt * 512:(tt + 1) * 512]))]
            for j in range(4):
                jobs.append(fm_job(ws, w_in, 32, 7584 + 128 * j, 128, ckv_epi(j)))
            jobs.append(tm_job_k)
            _sub = int(os.environ.get("KSUB", "255"))
            jobs = [jb for n_, jb in enumerate(jobs) if (_sub >> n_) & 1]
            run_gemm(st, h1T, 32, 8, jobs, xbufs=2)
            P.end_stage(touch)

        with ExitStack() as st:
            def sb(name, shape, dt):
                return st.enter_context(sbt(name, list(shape), dt))

            ws = WStream(st)
            ost = [sb("ost%d" % i, [128, 512], BF16) for i in range(3)]
            Bo = [Buf() for _ in range(3)]
            oi = [0]

            def kn_epi(h):
                def epi(tt, b):
                    s = oi[0] % 3
                    oi[0] += 1
                    P.act(lambda e, s=s, b=b: e.activation(out=ost[s][:], in_=banks[b][:], func=AF.Copy),
                          reads=[BK[b]], writes=[Bo[s]])
                    P.dma("sp", knT[h * 128:(h + 1) * 128, tt * 512:(tt + 1) * 512], ost[s][:], reads=[Bo[s]])
                return epi

            wuv = sb("wuv", [128, 4, 2048], BF16)
            Bwuv = Buf()
            P.dma("pool", wuv[:], w_uv.rearrange("(k p) e -> p k e", p=128), writes=[Bwuv])
            vbst = [sb("vbst%d" % i, [128, 2048], BF16) for i in range(2)]
            Bvb = [Buf() for _ in range(2)]

            def tm_job_vb(xt, Bxt, tt):
                for tb in range(4):
                    blk = tt * 4 + tb
                    s = blk % 2
                    for eg in range(4):
                        b = 2 + (eg % 2)
                        for k in range(4):
                            P.pe(lambda e, k=k, b=b, tb=tb, eg=eg: e.matmul(
                                banks[b][:], lhsT=xt[:, k, tb * 128:(tb + 1) * 128],
                                rhs=wuv[:, k, eg * 512:(eg + 1) * 512], start=(k == 0), stop=(k == 3)),
                                reads=[Bxt, Bwuv], writes=[BK[b]])
                        P.act(lambda e, s=s, b=b, eg=eg: e.activation(out=vbst[s][:, eg * 512:(eg + 1) * 512],
                                                                      in_=banks[b][:], func=AF.Copy),
                              reads=[BK[b]], writes=[Bvb[s]])
                    P.dma("sp", vb[blk * 128:(blk + 1) * 128, :], vbst[s][:], reads=[Bvb[s]])

            jobs = [fm_job(ws, w_uk, 4, h * 128, 128, kn_epi(h)) for h in range(16)]
            jobs.append(tm_job_vb)
            run_gemm(st, kvnT, 4, 8, jobs, xbufs=2)
            P.end_stage(touch)

        with ExitStack() as st:
            def sb(name, shape, dt):
                return st.enter_context(sbt(name, list(shape), dt))

            ws = WStream(st)
            ost = [sb("ost%d" % i, [128, 512], BF16) for i in range(3)]
            Bo = [Buf() for _ in range(3)]
            oi = [0]

            def copy_out(dst_fn, scale=1.0):
                def epi(tt, b):
                    s = oi[0] % 3
                    oi[0] += 1
                    P.act(lambda e, s=s, b=b: e.activation(out=ost[s][:], in_=banks[b][:], func=AF.Copy, scale=scale),
                          reads=[BK[b]], writes=[Bo[s]])
                    P.dma("sp", dst_fn(tt), ost[s][:], reads=[Bo[s]])
                return epi

            cq = sb("cq", [128, 8, 512], F32)
            sqb = sb("sqb", [128, 8, 512], BF16)
            rb = sb("rb", [128, 512], F32)
            cqn = sb("cqn", [128, 8, 512], BF16)
            Bc, Bsq, Brb, Bcqn = Buf(), Buf(), Buf(), Buf()
            cqnTr = cqnT.rearrange("(k p) t -> p k t", p=128)

            def cq_epi(j):
                def epi(tt, b):
                    P.dve(lambda e, b=b: e.tensor_copy(out=cq[:, j, :], in_=banks[b][:]), reads=[BK[b]], writes=[Bc])
                    P.act(lambda e, b=b: e.activation(out=sqb[:, j, :], in_=cq[:, j, :], func=AF.Square),
                          reads=[Bc], writes=[Bsq])
                    if j == 7:
                        for jj in range(8):
                            P.pe(lambda e, jj=jj: e.matmul(banks[4][:], lhsT=ones_bf[:], rhs=sqb[:, jj, :],
                                                           start=(jj == 0), stop=(jj == 7)),
                                 reads=[Bsq, B_const], writes=[BK[4]])
                        rsqrt_ops(rb[:], banks[4][:], 1.0 / 1024, [BK[4]], [Brb])
                        for jj in range(8):
                            P.dve(lambda e, jj=jj: e.scalar_tensor_tensor(
                                out=cqn[:, jj, :], in0=cq[:, jj, :], scalar=qg[:, jj:jj + 1], in1=rb[:],
                                op0=ALU.mult, op1=ALU.mult), reads=[Bc, Brb, B_const], writes=[Bcqn])
                        P.dma("sp", cqnTr[:, :, tt * 512:(tt + 1) * 512], cqn[:], reads=[Bcqn])
                return epi

            wist = sb("wist", [32, 512], F32)
            Bwi = Buf()
            IDX_SCALE = (128 ** -0.5) * (32 ** -0.5)

            def wi_epi(tt, b):
                P.act(lambda e, b=b: e.activation(out=wist[:], in_=banks[b][0:32, :], func=AF.Copy, scale=IDX_SCALE),
                      reads=[BK[b]], writes=[Bwi])
                dst = wi3[tt * 4 * 4096:(tt + 1) * 4 * 4096].rearrange("(bg f h) -> h bg f", h=32, f=4)
                P.dma("sp", dst, wist[:].rearrange("h (bg f) -> h bg f", f=4), reads=[Bwi], slow=True)

            jobs = []
            for h in range(16):
                jobs.append(fm_job(ws, w_in, 32, h * 128, 128,
                                   copy_out(lambda tt, h=h: qaT[h * 128:(h + 1) * 128, tt * 512:(tt + 1) * 512],
                                            scale=128 ** -0.5)))
            for h in range(32):
                jobs.append(fm_job(ws, w_in, 32, 2304 + h * 128, 128,
                                   copy_out(lambda tt, h=h: qiT[h * 128:(h + 1) * 128, tt * 512:(tt + 1) * 512])))
            jobs.append(fm_job(ws, w_in, 32, 6528, 32, wi_epi))
            for j in range(8):
                jobs.append(fm_job(ws, w_in, 32, 6560 + 128 * j, 128, cq_epi(j)))
            run_gemm(st, h1T, 32, 4, jobs, xbufs=2)
            P.end_stage(touch)

        with ExitStack() as st:
            def sb(name, shape, dt):
                return st.enter_context(sbt(name, list(shape), dt))

            QS = 192 ** -0.5
            wq = sb("wq", [128, 8, 3072], BF16)
            Bwq = Buf()
            wqr = w_uq.rearrange("(k p) e -> p k e", p=128)
            for k in range(8):
                P.dma("pool", wq[:, k, :], wqr[:, k, :], writes=[Bwq])
            ost = [sb("ost%d" % i, [128, 512], BF16) for i in range(3)]
            Bo = [Buf() for _ in range(3)]
            oi = [0]
            qrf = sb("qrf", [128, 8, 64], F32)
            qro = sb("qro", [128, 8, 64], F32)
            tmp8 = sb("tmp8", [128, 8, 32], F32)
            Bqr = Buf()
            qrst = sb("qrst", [64, 16, 512], BF16)
            Bqrst = Buf()
            wqrp = sb("wqrp", [128, 8, 16, 64], BF16)
            for k in range(8):
                P.dma("pool", wqrp[:, k, :, :], wqr[:, k, :].rearrange("p (h c) -> p h c", c=192)[:, :, 128:192],
                      writes=[Bwq])

            def qup_job(xt, Bxt, tt):
                for h in range(16):
                    b = gbank()
                    for k in range(8):
                        P.pe(lambda e, k=k, b=b, h=h: e.matmul(banks[b][:], lhsT=wq[:, k, h * 192:h * 192 + 128],
                                                               rhs=xt[:, k, :], start=(k == 0), stop=(k == 7)),
                             reads=[Bxt, Bwq], writes=[BK[b]])
                    s = oi[0] % 3
                    oi[0] += 1
                    P.act(lambda e, s=s, b=b: e.activation(out=ost[s][:], in_=banks[b][:], func=AF.Copy, scale=QS),
                          reads=[BK[b]], writes=[Bo[s]])
                    P.dma("sp", qnT[h * 128:(h + 1) * 128, tt * 512:(tt + 1) * 512], ost[s][:], reads=[Bo[s]])
                for tb in range(4):
                    blk = tt * 4 + tb
                    for hg in range(2):
                        b = 2 + hg
                        for k in range(8):
                            P.pe(lambda e, k=k, b=b, tb=tb, hg=hg: e.matmul(
                                banks[b][:],
                                lhsT=xt[:, k, tb * 128:(tb + 1) * 128], rhs=wqrp[:, k, hg * 8:(hg + 1) * 8, :],
                                start=(k == 0), stop=(k == 7)), reads=[Bxt, Bwq], writes=[BK[b]])
                        P.act(lambda e, b=b: e.activation(out=qrf[:].rearrange("p h c -> p (h c)"), in_=banks[b][:],
                                                          func=AF.Copy, scale=QS), reads=[BK[b]], writes=[Bqr])
                        rope_tok(([Bqr, B_const], [Bqr]), qrf[:], qro[:], blk, 8, tmp8[:])
                        for hh in range(8):
                            h = hg * 8 + hh
                            pb = 4 + (hh % 2)
                            P.pe(lambda e, hh=hh, pb=pb: e.transpose(banks[pb][0:64, 0:128], qro[:, hh, :], ident[:]),
                                 reads=[Bqr, B_const], writes=[BK[pb]])
                            P.act(lambda e, h=h, pb=pb, tb=tb: e.activation(
                                out=qrst[:, h, tb * 128:(tb + 1) * 128], in_=banks[pb][0:64, 0:128], func=AF.Copy),
                                reads=[BK[pb]], writes=[Bqrst])
                P.dma("sp", qrT.rearrange("(h c) t -> c h t", c=64)[:, :, tt * 512:(tt + 1) * 512], qrst[:],
                      reads=[Bqrst])

            run_gemm(st, cqnT, 8, 4, [qup_job], xbufs=2)
            P.end_stage(touch)

        slopes = [2.0 ** (-8.0 * (h + 1) / 16.0) for h in range(16)]

        with ExitStack() as st:
            def sb(name, shape, dt):
                return st.enter_context(sbt(name, list(shape), dt))

            kis = sb("kis", [128, 4096], BF16)
            kas = sb("kas", [128, 4096], BF16)
            vas = sb("vas", [128, 32, 128], BF16)
            Bk = Buf()
            P.dma("sp", kis[:], kiT, writes=[Bk])
            P.dma("sp", kas[:], kaT, writes=[Bk])
            P.dma("sp", vas[:], va.rearrange("(b p) d -> p b d", p=128), writes=[Bk])
            chkb = sb("chkb", [128, 4096], F32)
            posb = sb("posb", [128, 2048], F32)
            pb_i = sb("pb_i", [128, 4096], I32)
            Bpb = Buf()
            P.dma("sp", pb_i[:], posr.partition_broadcast(128)[:, 0, :], writes=[Bpb])
            P.dve(lambda e: e.tensor_copy(out=posb[:], in_=pb_i[:, 0:2048]), reads=[Bpb], writes=[Bk])
            P.dve(lambda e: e.tensor_scalar(out=pb_i[:], in0=pb_i[:], scalar1=6, scalar2=None,
                                            op0=ALU.arith_shift_right), reads=[Bpb, Bk], writes=[Bpb])
            P.dve(lambda e: e.tensor_copy(out=chkb[:], in_=pb_i[:]), reads=[Bpb], writes=[Bk])
            d4i = sb("d4i", [128, 4], I32)
            d4 = sb("d4", [128, 4], F32)
            d4b = sb("d4b", [128, 4], F32)
            P.pool(lambda e: e.iota(d4i[:], pattern=[[-32, 4]], base=0, channel_multiplier=1), writes=[Bk])
            P.dve(lambda e: e.tensor_copy(out=d4[:], in_=d4i[:]), reads=[Bk], writes=[Bk])
            P.dve(lambda e: e.tensor_scalar(out=d4b[:], in0=d4[:], scalar1=31.0, scalar2=None, op0=ALU.is_le),
                  reads=[Bk], writes=[Bk])
            P.dve(lambda e: e.tensor_scalar(out=d4[:], in0=d4[:], scalar1=0.0, scalar2=None, op0=ALU.is_ge),
                  reads=[Bk], writes=[Bk])
            P.dve(lambda e: e.tensor_tensor(out=d4[:], in0=d4[:], in1=d4b[:], op=ALU.mult), reads=[Bk], writes=[Bk])
            wbd = sb("wbd", [128, 32, 128], BF16)
            Bwbd = Buf()
            P.pool(lambda e: e.memset(wbd[:], 0.0), writes=[Bwbd])
            WT = sb("WT", [128, 32], F32)
            BWT = Buf()
            qib = [sb("qib%d" % i, [128, 128, 32], BF16) for i in range(2)]
            Bqi = [Buf() for _ in range(2)]
            qil = sb("qil", [128, 32, 128], BF16)
            Bqil = Buf()
            qab = [sb("qab%d" % i, [128, 16, 128], BF16) for i in range(2)]
            Bqa = [Buf() for _ in range(2)]
            Rt = [sb("Rt%d" % i, [128, 512], BF16) for i in range(4)]
            BR = [Buf() for _ in range(4)]
            Isc = sb("Isc", [128, 4096], F32)
            BI = Buf()
            pen = sb("pen", [128, 512], F32)
            Bpen = Buf()
            jk = sb("jk", [128, 4096], BF16)
            Bjk = Buf()
            sm = sb("sm", [128, 16], F32)
            Bsm = Buf()
            DmT = sb("DmT", [128, 32, 128], F32)
            BDm = Buf()
            mtmp = sb("mtmp", [128, 128], F32)
            Bmt = Buf()
            Zt = [sb("Zt%d" % i, [128, 512], F32) for i in range(2)]
            BZ = [[Buf() for _ in range(4)] for _ in range(2)]
            Pt = [sb("Pt%d" % i, [128, 512], BF16) for i in range(2)]
            BP = [Buf() for _ in range(2)]
            rden = sb("rden", [128, 512], F32)
            Brd = Buf()
            ostA = [sb("ostA%d" % i, [128, 4, 128], BF16) for i in range(2)]
            BoA = [Buf() for _ in range(2)]
            qiTr = qiT.rearrange("(h d) t -> d h t", d=128)
            qaTr = qaT.rearrange("(h d) t -> d h t", d=128)
            mixTr = mixT.rearrange("(h d) t -> d h t", d=128)
            ri_ = [0]
            for j in range(16):
                i = j // 4
                nkb = 4 * (i + 1)
                kbl = list(range(0, nkb)) + list(range(16, 16 + nkb))
                kgl = [kb for kb in kbl if kb % 4 == 0]
                nk = len(kbl) * 128
                qi_ = qib[j % 2]
                qa_ = qab[j % 2]
                P.dma("sp", qil[:], qiTr[:, :, j * 128:(j + 1) * 128], writes=[Bqil])
                P.pool(lambda e, qi_=qi_: e.tensor_copy(out=qi_[:], in_=qil[:].rearrange("p h t -> p t h")),
                       reads=[Bqil], writes=[Bqi[j % 2]])
                P.dma("sp", qa_[:], qaTr[:, :, j * 128:(j + 1) * 128], writes=[Bqa[j % 2]])
                P.dma("sp", WT[:], wi3[j * 4096:(j + 1) * 4096].rearrange("(g p) -> p g", p=128), writes=[BWT],
                      slow=True)
                for c4 in range(4):
                    dst = wbd[:].rearrange("p g c -> p (g c)")[:, c4:4096:132]
                    P.dve(lambda e, dst=dst, c4=c4: e.tensor_scalar(out=dst, in0=WT[:], scalar1=d4[:, c4:c4 + 1],
                                                                    scalar2=None, op0=ALU.mult),
                          reads=[BWT, Bk], writes=[Bwbd])
                for gi, kb0 in enumerate(kgl):
                    def emit_L(g, kb0=kb0, qi_=qi_):
                        lb = 2 + (g % 2)
                        P.pe(lambda e, g=g, lb=lb, kb0=kb0, qi_=qi_: e.matmul(
                            banks[lb][:], lhsT=qi_[:, 4 * g:4 * g + 4, :], rhs=kis[:, kb0 * 128:kb0 * 128 + 512],
                            start=True, stop=True), reads=[Bqi[j % 2], Bk], writes=[BK[lb]])
                    emit_L(0)
                    for g in range(32):
                        lb = 2 + (g % 2)
                        if g + 1 < 32:
                            emit_L(g + 1)
                        r = ri_[0] % 4
                        ri_[0] += 1
                        if r % 2 == 0:
                            P.act(lambda e, r=r, lb=lb: e.activation(out=Rt[r][:], in_=banks[lb][:], func=AF.Relu),
                                  reads=[BK[lb]], writes=[BR[r]])
                        else:
                            P.dve(lambda e, r=r, lb=lb: e.tensor_scalar(out=Rt[r][:], in0=banks[lb][:], scalar1=0.0,
                                                                        scalar2=None, op0=ALU.max),
                                  reads=[BK[lb]], writes=[BR[r]])
                        P.pe(lambda e, g=g, r=r: e.matmul(banks[4][:], lhsT=wbd[:, g, :], rhs=Rt[r][:],
                                                          start=(g == 0), stop=(g == 31)),
                             reads=[Bwbd, BR[r]], writes=[BK[4]])
                    P.dve(lambda e, kb0=kb0, j=j: e.tensor_scalar(
                        out=pen[:], in0=chkb[:, kb0 * 128:kb0 * 128 + 512], scalar1=chkf[:, j:j + 1], scalar2=-1e30,
                        op0=ALU.is_gt, op1=ALU.mult), reads=[Bk, B_const], writes=[Bpen])
                    P.dve(lambda e, gi=gi: e.tensor_tensor(out=Isc[:, gi * 512:(gi + 1) * 512], in0=banks[4][:],
                                                           in1=pen[:], op=ALU.add),
                          reads=[BK[4], Bpen], writes=[BI])
                Iv = Isc[:, 0:nk]
                P.dve(lambda e, Iv=Iv: e.reduce_max(out=sm[:, 0:1], in_=Iv, axis=AX.X), reads=[BI], writes=[Bsm])
                P.dve(lambda e: e.tensor_scalar(out=sm[:, 0:1], in0=sm[:, 0:1], scalar1=-7.5, scalar2=None,
                                                op0=ALU.add), reads=[Bsm], writes=[Bsm])
                wdt = 8.5
                for it in range(NBIS):
                    P.dve(lambda e, Iv=Iv, nk=nk: e.tensor_scalar(out=jk[:, 0:nk], in0=Iv, scalar1=sm[:, 0:1],
                                                                  scalar2=None, op0=ALU.is_ge, op1=ALU.add,
                                                                  accum_out=sm[:, 2:3]),
                          reads=[BI, Bsm], writes=[Bjk, Bsm])
                    nw = wdt * 0.5 if it < NBIS - 1 else wdt
                    mul = 2.0 * nw if it < NBIS - 1 else wdt
                    P.dve(lambda e, mul=mul: e.tensor_scalar(out=sm[:, 3:4], in0=sm[:, 2:3], scalar1=255.5, scalar2=mul,
                                                             op0=ALU.is_ge, op1=ALU.mult), reads=[Bsm], writes=[Bsm])
                    P.dve(lambda e, nw=nw: e.scalar_tensor_tensor(out=sm[:, 0:1], in0=sm[:, 0:1], scalar=-nw,
                                                                  in1=sm[:, 3:4], op0=ALU.add, op1=ALU.add),
                          reads=[Bsm], writes=[Bsm])
                    wdt = nw
                P.dve(lambda e, Iv=Iv, nk=nk: e.tensor_scalar(out=jk[:, 0:nk], in0=Iv, scalar1=-1e29, scalar2=None,
                                                              op0=ALU.is_ge, op1=ALU.add, accum_out=sm[:, 4:5]),
                      reads=[BI, Bsm], writes=[Bjk, Bsm])
                P.dve(lambda e: e.tensor_scalar(out=sm[:, 5:6], in0=sm[:, 4:5], scalar1=256.5, scalar2=None,
                                                op0=ALU.is_gt), reads=[Bsm], writes=[Bsm])
                P.dve(lambda e: e.tensor_tensor(out=sm[:, 6:7], in0=sm[:, 0:1], in1=sm[:, 5:6], op=ALU.mult),
                      reads=[Bsm], writes=[Bsm])
                P.dve(lambda e: e.tensor_scalar(out=sm[:, 7:8], in0=sm[:, 5:6], scalar1=-1.0, scalar2=1e29,
                                                op0=ALU.add, op1=ALU.mult), reads=[Bsm], writes=[Bsm])
                P.dve(lambda e: e.tensor_tensor(out=sm[:, 0:1], in0=sm[:, 6:7], in1=sm[:, 7:8], op=ALU.add),
                      reads=[Bsm], writes=[Bsm])
                P.dve(lambda e, Iv=Iv: e.tensor_scalar(out=Iv, in0=Iv, scalar1=sm[:, 0:1], scalar2=None, op0=ALU.is_ge),
                      reads=[BI, Bsm], writes=[BI])
                for kbi, kb in enumerate(kbl):
                    tbk = 5
                    P.pe(lambda e, kbi=kbi: e.transpose(banks[5][:, 0:128], Isc[:, kbi * 128:(kbi + 1) * 128], ident[:]),
                         reads=[BI, B_const], writes=[BK[5]])
                    P.dve(lambda e: e.tensor_scalar(out=mtmp[:], in0=banks[5][:, 0:128], scalar1=-1e6, scalar2=1e6,
                                                    op0=ALU.mult, op1=ALU.add), reads=[BK[5]], writes=[Bmt])
                    P.dve(lambda e, kbi=kbi, kb=kb, j=j: e.tensor_scalar(
                        out=DmT[:, kbi, :], in0=posb[:, j * 128:(j + 1) * 128], scalar1=posf[:, kb:kb + 1], scalar2=None,
                        op0=ALU.subtract), reads=[Bk, B_const], writes=[BDm])
                    P.dve(lambda e, kbi=kbi: e.scalar_tensor_tensor(
                        out=DmT[:, kbi, :], in0=DmT[:, kbi, :], scalar=-1.0, in1=DmT[:, kbi, :], op0=ALU.mult,
                        op1=ALU.max), reads=[BDm], writes=[BDm])
                    P.dve(lambda e, kbi=kbi: e.tensor_tensor(out=DmT[:, kbi, :], in0=DmT[:, kbi, :], in1=mtmp[:],
                                                             op=ALU.add), reads=[BDm, Bmt], writes=[BDm])
                for hg in range(4):
                    ob, db = 6, 7
                    def emit_S(kbi, hg=hg, qa_=qa_, kbl=kbl):
                        sbk = kbi % 2
                        kb = kbl[kbi]
                        P.pe(lambda e, sbk=sbk, kb=kb, hg=hg, qa_=qa_: e.matmul(
                            banks[sbk][:], lhsT=kas[:, kb * 128:(kb + 1) * 128],
                            rhs=qa_[:, 4 * hg:4 * hg + 4, :], start=True, stop=True),
                            reads=[Bk, Bqa[j % 2]], writes=[BK[sbk]])
                    emit_S(0)
                    for kbi, kb in enumerate(kbl):
                        sbk = kbi % 2
                        if kbi + 1 < len(kbl):
                            emit_S(kbi + 1)
                        z = Zt[kbi % 2]
                        for hh in range(4):
                            h = 4 * hg + hh
                            P.dve(lambda e, z=z, hh=hh, h=h, kbi=kbi, sbk=sbk: e.scalar_tensor_tensor(
                                out=z[:, hh * 128:(hh + 1) * 128], in0=DmT[:, kbi, :], scalar=-slopes[h],
                                in1=banks[sbk][:, hh * 128:(hh + 1) * 128], op0=ALU.mult, op1=ALU.add),
                                reads=[BDm, BK[sbk]], writes=[BZ[kbi % 2][hh]])
                        p_ = Pt[kbi % 2]
                        P.act(lambda e, z=z, p_=p_: e.activation(out=p_[:], in_=z[:], func=AF.Exp),
                              reads=BZ[kbi % 2], writes=[BP[kbi % 2]])
                        P.pe(lambda e, p_=p_, kb=kb, kbi=kbi, nl=len(kbl): e.matmul(banks[ob][:], lhsT=vas[:, kb, :], rhs=p_[:],
                                                                       start=(kbi == 0), stop=(kbi == nl - 1)),
                             reads=[Bk, BP[kbi % 2]], writes=[BK[ob]])
                        P.pe(lambda e, p_=p_, kbi=kbi, nl=len(kbl): e.matmul(banks[db][:], lhsT=ones_bf[:], rhs=p_[:],
                                                                start=(kbi == 0), stop=(kbi == nl - 1)),
                             reads=[B_const, BP[kbi % 2]], writes=[BK[db]])
                    P.dve(lambda e: e.reciprocal(out=rden[:], in_=banks[db][:]), reads=[BK[db]], writes=[Brd])
                    oa = ostA[hg % 2]
                    P.dve(lambda e, oa=oa: e.tensor_tensor(out=oa[:].rearrange("p h t -> p (h t)"), in0=banks[ob][:],
                                                           in1=rden[:], op=ALU.mult),
                          reads=[BK[ob], Brd], writes=[BoA[hg % 2]])
                    P.dma("sp", mixTr[:, 4 * hg:4 * hg + 4, j * 128:(j + 1) * 128], oa[:], reads=[BoA[hg % 2]])
            P.end_stage(touch)

        with ExitStack() as st:
            def sb(name, shape, dt):
                return st.enter_context(sbt(name, list(shape), dt))

            krs = sb("krs", [64, 4096], BF16)
            Bk = Buf()
            P.dma("sp", krs[:], krT, writes=[Bk])
            ctb = sb("ctb", [128, 2048], F32)
            pb_i = sb("pb_i", [128, 2048], I32)
            P.dma("sp", pb_i[:], posr[:, 0:2048].partition_broadcast(128)[:, 0, :], writes=[Bk])
            P.dve(lambda e: e.tensor_scalar(out=pb_i[:], in0=pb_i[:], scalar1=6, scalar2=None,
                                            op0=ALU.arith_shift_right), reads=[Bk], writes=[Bk])
            P.dve(lambda e: e.tensor_copy(out=ctb[:], in_=pb_i[:]), reads=[Bk], writes=[Bk])
            cm = sb("cm", [128, 8, 512], BF16)
            Bcm = Buf()
            kn = [sb("kn%d" % i, [128, 4096], BF16) for i in range(2)]
            vbh = [sb("vbh%d" % i, [128, 32, 128], BF16) for i in range(2)]
            Bkn = [Buf() for _ in range(2)]
            qn = [sb("qn%d" % i, [128, 512], BF16) for i in range(2)]
            qr = [sb("qr%d" % i, [64, 512], BF16) for i in range(2)]
            Bq = [Buf() for _ in range(2)]
            Pt = [sb("PtB%d" % i, [128, 512], BF16) for i in range(3)]
            BP = [Buf() for _ in range(3)]
            rden = sb("rdenB", [128, 512], F32)
            Brd = Buf()
            ostB = [sb("ostB%d" % i, [128, 512], BF16) for i in range(2)]
            BoB = [Buf() for _ in range(2)]
            vbr = vb.rearrange("(b p) e -> p b e", p=128)
            hi_ = 0
            pi_ = 0
            for i in range(4):
                nkb = 4 * (i + 1)
                kbl = list(range(0, nkb)) + list(range(16, 16 + nkb))
                band = list(range(4 * i, 4 * i + 4)) + list(range(16 + 4 * i, 16 + 4 * i + 4))
                for bi_, kb in enumerate(band):
                    P.dve(lambda e, bi_=bi_, kb=kb, i=i: e.tensor_scalar(
                        out=cm[:, bi_, :], in0=ctb[:, i * 512:(i + 1) * 512], scalar1=chkf[:, kb:kb + 1], scalar2=None,
                        op0=ALU.is_ge), reads=[Bk, B_const], writes=[Bcm])
                for h in range(16):
                    s = hi_ % 2
                    hi_ += 1
                    P.dma("sp", kn[s][:, 0:nkb * 128], knT[h * 128:(h + 1) * 128, 0:nkb * 128], writes=[Bkn[s]])
                    P.dma("sp", kn[s][:, 2048:2048 + nkb * 128], knT[h * 128:(h + 1) * 128, 2048:2048 + nkb * 128],
                          writes=[Bkn[s]])
                    P.dma("sp", vbh[s][:, 0:nkb, :], vbr[:, 0:nkb, h * 128:(h + 1) * 128], writes=[Bkn[s]])
                    P.dma("sp", vbh[s][:, 16:16 + nkb, :], vbr[:, 16:16 + nkb, h * 128:(h + 1) * 128], writes=[Bkn[s]])
                    P.dma("sp", qn[s][:], qnT[h * 128:(h + 1) * 128, i * 512:(i + 1) * 512], writes=[Bq[s]])
                    P.dma("sp", qr[s][:], qrT[h * 64:(h + 1) * 64, i * 512:(i + 1) * 512], writes=[Bq[s]])
                    ob = 4 + (h % 2)
                    db = 6 + (h % 2)
                    def emit_QK(kbi, s=s, kbl=kbl):
                        sbk = kbi % 2
                        kb = kbl[kbi]
                        P.pe(lambda e, sbk=sbk, kb=kb, s=s: e.matmul(banks[sbk][:], lhsT=kn[s][:, kb * 128:(kb + 1) * 128],
                                                                     rhs=qn[s][:], start=True, stop=False),
                             reads=[Bkn[s], Bq[s]], writes=[BK[sbk]])
                        P.pe(lambda e, sbk=sbk, kb=kb, s=s: e.matmul(banks[sbk][:], lhsT=krs[:, kb * 128:(kb + 1) * 128],
                                                                     rhs=qr[s][:], start=False, stop=True),
                             reads=[Bk, Bq[s]], writes=[BK[sbk]])
                    emit_QK(0)
                    for kbi, kb in enumerate(kbl):
                        sbk = kbi % 2
                        if kbi + 1 < len(kbl):
                            emit_QK(kbi + 1)
                        pp = pi_ % 3
                        pi_ += 1
                        p_ = Pt[pp]
                        P.act(lambda e, p_=p_, sbk=sbk: e.activation(out=p_[:], in_=banks[sbk][:], func=AF.Exp),
                              reads=[BK[sbk]], writes=[BP[pp]])
                        if kb in band:
                            bi_ = band.index(kb)
                            P.dve(lambda e, p_=p_, bi_=bi_: e.tensor_tensor(out=p_[:], in0=p_[:], in1=cm[:, bi_, :],
                                                                            op=ALU.mult),
                                  reads=[BP[pp], Bcm], writes=[BP[pp]])
                        P.pe(lambda e, p_=p_, kb=kb, kbi=kbi, s=s, ob=ob, nl=len(kbl): e.matmul(
                            banks[ob][:], lhsT=vbh[s][:, kb, :], rhs=p_[:], start=(kbi == 0),
                            stop=(kbi == nl - 1)), reads=[Bkn[s], BP[pp]], writes=[BK[ob]])
                        P.pe(lambda e, p_=p_, kbi=kbi, db=db, nl=len(kbl): e.matmul(
                            banks[db][:], lhsT=ones_bf[:], rhs=p_[:], start=(kbi == 0), stop=(kbi == nl - 1)),
                            reads=[B_const, BP[pp]], writes=[BK[db]])
                    P.dve(lambda e, db=db: e.reciprocal(out=rden[:], in_=banks[db][:]), reads=[BK[db]], writes=[Brd])
                    o_ = ostB[h % 2]
                    P.dve(lambda e, o_=o_, ob=ob: e.tensor_tensor(out=o_[:], in0=banks[ob][:], in1=rden[:], op=ALU.mult),
                          reads=[BK[ob], Brd], writes=[BoB[h % 2]])
                    P.dma("sp", mixT[(16 + h) * 128:(17 + h) * 128, i * 512:(i + 1) * 512], o_[:], reads=[BoB[h % 2]])
            P.end_stage(touch)

        with ExitStack() as st:
            def sb(name, shape, dt):
                return st.enter_context(sbt(name, list(shape), dt))

            ws = WStream(st)
            xch = [sb("xch%d" % i, [128, 512], F32) for i in range(2)]
            Bxc = [Buf() for _ in range(2)]
            sq = [sb("sq%d" % i, [128, 512], BF16) for i in range(2)]
            Bsq = [Buf() for _ in range(2)]
            ci = [0]

            def wo_epi(ec):
                def epi(tt, b):
                    s = ci[0] % 2
                    ci[0] += 1
                    P.dma("sp", xch[s][:], xT[ec * 128:(ec + 1) * 128, tt * 512:(tt + 1) * 512], writes=[Bxc[s]])
                    P.dve(lambda e, s=s, b=b: e.scalar_tensor_tensor(
                        out=xch[s][:], in0=banks[b][:], scalar=gate1[:, ec:ec + 1], in1=xch[s][:], op0=ALU.mult,
                        op1=ALU.add), reads=[BK[b], B_mod, Bxc[s]], writes=[Bxc[s]])
                    P.dma("sp", x2T[ec * 128:(ec + 1) * 128, tt * 512:(tt + 1) * 512], xch[s][:], reads=[Bxc[s]])
                    P.act(lambda e, s=s: e.activation(out=sq[s][:], in_=xch[s][:], func=AF.Square), reads=[Bxc[s]],
                          writes=[Bsq[s]])
                    P.pe(lambda e, s=s: e.matmul(banks[4][:], lhsT=ones_bf[:], rhs=sq[s][:], start=(ec == 0),
                                                 stop=(ec == 31)), reads=[B_const, Bsq[s]], writes=[BK[4]])
                    if ec == 31:
                        rsqrt_ops(r2b[:, tt * 512:(tt + 1) * 512], banks[4][:], 1.0 / D, [BK[4]], [B_r2b])
                return epi

            jobs = [fm_job(ws, w_o, 32, ec * 128, 128, wo_epi(ec)) for ec in range(32)]
            run_gemm(st, mixT, 32, 4, jobs, xbufs=2)
            P.end_stage(touch)

        with ExitStack() as st:
            def sb(name, shape, dt):
                return st.enter_context(sbt(name, list(shape), dt))

            xch = [sb("xch%d" % i, [128, 512], F32) for i in range(3)]
            Bxc = [Buf() for _ in range(3)]
            hch = [sb("hch%d" % i, [128, 512], BF16) for i in range(3)]
            Bhc = [Buf() for _ in range(3)]
            n = 0
            for tt in range(4):
                for k in range(32):
                    s = n % 3
                    n += 1
                    P.dma("sp", xch[s][:], x2T[k * 128:(k + 1) * 128, tt * 512:(tt + 1) * 512], writes=[Bxc[s]])
                    P.dve(lambda e, s=s, k=k, tt=tt: e.scalar_tensor_tensor(
                        out=xch[s][:], in0=xch[s][:], scalar=gs2[:, k:k + 1], in1=r2b[:, tt * 512:(tt + 1) * 512],
                        op0=ALU.mult, op1=ALU.mult), reads=[Bxc[s], B_mod, B_r2b], writes=[Bxc[s]])
                    P.act(lambda e, s=s, k=k: e.activation(out=hch[s][:], in_=xch[s][:], func=AF.Identity,
                                                           bias=sh2[:, k:k + 1], scale=1.0),
                          reads=[Bxc[s], B_mod], writes=[Bhc[s]])
                    P.dma("sp", h2T[k * 128:(k + 1) * 128, tt * 512:(tt + 1) * 512], hch[s][:], reads=[Bhc[s]])
            P.end_stage(touch)

        with ExitStack() as st:
            def sb(name, shape, dt):
                return st.enter_context(sbt(name, list(shape), dt))

            ws = WStream(st)
            u = [sb("u%d" % i, [128, 512], BF16) for i in range(3)]
            Bu = [Buf() for _ in range(3)]
            ci = [0]

            def m1_epi(fc):
                def epi(tt, b):
                    s = ci[0] % 3
                    ci[0] += 1
                    P.act(lambda e, s=s, b=b: e.activation(out=u[s][:], in_=banks[b][:], func=AF.Relu),
                          reads=[BK[b]], writes=[Bu[s]])
                    P.dve(lambda e, s=s: e.tensor_tensor(out=u[s][:], in0=u[s][:], in1=u[s][:], op=ALU.mult),
                          reads=[Bu[s]], writes=[Bu[s]])
                    P.dma("sp", hidT[fc * 128:(fc + 1) * 128, tt * 512:(tt + 1) * 512], u[s][:], reads=[Bu[s]])
                return epi

            jobs = [fm_job(ws, w1, 32, fc * 128, 128, m1_epi(fc)) for fc in range(128)]
            run_gemm(st, h2T, 32, 4, jobs, xbufs=2)
            P.end_stage(touch)

        with ExitStack() as st:
            def sb(name, shape, dt):
                return st.enter_context(sbt(name, list(shape), dt))

            ws = WStream(st)
            xch = [sb("xch%d" % i, [128, 512], F32) for i in range(2)]
            Bxc = [Buf() for _ in range(2)]
            sq = [sb("sq%d" % i, [128, 512], BF16) for i in range(2)]
            Bsq = [Buf() for _ in range(2)]
            sscol = sb("sscol", [128, 16], F32)
            Bss = Buf()
            P.pool(lambda e: e.memset(sscol[:], 0.0), writes=[Bss])
            ci = [0]

            def m2_epi(ec):
                def epi(tt, b):
                    s = ci[0] % 2
                    ci[0] += 1
                    P.dma("sp", xch[s][:], x2T[ec * 128:(ec + 1) * 128, tt * 512:(tt + 1) * 512], writes=[Bxc[s]])
                    P.dve(lambda e, s=s, b=b: e.scalar_tensor_tensor(
                        out=xch[s][:], in0=banks[b][:], scalar=gate2[:, ec:ec + 1], in1=xch[s][:], op0=ALU.mult,
                        op1=ALU.add), reads=[BK[b], B_mod, Bxc[s]], writes=[Bxc[s]])
                    P.act(lambda e, s=s: e.activation(out=sq[s][:], in_=xch[s][:], func=AF.Square), reads=[Bxc[s]],
                          writes=[Bsq[s]])
                    for tb in range(4):
                        P.pe(lambda e, s=s, tb=tb: e.matmul(banks[4][:, tb:tb + 1], lhsT=sq[s][:, tb * 128:(tb + 1) * 128],
                                                            rhs=ones_bf[:, 0:1], start=True, stop=True),
                             reads=[B_const, Bsq[s]], writes=[BK[4]])
                    P.dve(lambda e, tt=tt: e.tensor_tensor(out=sscol[:, tt * 4:tt * 4 + 4], in0=sscol[:, tt * 4:tt * 4 + 4],
                                                           in1=banks[4][:, 0:4], op=ALU.add),
                          reads=[BK[4], Bss], writes=[Bss])
                    P.dve(lambda e, s=s: e.tensor_scalar(out=xch[s][:], in0=xch[s][:], scalar1=fg[:, ec:ec + 1],
                                                         scalar2=None, op0=ALU.mult),
                          reads=[Bxc[s], Bsq[s], B_const], writes=[Bxc[s]])
                    P.dma("sp", x3T[ec * 128:(ec + 1) * 128, tt * 512:(tt + 1) * 512], xch[s][:], reads=[Bxc[s]])
                    if ec == 31 and tt == 3:
                        rsqrt_ops(rstd3[:], sscol[:], 1.0 / D, [Bss], [B_r3])
                return epi

            jobs = [fm_job(ws, w2, 128, ec * 128, 128, m2_epi(ec)) for ec in range(32)]
            run_gemm(st, hidT, 128, 4, jobs, xbufs=1)
            P.end_stage(touch)

        with ExitStack() as st:
            def sb(name, shape, dt):
                return st.enter_context(sbt(name, list(shape), dt))

            x3s = [sb("x3s%d" % i, [128, 32, 128], F32) for i in range(2)]
            Bx3 = [Buf() for _ in range(2)]
            yst = [sb("yst%d" % i, [128, 4096], F32) for i in range(2)]
            By = [Buf() for _ in range(2)]
            x3Tr = x3T.rearrange("(k p) t -> p k t", p=128)
            for blk in range(16):
                s = blk % 2
                P.dma("sp", x3s[s][:], x3Tr[:, :, blk * 128:(blk + 1) * 128], writes=[Bx3[s]])
                for k in range(32):
                    b = (k // 4) % 2
                    P.pe(lambda e, s=s, k=k, b=b: e.transpose(banks[b][:, (k % 4) * 128:(k % 4 + 1) * 128], x3s[s][:, k, :],
                                                              ident[:]), reads=[Bx3[s], B_const], writes=[BK[b]])
                    if k % 4 == 3:
                        P.act(lambda e, s=s, k=k, b=b, blk=blk: e.activation(
                            out=yst[s][:, (k - 3) * 128:(k + 1) * 128], in_=banks[b][:], func=AF.Identity,
                            scale=rstd3[:, blk:blk + 1]), reads=[BK[b], B_r3], writes=[By[s]])
                P.dma("sp", y[blk * 128:(blk + 1) * 128, :], yst[s][:], reads=[By[s]])
            P.end_stage(touch)
    return nc


_NC_CACHE = {}


def kernel(x, c, positions, w_ada, b_ada, ln1_g, w_in, q_norm_g, kv_norm_g, w_uq, w_uk, w_uv, w_o, ln2_g,
           w_mlp_in, w_mlp_out, final_g):
    x = np.asarray(x)
    positions = np.asarray(positions)
    B, S, _ = x.shape

    def colT(v, k):
        return np.ascontiguousarray(np.asarray(v, dtype=np.float32).reshape(k, 128).T)

    shared = {
        "w_ada": np.ascontiguousarray(np.asarray(w_ada)[0]), "b_adaT": colT(np.asarray(b_ada)[0], 192),
        "g1T": colT(np.asarray(ln1_g)[0], 32), "w_in": np.ascontiguousarray(np.asarray(w_in)[0]),
        "qgT": colT(np.asarray(q_norm_g)[0], 8), "kvgT": colT(np.asarray(kv_norm_g)[0], 4),
        "w_uq": np.ascontiguousarray(np.asarray(w_uq)[0]), "w_uk": np.ascontiguousarray(np.asarray(w_uk)[0]),
        "w_uv": np.ascontiguousarray(np.asarray(w_uv)[0]), "w_o": np.ascontiguousarray(np.asarray(w_o)[0]),
        "g2T": colT(np.asarray(ln2_g)[0], 32), "w1": np.ascontiguousarray(np.asarray(w_mlp_in)[0]),
        "w2": np.ascontiguousarray(np.asarray(w_mlp_out)[0]), "fgT": colT(np.asarray(final_g), 32),
    }
    in_maps = []
    perms = []
    for core in range(8):
        b, p = core // 2, core % 2
        own = [2 * j + p for j in range(16)]
        oth = [2 * j + 1 - p for j in range(16)]
        blocks = own + oth
        idx = np.concatenate([np.arange(g * 128, (g + 1) * 128) for g in blocks])
        perms.append((b, own))
        m = dict(shared)
        m["xc"] = np.ascontiguousarray(x[b][idx])
        pp = np.ascontiguousarray(positions[b][idx].astype(np.int32))
        m["posr"] = pp.reshape(1, 4096)
        m["posc"] = np.ascontiguousarray(pp.reshape(32, 128).T)
        m["cT"] = colT(np.asarray(c)[b], 32)
        in_maps.append(m)
    if "nc" not in _NC_CACHE:
        _NC_CACHE["nc"] = build_nc()
    res = run_bass_kernel_spmd(_NC_CACHE["nc"], in_maps, core_ids=list(range(8)))
    out = np.empty((B, S, D), dtype=np.float32)
    for core in range(8):
        b, own = perms[core]
        yv = np.asarray(res.results[core]["y"]).reshape(16, 128, D)
        for j, g in enumerate(own):
            out[b, g * 128:(g + 1) * 128, :] = yv[j]
    return out
```

```python
import math
import os
from contextlib import ExitStack
import numpy as np
import concourse.bass as bass
import concourse.mybir as mybir
from concourse.bass_utils import run_bass_kernel_spmd

F32 = mybir.dt.float32
BF16 = mybir.dt.bfloat16
I32 = mybir.dt.int32
AF = mybir.ActivationFunctionType
ALU = mybir.AluOpType
AX = mybir.AxisListType

ENGS = ("pe", "act", "dve", "pool", "sp")
SAME_ENGINE_SYNC = os.environ.get("KSES", "1") == "1"
D = 4096
EPS = 1e-6
NBIS = 20


import os
MAXSTAGE = int(os.environ.get("KSTAGE", "99"))
DBG_OUT = set(filter(None, os.environ.get("KDBG", "").split(",")))


class StopBuild(Exception):
    pass


class Buf:
    __slots__ = ("writers", "readers")

    def __init__(self):
        self.writers = []
        self.readers = []


class Op:
    __slots__ = ("eng", "fn", "deps", "is_dma", "seq", "signals", "dsem", "dtarget", "emitted", "touch")

    def __init__(self, eng, fn, is_dma):
        self.eng = eng
        self.fn = fn
        self.deps = []
        self.is_dma = is_dma
        self.seq = None
        self.signals = False
        self.dsem = None
        self.dtarget = None
        self.emitted = False
        self.touch = False


class Prog:
    def __init__(self, nc, stack, n_dma_sems=10):
        self.nc = nc
        self.pending = {e: [] for e in ENGS}
        self.stage_deps = {e: [] for e in ENGS}
        self.csem = {e: stack.enter_context(nc.semaphore("c_" + e)) for e in ENGS}
        self.dsems = {e: [stack.enter_context(nc.semaphore("d_%s_%d" % (e, i))) for i in range(n_dma_sems)]
                      for e in ("sp", "pool")}
        self.cnt = {e: 0 for e in ENGS}
        self.dma_k = {e: 0 for e in self.dsems}
        self.dma_uses = {e: [0] * n_dma_sems for e in self.dsems}
        self.waited = {e: {} for e in ENGS}
        self.stage_no = 0

    def add(self, eng, fn, reads=(), writes=(), dma=False, extra_deps=()):
        op = Op(eng, fn, dma)
        deps = []
        for b in reads:
            deps.extend(b.writers)
        for b in writes:
            deps.extend(b.writers)
            deps.extend(b.readers)
        deps.extend(extra_deps)
        if self.stage_deps[eng]:
            deps.extend(self.stage_deps[eng])
            self.stage_deps[eng] = []
        seen = set()
        for d in deps:
            if d is op or id(d) in seen or (d.emitted and not d.touch):
                continue
            seen.add(id(d))
            op.deps.append(d)
            if not d.is_dma and not (d.eng == eng and (eng == "pe" or not SAME_ENGINE_SYNC)):
                d.signals = True
        for b in reads:
            b.readers.append(op)
            if len(b.readers) > 48:
                b.readers = b.readers[-48:]
        for b in writes:
            if b.readers:
                b.readers = []
                b.writers = [op]
            else:
                b.writers.append(op)
                if len(b.writers) > 48:
                    b.writers = b.writers[-48:]
        self.pending[eng].append(op)
        return op

    def pe(self, fn, reads=(), writes=(), **kw):
        return self.add("pe", fn, reads, writes, **kw)

    def act(self, fn, reads=(), writes=(), **kw):
        return self.add("act", fn, reads, writes, **kw)

    def dve(self, fn, reads=(), writes=(), **kw):
        return self.add("dve", fn, reads, writes, **kw)

    def pool(self, fn, reads=(), writes=(), **kw):
        return self.add("pool", fn, reads, writes, **kw)

    def dma(self, q, out, in_, reads=(), writes=(), slow=False, **kw):
        if slow:
            fn = lambda e: e.dma_start(out=out, in_=in_, allow_slow_non_contiguous=True)
        else:
            fn = lambda e: e.dma_start(out=out, in_=in_)
        return self.add(q, fn, reads, writes, dma=True, **kw)

    def end_stage(self, touch):
        nc = self.nc
        tops = []
        for e in ("act", "dve", "pool", "sp"):
            t = Op(e, touch[e], e == "sp")
            t.signals = True
            t.touch = True
            self.pending[e].append(("T", t))
            tops.append(t)
        for e in ENGS:
            for item in self.pending[e]:
                op = item[1] if isinstance(item, tuple) else item
                if op.is_dma:
                    i = self.dma_k[e] % len(self.dsems[e])
                    self.dma_k[e] += 1
                    self.dma_uses[e][i] += 1
                    op.dsem = (e, i)
                    op.dtarget = 16 * self.dma_uses[e][i]
                elif op.signals:
                    self.cnt[e] += 1
                    op.seq = self.cnt[e]
        with nc.Block() as block:
            engmap = {"pe": block.tensor, "act": block.scalar, "dve": block.vector, "pool": block.gpsimd,
                      "sp": block.sync}

            def make(ename):
                items = self.pending[ename]
                waited = self.waited[ename]

                def body(eng):
                    def wait(key, sem, val):
                        if waited.get(key, 0) >= val:
                            return
                        waited[key] = val
                        eng.wait_ge(sem, val)

                    def wait_all_dma():
                        if ename in self.dsems:
                            for i, s in enumerate(self.dsems[ename]):
                                tot = 16 * self.dma_uses[ename][i]
                                if tot:
                                    wait(("d", ename, i), s, tot)

                    for item in items:
                        if isinstance(item, tuple):
                            op = item[1]
                            if ename in self.dsems:
                                for i, s in enumerate(self.dsems[ename]):
                                    tot = 16 * self.dma_uses[ename][i]
                                    if op.is_dma and op.dsem == (ename, i):
                                        tot -= 16
                                    if tot:
                                        wait(("d", ename, i), s, tot)
                        else:
                            op = item
                        if op.is_dma and op.dtarget > 16:
                            wait(("d",) + op.dsem, self.dsems[op.dsem[0]][op.dsem[1]], op.dtarget - 16)
                        for d in op.deps:
                            if d.is_dma:
                                wait(("d",) + d.dsem, self.dsems[d.dsem[0]][d.dsem[1]], d.dtarget)
                            else:
                                if d.eng == ename and (ename == "pe" or not SAME_ENGINE_SYNC):
                                    continue
                                wait(("c", d.eng), self.csem[d.eng], d.seq)
                        ins = op.fn(eng)
                        if op.is_dma:
                            ins.then_inc(self.dsems[op.dsem[0]][op.dsem[1]], 16)
                        elif op.signals:
                            ins.then_inc(self.csem[ename], 1)
                    wait_all_dma()

                return body

            for e in ENGS:
                if self.pending[e]:
                    engmap[e](make(e))
        for e in ENGS:
            for item in self.pending[e]:
                (item[1] if isinstance(item, tuple) else item).emitted = True
        self.pending = {e: [] for e in ENGS}
        for e in ENGS:
            self.stage_deps[e] = list(tops)
        self.stage_no += 1
        if self.stage_no > MAXSTAGE:
            raise StopBuild()


def build_nc():
    nc = bass.Bass("TRN2", target_bir_lowering=False)

    def inp(name, shape, dt=F32):
        return nc.dram_tensor(name, list(shape), dt, kind="ExternalInput").ap()

    _uc = [0]

    def sbt(name, shape, dt):
        _uc[0] += 1
        return nc.sbuf_tensor("%s_u%d" % (name, _uc[0]), shape, dt)

    def scr(name, shape, dt):
        return nc.dram_tensor(name, list(shape), dt, kind=("ExternalOutput" if name in DBG_OUT else "Internal")).ap()

    xc = inp("xc", [4096, 4096])
    posr = inp("posr", [1, 4096], I32)
    posc = inp("posc", [128, 32], I32)
    cT = inp("cT", [128, 32])
    w_ada = inp("w_ada", [4096, 24576])
    b_adaT = inp("b_adaT", [128, 192])
    g1T = inp("g1T", [128, 32])
    w_in = inp("w_in", [4096, 8160])
    qgT = inp("qgT", [128, 8])
    kvgT = inp("kvgT", [128, 4])
    w_uq = inp("w_uq", [1024, 3072])
    w_uk = inp("w_uk", [512, 2048])
    w_uv = inp("w_uv", [512, 2048])
    w_o = inp("w_o", [4096, 4096])
    g2T = inp("g2T", [128, 32])
    w1 = inp("w1", [4096, 16384])
    w2 = inp("w2", [16384, 4096])
    fgT = inp("fgT", [128, 32])
    y = nc.dram_tensor("y", [2048, 4096], F32, kind="ExternalOutput").ap()

    h1T = scr("h1T", [4096, 4096], BF16)
    xT = scr("xT", [4096, 2048], F32)
    kaT = scr("kaT", [128, 4096], BF16)
    va = scr("va", [4096, 128], BF16)
    kiT = scr("kiT", [128, 4096], BF16)
    krT = scr("krT", [64, 4096], BF16)
    kvnT = scr("kvnT", [512, 4096], BF16)
    knT = scr("knT", [2048, 4096], BF16)
    vb = scr("vb", [4096, 2048], BF16)
    qaT = scr("qaT", [2048, 2048], BF16)
    qiT = scr("qiT", [4096, 2048], BF16)
    wi3 = scr("wi3", [16 * 32 * 128], F32)
    cqnT = scr("cqnT", [1024, 2048], BF16)
    qnT = scr("qnT", [2048, 2048], BF16)
    qrT = scr("qrT", [16 * 64, 2048], BF16)
    mixT = scr("mixT", [4096, 2048], BF16)
    x2T = scr("x2T", [4096, 2048], F32)
    h2T = scr("h2T", [4096, 2048], BF16)
    hidT = scr("hidT", [16384, 2048], BF16)
    x3T = scr("x3T", [4096, 2048], F32)
    tdr = scr("tdr", [1, 64], F32)

    try:
        _build_body(nc, locals())
    except StopBuild:
        pass
    return nc


def _build_body(nc, L):
    globals().update({k: v for k, v in L.items() if k not in ("nc",)})
    with ExitStack() as gst:
        P = Prog(nc, gst)

        def gsb(name, shape, dt):
            return gst.enter_context(sbt(name, list(shape), dt))

        ident = gsb("ident", [128, 128], F32)
        ones_bf = gsb("ones_bf", [128, 128], BF16)
        modT = gsb("modT", [128, 192], F32)
        gs1 = gsb("gs1", [128, 32], F32)
        gs2 = gsb("gs2", [128, 32], F32)
        fg = gsb("fg", [128, 32], F32)
        kvg = gsb("kvg", [128, 4], F32)
        qg = gsb("qg", [128, 8], F32)
        posf = gsb("posf", [128, 32], F32)
        chkf = gsb("chkf", [128, 32], F32)
        cosT = gsb("cosT", [128, 32, 32], F32)
        sinT = gsb("sinT", [128, 32, 32], F32)
        r2b = gsb("r2b", [128, 2048], F32)
        rstd3 = gsb("rstd3", [128, 16], F32)
        tch = gsb("tch", [128, 8], F32)
        B_const = Buf()
        B_mod = Buf()
        B_r2b = Buf()
        B_r3 = Buf()
        banks = [gst.enter_context(nc.psum_tensor("bank%d" % i, [128, 512], F32)) for i in range(8)]
        BK = [Buf() for _ in range(8)]
        sh1 = modT[:, 0:32]
        gate1 = modT[:, 64:96]
        sh2 = modT[:, 96:128]
        gate2 = modT[:, 160:192]

        touch = {
            "act": lambda e: e.activation(out=tch[0:1, 0:1], in_=tch[0:1, 1:2], func=AF.Copy),
            "dve": lambda e: e.tensor_copy(out=tch[0:1, 2:3], in_=tch[0:1, 3:4]),
            "pool": lambda e: e.memset(tch[0:1, 4:5], 0.0),
            "sp": lambda e: e.dma_start(out=tdr[0:1, 0:2], in_=tch[0:1, 6:8]),
        }

        def rsqrt_ops(dst, src, scale, reads, writes):
            P.dve(lambda e: e.tensor_scalar(out=dst, in0=src, scalar1=scale, scalar2=EPS, op0=ALU.mult, op1=ALU.add),
                  reads=reads, writes=writes)
            P.act(lambda e: e.activation(out=dst, in_=dst, func=AF.Sqrt), reads=writes, writes=writes)
            P.dve(lambda e: e.reciprocal(out=dst, in_=dst), reads=writes, writes=writes)

        with ExitStack() as st:
            def sb(name, shape, dt):
                return st.enter_context(sbt(name, list(shape), dt))

            io_i = sb("io_i", [128, 128], I32)
            P.pool(lambda e: e.iota(io_i[:], pattern=[[1, 128]], base=0, channel_multiplier=-1), writes=[B_const])
            P.dve(lambda e: e.tensor_copy(out=ident[:], in_=io_i[:]), reads=[B_const], writes=[B_const])
            P.dve(lambda e: e.tensor_scalar(out=ident[:], in0=ident[:], scalar1=0.0, scalar2=None, op0=ALU.is_equal),
                  reads=[B_const], writes=[B_const])
            P.pool(lambda e: e.memset(ones_bf[:], 1.0), writes=[B_const])
            P.pool(lambda e: e.memset(tch[:], 0.0), writes=[B_const])
            Bp = Buf()
            ct_sb = sb("ct_sb", [128, 32], F32)
            badaT = sb("badaT", [128, 192], F32)
            g1s = sb("g1s", [128, 32], F32)
            g2s = sb("g2s", [128, 32], F32)
            posc_i = sb("posc_i", [128, 32], I32)
            chk_i = sb("chk_i", [128, 32], I32)
            for (dst, src) in ((ct_sb, cT), (badaT, b_adaT), (g1s, g1T), (g2s, g2T), (fg, fgT), (kvg, kvgT),
                               (qg, qgT), (posc_i, posc)):
                P.dma("sp", dst[:], src, writes=[Bp])
            P.dve(lambda e: e.tensor_copy(out=posf[:], in_=posc_i[:]), reads=[Bp], writes=[B_const])
            P.dve(lambda e: e.tensor_scalar(out=chk_i[:], in0=posc_i[:], scalar1=6, scalar2=None,
                                            op0=ALU.arith_shift_right), reads=[Bp], writes=[Bp])
            P.dve(lambda e: e.tensor_copy(out=chkf[:], in_=chk_i[:]), reads=[Bp], writes=[B_const])
            inv_i = sb("inv_i", [128, 32], I32)
            invf = sb("invf", [128, 32], F32)
            for i_ in range(32):
                P.pool(lambda e, i_=i_: e.memset(invf[:, i_:i_ + 1], float(np.float32(10000.0) ** np.float32(-2.0 * i_ / 64.0))),
                       writes=[Bp])
            rr = sb("rr", [128, 32, 32], F32)
            ri2 = [sb("ri%d" % i_, [128, 32, 32], I32) for i_ in range(2)]
            rf2 = [sb("rf%d" % i_, [128, 32, 32], F32) for i_ in range(2)]
            Br = Buf()
            for blk in range(32):
                P.dve(lambda e, blk=blk: e.tensor_scalar(out=rr[:, blk, :], in0=invf[:], scalar1=posf[:, blk:blk + 1],
                                                         scalar2=1.0 / (2 * math.pi), op0=ALU.mult, op1=ALU.mult),
                      reads=[Bp, B_const], writes=[Br])
            Bt = Buf()
            for (tab, off) in ((cosT, 0.25), (sinT, 0.0)):
                rrf = rr[:].rearrange("p a b -> p (a b)")
                rif = ri2[0 if off else 1][:].rearrange("p a b -> p (a b)")
                rff = rf2[0 if off else 1][:].rearrange("p a b -> p (a b)")
                tabf = tab[:].rearrange("p a b -> p (a b)")
                P.dve(lambda e, off=off, rff=rff, rrf=rrf: e.tensor_scalar(out=rff, in0=rrf, scalar1=off, scalar2=None,
                                                                           op0=ALU.add), reads=[Br], writes=[Bt])
                P.dve(lambda e, rff=rff, rif=rif: e.tensor_copy(out=rif, in_=rff), reads=[Bt], writes=[Bt])
                P.dve(lambda e, tabf=tabf, rif=rif: e.tensor_copy(out=tabf, in_=rif), reads=[Bt], writes=[B_const])
                P.dve(lambda e, rff=rff, tabf=tabf: e.tensor_tensor(out=rff, in0=rff, in1=tabf, op=ALU.subtract),
                      reads=[Bt, B_const], writes=[Bt])
                P.dve(lambda e, rff=rff, tabf=tabf: e.tensor_scalar(out=tabf, in0=rff, scalar1=0.5, scalar2=None,
                                                                    op0=ALU.is_gt), reads=[Bt], writes=[B_const])
                P.dve(lambda e, rff=rff, tabf=tabf: e.tensor_tensor(out=rff, in0=rff, in1=tabf, op=ALU.subtract),
                      reads=[Bt, B_const], writes=[Bt])
                P.dve(lambda e, rff=rff, tabf=tabf: e.tensor_scalar(out=tabf, in0=rff, scalar1=-0.5, scalar2=None,
                                                                    op0=ALU.is_lt), reads=[Bt], writes=[B_const])
                P.dve(lambda e, rff=rff, tabf=tabf: e.tensor_tensor(out=rff, in0=rff, in1=tabf, op=ALU.add),
                      reads=[Bt, B_const], writes=[Bt])
                P.act(lambda e, rff=rff, tabf=tabf: e.activation(out=tabf, in_=rff, func=AF.Sin, scale=2 * math.pi),
                      reads=[Bt], writes=[B_const])
            if "dbgtab" in DBG_OUT:
                dbgtab = scr("dbgtab", [128, 3072], F32)
                P.dma("sp", dbgtab[:, 0:1024], cosT[:].rearrange("p a b -> p (a b)"), reads=[B_const, Bt])
                P.dma("sp", dbgtab[:, 1024:2048], sinT[:].rearrange("p a b -> p (a b)"), reads=[B_const, Bt])
                P.dma("sp", dbgtab[:, 2048:3072], rr[:].rearrange("p a b -> p (a b)"), reads=[Br, Bt])
            scT = sb("scT", [128, 32], BF16)
            P.act(lambda e: e.activation(out=scT[:], in_=ct_sb[:], func=AF.Silu), reads=[Bp], writes=[Bp])
            wts = [sb("wa%d" % i, [128, 32, 128], BF16) for i in range(3)]
            Bw = [Buf() for _ in range(3)]
            war = w_ada.rearrange("(k p) e -> p k e", p=128)
            for ec in range(192):
                s = ec % 3
                P.dma("pool", wts[s][:], war[:, :, ec * 128:(ec + 1) * 128], writes=[Bw[s]])
                for k in range(32):
                    P.pe(lambda e, s=s, k=k, ec=ec: e.matmul(banks[0][:, ec:ec + 1], lhsT=wts[s][:, k, :],
                                                             rhs=scT[:, k:k + 1], start=(k == 0), stop=(k == 31)),
                         reads=[Bw[s], Bp], writes=[BK[0]])
            P.dve(lambda e: e.tensor_tensor(out=modT[:], in0=banks[0][:, 0:192], in1=badaT[:], op=ALU.add),
                  reads=[BK[0], Bp], writes=[B_mod])
            P.dve(lambda e: e.scalar_tensor_tensor(out=gs1[:], in0=modT[:, 32:64], scalar=1.0, in1=g1s[:],
                                                   op0=ALU.add, op1=ALU.mult), reads=[B_mod, Bp], writes=[B_mod])
            P.dve(lambda e: e.scalar_tensor_tensor(out=gs2[:], in0=modT[:, 128:160], scalar=1.0, in1=g2s[:],
                                                   op0=ALU.add, op1=ALU.mult), reads=[B_mod, Bp], writes=[B_mod])
            P.end_stage(touch)

        with ExitStack() as st:
            def sb(name, shape, dt):
                return st.enter_context(sbt(name, list(shape), dt))

            xb = [sb("xb%d" % i, [128, 4096], F32) for i in range(2)]
            Bx = [Buf() for _ in range(2)]
            junk = sb("junk", [128, 4096], BF16)
            Bj = Buf()
            hst = [sb("hst%d" % i, [128, 32, 512], BF16) for i in range(2)]
            Bh = [Buf() for _ in range(2)]
            xst = [sb("xst%d" % i, [128, 32, 128], F32) for i in range(2)]
            Bxs = [Buf() for _ in range(2)]
            ssq = sb("ssq", [128, 32], F32)
            Bs = Buf()
            h1Tr = h1T.rearrange("(k p) t -> p k t", p=128)
            xTr = xT.rearrange("(k p) t -> p k t", p=128)
            bi = 0
            for blk in range(32):
                s = blk % 2
                x_ = xb[s]
                P.dma("sp", x_[:], xc[blk * 128:(blk + 1) * 128, :], writes=[Bx[s]])
                if blk < 16:
                    xs = xst[blk % 2]
                    for k in range(32):
                        b = bi % 2
                        bi_k = k % 4
                        P.pe(lambda e, x_=x_, k=k, b=b, bi_k=bi_k: e.transpose(
                            banks[b][:, bi_k * 128:(bi_k + 1) * 128], x_[:, k * 128:(k + 1) * 128], ident[:]),
                            reads=[Bx[s], B_const], writes=[BK[b]])
                        if bi_k == 3:
                            P.dve(lambda e, xs=xs, k=k, b=b: e.tensor_copy(
                                out=xs[:, k - 3:k + 1, :], in_=banks[b][:].rearrange("p (a t) -> p a t", a=4)),
                                reads=[BK[b]], writes=[Bxs[blk % 2]])
                            bi += 1
                    P.dma("sp", xTr[:, :, blk * 128:(blk + 1) * 128], xs[:], reads=[Bxs[blk % 2]])
                P.act(lambda e, x_=x_, blk=blk: e.activation(out=junk[:], in_=x_[:], func=AF.Square,
                                                             accum_out=ssq[:, blk:blk + 1]),
                      reads=[Bx[s]], writes=[Bj, Bs])
                rsqrt_ops(ssq[:, blk:blk + 1], ssq[:, blk:blk + 1], 1.0 / D, [Bs], [Bs])
                P.dve(lambda e, x_=x_, blk=blk: e.tensor_scalar(out=x_[:], in0=x_[:], scalar1=ssq[:, blk:blk + 1],
                                                                scalar2=None, op0=ALU.mult),
                      reads=[Bx[s], Bs], writes=[Bx[s]])
                hs = hst[(blk // 4) % 2]
                Bhs = Bh[(blk // 4) % 2]
                tb = blk % 4
                for k in range(32):
                    b = 2 + (bi % 2)
                    bi_k = k % 4
                    P.pe(lambda e, x_=x_, k=k, b=b, bi_k=bi_k: e.transpose(
                        banks[b][:, bi_k * 128:(bi_k + 1) * 128], x_[:, k * 128:(k + 1) * 128], ident[:]),
                        reads=[Bx[s], B_const], writes=[BK[b]])
                    if bi_k == 3:
                        for kk in range(k - 3, k + 1):
                            P.act(lambda e, hs=hs, kk=kk, b=b, tb=tb: e.activation(
                                out=hs[:, kk, tb * 128:(tb + 1) * 128], in_=banks[b][:, (kk % 4) * 128:(kk % 4 + 1) * 128],
                                func=AF.Identity, scale=gs1[:, kk:kk + 1], bias=sh1[:, kk:kk + 1]),
                                reads=[BK[b], B_mod], writes=[Bhs])
                        bi += 1
                if tb == 3:
                    t0 = (blk // 4) * 512
                    P.dma("sp", h1Tr[:, :, t0:t0 + 512], hs[:], reads=[Bhs])
            P.end_stage(touch)

        def run_gemm(st, XT, KC, n_tt, jobs, xbufs=1, pre_tt=None):
            xts = [st.enter_context(sbt("xt%d" % i, [128, KC, 512], BF16)) for i in range(xbufs)]
            Bxt = [Buf() for _ in range(xbufs)]
            XTr = XT.rearrange("(k p) t -> p k t", p=128)
            for tt in range(n_tt):
                s = tt % xbufs
                for k0 in range(0, KC, 32):
                    k1 = min(KC, k0 + 32)
                    P.dma("sp", xts[s][:, k0:k1, :], XTr[:, k0:k1, tt * 512:(tt + 1) * 512], writes=[Bxt[s]])
                if pre_tt is not None:
                    pre_tt(tt)
                for job in jobs:
                    job(xts[s], Bxt[s], tt)

        class WStream:
            def __init__(self, st, n=3):
                self.t = [st.enter_context(sbt("ws%d" % i, [128, 32, 128], BF16)) for i in range(n)]
                self.B = [Buf() for _ in range(n)]
                self.i = 0

            def load(self, W, r0, nk, c0, ncol):
                s = self.i % len(self.t)
                self.i += 1
                src = W[r0:r0 + nk * 128, c0:c0 + ncol].rearrange("(k p) e -> p k e", p=128)
                P.dma("pool", self.t[s][:, 0:nk, 0:ncol], src, writes=[self.B[s]])
                return self.t[s], self.B[s]

        gb = [0]

        def gbank():
            gb[0] += 1
            return gb[0] % 2

        def fm_job(ws, W, KC, c0, ncol, epi):
            def job(xt, Bxt, tt):
                b = gbank()
                for k0 in range(0, KC, 32):
                    nk = min(32, KC - k0)
                    wt, Bw = ws.load(W, k0 * 128, nk, c0, ncol)
                    for k in range(nk):
                        P.pe(lambda e, wt=wt, k=k, k0=k0, b=b: e.matmul(
                            banks[b][0:ncol, :], lhsT=wt[:, k, 0:ncol], rhs=xt[:, k0 + k, :],
                            start=(k0 + k == 0), stop=(k0 + k == KC - 1)), reads=[Bw, Bxt], writes=[BK[b]])
                epi(tt, b)
            return job

        def run_gemm_pair(st, XT, KC, n_tt, jobs):
            xts = [st.enter_context(sbt("xtp%d" % i, [128, KC, 512], BF16)) for i in range(2)]
            Bxt = [Buf() for _ in range(2)]
            XTr = XT.rearrange("(k p) t -> p k t", p=128)
            for tp_ in range(n_tt // 2):
                for s in range(2):
                    tt = 2 * tp_ + s
                    P.dma("sp", xts[s][:, 0:KC, :], XTr[:, 0:KC, tt * 512:(tt + 1) * 512], writes=[Bxt[s]])
                for job in jobs:
                    job(xts, Bxt, 2 * tp_)

        pbk = [0]

        def fm_job_pair(ws, W, c0, ncol, epi):
            def job(xts, Bxt, tt0):
                pbk[0] ^= 1
                bb = (0, 1) if pbk[0] else (2, 3)
                wt, Bw = ws.load(W, 0, 32, c0, ncol)
                for k in range(32):
                    for s in range(2):
                        b = bb[s]
                        P.pe(lambda e, wt=wt, k=k, b=b, s=s: e.matmul(
                            banks[b][0:ncol, :], lhsT=wt[:, k, 0:ncol], rhs=xts[s][:, k, :],
                            start=(k == 0), stop=(k == 31)), reads=[Bw, Bxt[s]], writes=[BK[b]])
                epi(tt0, bb[0])
                epi(tt0 + 1, bb[1])
            return job

        with ExitStack() as st:
            def sb(name, shape, dt):
                return st.enter_context(sbt(name, list(shape), dt))

            ws = WStream(st)
            ost = [sb("ost%d" % i, [128, 512], BF16) for i in range(3)]
            Bo = [Buf() for _ in range(3)]
            oi = [0]

            def copy_out(dst_fn, scale=1.0):
                def epi(tt, b):
                    s = oi[0] % 3
                    oi[0] += 1
                    P.act(lambda e, s=s, b=b: e.activation(out=ost[s][:], in_=banks[b][:], func=AF.Copy, scale=scale),
                          reads=[BK[b]], writes=[Bo[s]])
                    P.dma("sp", dst_fn(tt), ost[s][:], reads=[Bo[s]])
                return epi

            ckv = sb("ckv", [128, 4, 512], F32)
            sqb = sb("sqb", [128, 4, 512], BF16)
            rb = sb("rb", [128, 512], F32)
            kvn = sb("kvn", [128, 4, 512], BF16)
            Bc = Buf()
            Bsq = Buf()
            Brb = Buf()
            Bkvn = Buf()
            kvnTr = kvnT.rearrange("(k p) t -> p k t", p=128)

            def ckv_epi(j):
                def epi(tt, b):
                    P.dve(lambda e, b=b: e.tensor_copy(out=ckv[:, j, :], in_=banks[b][:]), reads=[BK[b]], writes=[Bc])
                    P.act(lambda e, b=b: e.activation(out=sqb[:, j, :], in_=ckv[:, j, :], func=AF.Square),
                          reads=[Bc], writes=[Bsq])
                    if j == 3:
                        for jj in range(4):
                            P.pe(lambda e, jj=jj: e.matmul(banks[4][:], lhsT=ones_bf[:], rhs=sqb[:, jj, :],
                                                           start=(jj == 0), stop=(jj == 3)),
                                 reads=[Bsq, B_const], writes=[BK[4]])
                        rsqrt_ops(rb[:], banks[4][:], 1.0 / 512, [BK[4]], [Brb])
                        for jj in range(4):
                            P.dve(lambda e, jj=jj: e.scalar_tensor_tensor(
                                out=kvn[:, jj, :], in0=ckv[:, jj, :], scalar=kvg[:, jj:jj + 1], in1=rb[:],
                                op0=ALU.mult, op1=ALU.mult), reads=[Bc, Brb, Bp0], writes=[Bkvn])
                        P.dma("sp", kvnTr[:, :, tt * 512:(tt + 1) * 512], kvn[:], reads=[Bkvn])
                return epi

            Bp0 = B_const
            wtm = sb("wtm", [128, 32, 192], BF16)
            Bwtm = Buf()
            w_in_r = w_in.rearrange("(k p) e -> p k e", p=128)
            P.dma("pool", wtm[:, :, 0:128], w_in_r[:, :, 2176:2304], writes=[Bwtm])
            P.dma("pool", wtm[:, :, 128:192], w_in_r[:, :, 8096:8160], writes=[Bwtm])
            vst = [sb("vst%d" % i, [128, 128], BF16) for i in range(2)]
            Bv = [Buf() for _ in range(2)]
            krf = sb("krf", [128, 64], F32)
            kro = sb("kro", [128, 64], F32)
            tmp1 = sb("tmp1", [128, 32], F32)
            Bkr = Buf()
            krst = sb("krst", [64, 512], BF16)
            Bkrst = Buf()

            def rope_tok(e_list, src, dst, blk, nh, tmp):
                c = cosT[:, blk, :].unsqueeze(1).to_broadcast([128, nh, 32])
                s_ = sinT[:, blk, :].unsqueeze(1).to_broadcast([128, nh, 32])
                x1 = src[:, :, 0:32]
                x2 = src[:, :, 32:64]
                ops = [
                    (dst[:, :, 0:32], x1, c, ALU.mult), (tmp, x2, s_, ALU.mult),
                    (dst[:, :, 0:32], dst[:, :, 0:32], tmp, ALU.subtract),
                    (dst[:, :, 32:64], x1, s_, ALU.mult), (tmp, x2, c, ALU.mult),
                    (dst[:, :, 32:64], dst[:, :, 32:64], tmp, ALU.add)]
                for (o, a, b_, op) in ops:
                    P.dve(lambda e, o=o, a=a, b_=b_, op=op: e.tensor_tensor(out=o, in0=a, in1=b_, op=op),
                          reads=e_list[0], writes=e_list[1])

            def tm_job_k(xt, Bxt, tt):
                for tb in range(4):
                    blk = tt * 4 + tb
                    b = 2 + (tb % 2)
                    for k in range(32):
                        P.pe(lambda e, k=k, b=b, tb=tb: e.matmul(banks[b][:, 0:192], lhsT=xt[:, k, tb * 128:(tb + 1) * 128],
                                                                 rhs=wtm[:, k, :], start=(k == 0), stop=(k == 31)),
                             reads=[Bxt, Bwtm], writes=[BK[b]])
                    s = blk % 2
                    P.act(lambda e, s=s, b=b: e.activation(out=vst[s][:], in_=banks[b][:, 0:128], func=AF.Copy),
                          reads=[BK[b]], writes=[Bv[s]])
                    P.dma("sp", va[blk * 128:(blk + 1) * 128, :], vst[s][:], reads=[Bv[s]])
                    P.act(lambda e, b=b: e.activation(out=krf[:], in_=banks[b][:, 128:192], func=AF.Copy), reads=[BK[b]],
                          writes=[Bkr])
                    rope_tok(([Bkr, B_const], [Bkr]), krf[:].unsqueeze(1), kro[:].unsqueeze(1), blk, 1,
                             tmp1[:].unsqueeze(1))
                    P.pe(lambda e: e.transpose(banks[5][0:64, 0:128], kro[:], ident[:]), reads=[Bkr, B_const],
                         writes=[BK[5]])
                    P.act(lambda e, tb=tb: e.activation(out=krst[:, tb * 128:(tb + 1) * 128], in_=banks[5][0:64, 0:128],
                                                        func=AF.Copy), reads=[BK[5]], writes=[Bkrst])
                P.dma("sp", krT[:, tt * 512:(tt + 1) * 512], krst[:], reads=[Bkrst])

            jobs = [fm_job(ws, w_in, 32, 2048, 128, copy_out(lambda tt: kaT[:, tt * 512:(tt + 1) * 512])),
                    fm_job(ws, w_in, 32, 6400, 128, copy_out(lambda tt: kiT[:, tt * 512:(tt + 1) * 512]))]
            for j in range(4):
                jobs.append(fm_job(ws, w_in, 32, 7584 + 128 * j, 128, ckv_epi(j)))
            jobs.append(tm_job_k)
            _sub = int(os.environ.get("KSUB", "255"))
            jobs = [jb for n_, jb in enumerate(jobs) if (_sub >> n_) & 1]
            run_gemm(st, h1T, 32, 8, jobs, xbufs=2)
            P.end_stage(touch)

        with ExitStack() as st:
            def sb(name, shape, dt):
                return st.enter_context(sbt(name, list(shape), dt))

            ws = WStream(st)
            ost = [sb("ost%d" % i, [128, 512], BF16) for i in range(3)]
            Bo = [Buf() for _ in range(3)]
            oi = [0]

            def kn_epi(h):
                def epi(tt, b):
                    s = oi[0] % 3
                    oi[0] += 1
                    P.act(lambda e, s=s, b=b: e.activation(out=ost[s][:], in_=banks[b][:], func=AF.Copy),
                          reads=[BK[b]], writes=[Bo[s]])
                    P.dma("sp", knT[h * 128:(h + 1) * 128, tt * 512:(tt + 1) * 512], ost[s][:], reads=[Bo[s]])
                return epi

            wuv = sb("wuv", [128, 4, 2048], BF16)
            Bwuv = Buf()
            P.dma("pool", wuv[:], w_uv.rearrange("(k p) e -> p k e", p=128), writes=[Bwuv])
            vbst = [sb("vbst%d" % i, [128, 2048], BF16) for i in range(2)]
            Bvb = [Buf() for _ in range(2)]

            def tm_job_vb(xt, Bxt, tt):
                for tb in range(4):
                    blk = tt * 4 + tb
                    s = blk % 2
                    for eg in range(4):
                        b = 2 + (eg % 2)
                        for k in range(4):
                            P.pe(lambda e, k=k, b=b, tb=tb, eg=eg: e.matmul(
                                banks[b][:], lhsT=xt[:, k, tb * 128:(tb + 1) * 128],
                                rhs=wuv[:, k, eg * 512:(eg + 1) * 512], start=(k == 0), stop=(k == 3)),
                                reads=[Bxt, Bwuv], writes=[BK[b]])
                        P.act(lambda e, s=s, b=b, eg=eg: e.activation(out=vbst[s][:, eg * 512:(eg + 1) * 512],
                                                                      in_=banks[b][:], func=AF.Copy),
                              reads=[BK[b]], writes=[Bvb[s]])
                    P.dma("sp", vb[blk * 128:(blk + 1) * 128, :], vbst[s][:], reads=[Bvb[s]])

            jobs = [fm_job(ws, w_uk, 4, h * 128, 128, kn_epi(h)) for h in range(16)]
            jobs.append(tm_job_vb)
            run_gemm(st, kvnT, 4, 8, jobs, xbufs=2)
            P.end_stage(touch)

        with ExitStack() as st:
            def sb(name, shape, dt):
                return st.enter_context(sbt(name, list(shape), dt))

            ws = WStream(st)
            ost = [sb("ost%d" % i, [128, 512], BF16) for i in range(3)]
            Bo = [Buf() for _ in range(3)]
            oi = [0]

            def copy_out(dst_fn, scale=1.0):
                def epi(tt, b):
                    s = oi[0] % 3
                    oi[0] += 1
                    P.act(lambda e, s=s, b=b: e.activation(out=ost[s][:], in_=banks[b][:], func=AF.Copy, scale=scale),
                          reads=[BK[b]], writes=[Bo[s]])
                    P.dma("sp", dst_fn(tt), ost[s][:], reads=[Bo[s]])
                return epi

            cq = sb("cq", [128, 8, 512], F32)
            sqb = sb("sqb", [128, 8, 512], BF16)
            rb = sb("rb", [128, 512], F32)
            cqn = sb("cqn", [128, 8, 512], BF16)
            Bc, Bsq, Brb, Bcqn = Buf(), Buf(), Buf(), Buf()
            cqnTr = cqnT.rearrange("(k p) t -> p k t", p=128)

            def cq_epi(j):
                def epi(tt, b):
                    P.dve(lambda e, b=b: e.tensor_copy(out=cq[:, j, :], in_=banks[b][:]), reads=[BK[b]], writes=[Bc])
                    P.act(lambda e, b=b: e.activation(out=sqb[:, j, :], in_=cq[:, j, :], func=AF.Square),
                          reads=[Bc], writes=[Bsq])
                    if j == 7:
                        for jj in range(8):
                            P.pe(lambda e, jj=jj: e.matmul(banks[4][:], lhsT=ones_bf[:], rhs=sqb[:, jj, :],
                                                           start=(jj == 0), stop=(jj == 7)),
                                 reads=[Bsq, B_const], writes=[BK[4]])
                        rsqrt_ops(rb[:], banks[4][:], 1.0 / 1024, [BK[4]], [Brb])
                        for jj in range(8):
                            P.dve(lambda e, jj=jj: e.scalar_tensor_tensor(
                                out=cqn[:, jj, :], in0=cq[:, jj, :], scalar=qg[:, jj:jj + 1], in1=rb[:],
                                op0=ALU.mult, op1=ALU.mult), reads=[Bc, Brb, B_const], writes=[Bcqn])
                        P.dma("sp", cqnTr[:, :, tt * 512:(tt + 1) * 512], cqn[:], reads=[Bcqn])
                return epi

            wist = sb("wist", [32, 512], F32)
            Bwi = Buf()
            IDX_SCALE = (128 ** -0.5) * (32 ** -0.5)

            def wi_epi(tt, b):
                P.act(lambda e, b=b: e.activation(out=wist[:], in_=banks[b][0:32, :], func=AF.Copy, scale=IDX_SCALE),
                      reads=[BK[b]], writes=[Bwi])
                dst = wi3[tt * 4 * 4096:(tt + 1) * 4 * 4096].rearrange("(bg f h) -> h bg f", h=32, f=4)
                P.dma("sp", dst, wist[:].rearrange("h (bg f) -> h bg f", f=4), reads=[Bwi], slow=True)

            jobs = []
            for h in range(16):
                jobs.append(fm_job(ws, w_in, 32, h * 128, 128,
                                   copy_out(lambda tt, h=h: qaT[h * 128:(h + 1) * 128, tt * 512:(tt + 1) * 512],
                                            scale=128 ** -0.5)))
            for h in range(32):
                jobs.append(fm_job(ws, w_in, 32, 2304 + h * 128, 128,
                                   copy_out(lambda tt, h=h: qiT[h * 128:(h + 1) * 128, tt * 512:(tt + 1) * 512])))
            jobs.append(fm_job(ws, w_in, 32, 6528, 32, wi_epi))
            for j in range(8):
                jobs.append(fm_job(ws, w_in, 32, 6560 + 128 * j, 128, cq_epi(j)))
            run_gemm(st, h1T, 32, 4, jobs, xbufs=2)
            P.end_stage(touch)

        with ExitStack() as st:
            def sb(name, shape, dt):
                return st.enter_context(sbt(name, list(shape), dt))

            QS = 192 ** -0.5
            wq = sb("wq", [128, 8, 3072], BF16)
            Bwq = Buf()
            wqr = w_uq.rearrange("(k p) e -> p k e", p=128)
            for k in range(8):
                P.dma("pool", wq[:, k, :], wqr[:, k, :], writes=[Bwq])
            ost = [sb("ost%d" % i, [128, 512], BF16) for i in range(3)]
            Bo = [Buf() for _ in range(3)]
            oi = [0]
            qrf = sb("qrf", [128, 8, 64], F32)
            qro = sb("qro", [128, 8, 64], F32)
            tmp8 = sb("tmp8", [128, 8, 32], F32)
            Bqr = Buf()
            qrst = sb("qrst", [64, 16, 512], BF16)
            Bqrst = Buf()
            wqrp = sb("wqrp", [128, 8, 16, 64], BF16)
            for k in range(8):
                P.dma("pool", wqrp[:, k, :, :], wqr[:, k, :].rearrange("p (h c) -> p h c", c=192)[:, :, 128:192],
                      writes=[Bwq])

            def qup_job(xt, Bxt, tt):
                for h in range(16):
                    b = gbank()
                    for k in range(8):
                        P.pe(lambda e, k=k, b=b, h=h: e.matmul(banks[b][:], lhsT=wq[:, k, h * 192:h * 192 + 128],
                                                               rhs=xt[:, k, :], start=(k == 0), stop=(k == 7)),
                             reads=[Bxt, Bwq], writes=[BK[b]])
                    s = oi[0] % 3
                    oi[0] += 1
                    P.act(lambda e, s=s, b=b: e.activation(out=ost[s][:], in_=banks[b][:], func=AF.Copy, scale=QS),
                          reads=[BK[b]], writes=[Bo[s]])
                    P.dma("sp", qnT[h * 128:(h + 1) * 128, tt * 512:(tt + 1) * 512], ost[s][:], reads=[Bo[s]])
                for tb in range(4):
                    blk = tt * 4 + tb
                    for hg in range(2):
                        b = 2 + hg
                        for k in range(8):
                            P.pe(lambda e, k=k, b=b, tb=tb, hg=hg: e.matmul(
                                banks[b][:],
                                lhsT=xt[:, k, tb * 128:(tb + 1) * 128], rhs=wqrp[:, k, hg * 8:(hg + 1) * 8, :],
                                start=(k == 0), stop=(k == 7)), reads=[Bxt, Bwq], writes=[BK[b]])
                        P.act(lambda e, b=b: e.activation(out=qrf[:].rearrange("p h c -> p (h c)"), in_=banks[b][:],
                                                          func=AF.Copy, scale=QS), reads=[BK[b]], writes=[Bqr])
                        rope_tok(([Bqr, B_const], [Bqr]), qrf[:], qro[:], blk, 8, tmp8[:])
                        for hh in range(8):
                            h = hg * 8 + hh
                            pb = 4 + (hh % 2)
                            P.pe(lambda e, hh=hh, pb=pb: e.transpose(banks[pb][0:64, 0:128], qro[:, hh, :], ident[:]),
                                 reads=[Bqr, B_const], writes=[BK[pb]])
                            P.act(lambda e, h=h, pb=pb, tb=tb: e.activation(
                                out=qrst[:, h, tb * 128:(tb + 1) * 128], in_=banks[pb][0:64, 0:128], func=AF.Copy),
                                reads=[BK[pb]], writes=[Bqrst])
                P.dma("sp", qrT.rearrange("(h c) t -> c h t", c=64)[:, :, tt * 512:(tt + 1) * 512], qrst[:],
                      reads=[Bqrst])

            run_gemm(st, cqnT, 8, 4, [qup_job], xbufs=2)
            P.end_stage(touch)

        slopes = [2.0 ** (-8.0 * (h + 1) / 16.0) for h in range(16)]

        with ExitStack() as st:
            def sb(name, shape, dt):
                return st.enter_context(sbt(name, list(shape), dt))

            kis = sb("kis", [128, 4096], BF16)
            kas = sb("kas", [128, 4096], BF16)
            vas = sb("vas", [128, 32, 128], BF16)
            Bk = Buf()
            P.dma("sp", kis[:], kiT, writes=[Bk])
            P.dma("sp", kas[:], kaT, writes=[Bk])
            P.dma("sp", vas[:], va.rearrange("(b p) d -> p b d", p=128), writes=[Bk])
            chkb = sb("chkb", [128, 4096], F32)
            posb = sb("posb", [128, 2048], F32)
            pb_i = sb("pb_i", [128, 4096], I32)
            Bpb = Buf()
            P.dma("sp", pb_i[:], posr.partition_broadcast(128)[:, 0, :], writes=[Bpb])
            P.dve(lambda e: e.tensor_copy(out=posb[:], in_=pb_i[:, 0:2048]), reads=[Bpb], writes=[Bk])
            P.dve(lambda e: e.tensor_scalar(out=pb_i[:], in0=pb_i[:], scalar1=6, scalar2=None,
                                            op0=ALU.arith_shift_right), reads=[Bpb, Bk], writes=[Bpb])
            P.dve(lambda e: e.tensor_copy(out=chkb[:], in_=pb_i[:]), reads=[Bpb], writes=[Bk])
            d4i = sb("d4i", [128, 4], I32)
            d4 = sb("d4", [128, 4], F32)
            d4b = sb("d4b", [128, 4], F32)
            P.pool(lambda e: e.iota(d4i[:], pattern=[[-32, 4]], base=0, channel_multiplier=1), writes=[Bk])
            P.dve(lambda e: e.tensor_copy(out=d4[:], in_=d4i[:]), reads=[Bk], writes=[Bk])
            P.dve(lambda e: e.tensor_scalar(out=d4b[:], in0=d4[:], scalar1=31.0, scalar2=None, op0=ALU.is_le),
                  reads=[Bk], writes=[Bk])
            P.dve(lambda e: e.tensor_scalar(out=d4[:], in0=d4[:], scalar1=0.0, scalar2=None, op0=ALU.is_ge),
                  reads=[Bk], writes=[Bk])
            P.dve(lambda e: e.tensor_tensor(out=d4[:], in0=d4[:], in1=d4b[:], op=ALU.mult), reads=[Bk], writes=[Bk])
            wbd = sb("wbd", [128, 32, 128], BF16)
            Bwbd = Buf()
            P.pool(lambda e: e.memset(wbd[:], 0.0), writes=[Bwbd])
            WT = sb("WT", [128, 32], F32)
            BWT = Buf()
            qib = [sb("qib%d" % i, [128, 128, 32], BF16) for i in range(2)]
            Bqi = [Buf() for _ in range(2)]
            qil = sb("qil", [128, 32, 128], BF16)
            Bqil = Buf()
            qab = [sb("qab%d" % i, [128, 16, 128], BF16) for i in range(2)]
            Bqa = [Buf() for _ in range(2)]
            Rt = [sb("Rt%d" % i, [128, 512], BF16) for i in range(4)]
            BR = [Buf() for _ in range(4)]
            Isc = sb("Isc", [128, 4096], F32)
            BI = Buf()
            pen = sb("pen", [128, 512], F32)
            Bpen = Buf()
            jk = sb("jk", [128, 4096], BF16)
            Bjk = Buf()
            sm = sb("sm", [128, 16], F32)
            Bsm = Buf()
            DmT = sb("DmT", [128, 32, 128], F32)
            BDm = Buf()
            mtmp = sb("mtmp", [128, 128], F32)
            Bmt = Buf()
            Zt = [sb("Zt%d" % i, [128, 512], F32) for i in range(2)]
            BZ = [[Buf() for _ in range(4)] for _ in range(2)]
            Pt = [sb("Pt%d" % i, [128, 512], BF16) for i in range(2)]
            BP = [Buf() for _ in range(2)]
            rden = sb("rden", [128, 512], F32)
            Brd = Buf()
            ostA = [sb("ostA%d" % i, [128, 4, 128], BF16) for i in range(2)]
            BoA = [Buf() for _ in range(2)]
            qiTr = qiT.rearrange("(h d) t -> d h t", d=128)
            qaTr = qaT.rearrange("(h d) t -> d h t", d=128)
            mixTr = mixT.rearrange("(h d) t -> d h t", d=128)
            ri_ = [0]
            for j in range(16):
                i = j // 4
                nkb = 4 * (i + 1)
                kbl = list(range(0, nkb)) + list(range(16, 16 + nkb))
                kgl = [kb for kb in kbl if kb % 4 == 0]
                nk = len(kbl) * 128
                qi_ = qib[j % 2]
                qa_ = qab[j % 2]
                P.dma("sp", qil[:], qiTr[:, :, j * 128:(j + 1) * 128], writes=[Bqil])
                P.pool(lambda e, qi_=qi_: e.tensor_copy(out=qi_[:], in_=qil[:].rearrange("p h t -> p t h")),
                       reads=[Bqil], writes=[Bqi[j % 2]])
                P.dma("sp", qa_[:], qaTr[:, :, j * 128:(j + 1) * 128], writes=[Bqa[j % 2]])
                P.dma("sp", WT[:], wi3[j * 4096:(j + 1) * 4096].rearrange("(g p) -> p g", p=128), writes=[BWT],
                      slow=True)
                for c4 in range(4):
                    dst = wbd[:].rearrange("p g c -> p (g c)")[:, c4:4096:132]
                    P.dve(lambda e, dst=dst, c4=c4: e.tensor_scalar(out=dst, in0=WT[:], scalar1=d4[:, c4:c4 + 1],
                                                                    scalar2=None, op0=ALU.mult),
                          reads=[BWT, Bk], writes=[Bwbd])
                for gi, kb0 in enumerate(kgl):
                    def emit_L(g, kb0=kb0, qi_=qi_):
                        lb = 2 + (g % 2)
                        P.pe(lambda e, g=g, lb=lb, kb0=kb0, qi_=qi_: e.matmul(
                            banks[lb][:], lhsT=qi_[:, 4 * g:4 * g + 4, :], rhs=kis[:, kb0 * 128:kb0 * 128 + 512],
                            start=True, stop=True), reads=[Bqi[j % 2], Bk], writes=[BK[lb]])
                    emit_L(0)
                    for g in range(32):
                        lb = 2 + (g % 2)
                        if g + 1 < 32:
                            emit_L(g + 1)
                        r = ri_[0] % 4
                        ri_[0] += 1
                        if r % 2 == 0:
                            P.act(lambda e, r=r, lb=lb: e.activation(out=Rt[r][:], in_=banks[lb][:], func=AF.Relu),
                                  reads=[BK[lb]], writes=[BR[r]])
                        else:
                            P.dve(lambda e, r=r, lb=lb: e.tensor_scalar(out=Rt[r][:], in0=banks[lb][:], scalar1=0.0,
                                                                        scalar2=None, op0=ALU.max),
                                  reads=[BK[lb]], writes=[BR[r]])
                        P.pe(lambda e, g=g, r=r: e.matmul(banks[4][:], lhsT=wbd[:, g, :], rhs=Rt[r][:],
                                                          start=(g == 0), stop=(g == 31)),
                             reads=[Bwbd, BR[r]], writes=[BK[4]])
                    P.dve(lambda e, kb0=kb0, j=j: e.tensor_scalar(
                        out=pen[:], in0=chkb[:, kb0 * 128:kb0 * 128 + 512], scalar1=chkf[:, j:j + 1], scalar2=-1e30,
                        op0=ALU.is_gt, op1=ALU.mult), reads=[Bk, B_const], writes=[Bpen])
                    P.dve(lambda e, gi=gi: e.tensor_tensor(out=Isc[:, gi * 512:(gi + 1) * 512], in0=banks[4][:],
                                                           in1=pen[:], op=ALU.add),
                          reads=[BK[4], Bpen], writes=[BI])
                Iv = Isc[:, 0:nk]
                P.dve(lambda e, Iv=Iv: e.reduce_max(out=sm[:, 0:1], in_=Iv, axis=AX.X), reads=[BI], writes=[Bsm])
                P.dve(lambda e: e.tensor_scalar(out=sm[:, 0:1], in0=sm[:, 0:1], scalar1=-7.5, scalar2=None,
                                                op0=ALU.add), reads=[Bsm], writes=[Bsm])
                wdt = 8.5
                for it in range(NBIS):
                    P.dve(lambda e, Iv=Iv, nk=nk: e.tensor_scalar(out=jk[:, 0:nk], in0=Iv, scalar1=sm[:, 0:1],
                                                                  scalar2=None, op0=ALU.is_ge, op1=ALU.add,
                                                                  accum_out=sm[:, 2:3]),
                          reads=[BI, Bsm], writes=[Bjk, Bsm])
                    nw = wdt * 0.5 if it < NBIS - 1 else wdt
                    mul = 2.0 * nw if it < NBIS - 1 else wdt
                    P.dve(lambda e, mul=mul: e.tensor_scalar(out=sm[:, 3:4], in0=sm[:, 2:3], scalar1=255.5, scalar2=mul,
                                                             op0=ALU.is_ge, op1=ALU.mult), reads=[Bsm], writes=[Bsm])
                    P.dve(lambda e, nw=nw: e.scalar_tensor_tensor(out=sm[:, 0:1], in0=sm[:, 0:1], scalar=-nw,
                                                                  in1=sm[:, 3:4], op0=ALU.add, op1=ALU.add),
                          reads=[Bsm], writes=[Bsm])
                    wdt = nw
                P.dve(lambda e, Iv=Iv, nk=nk: e.tensor_scalar(out=jk[:, 0:nk], in0=Iv, scalar1=-1e29, scalar2=None,
                                                              op0=ALU.is_ge, op1=ALU.add, accum_out=sm[:, 4:5]),
                      reads=[BI, Bsm], writes=[Bjk, Bsm])
                P.dve(lambda e: e.tensor_scalar(out=sm[:, 5:6], in0=sm[:, 4:5], scalar1=256.5, scalar2=None,
                                                op0=ALU.is_gt), reads=[Bsm], writes=[Bsm])
                P.dve(lambda e: e.tensor_tensor(out=sm[:, 6:7], in0=sm[:, 0:1], in1=sm[:, 5:6], op=ALU.mult),
                      reads=[Bsm], writes=[Bsm])
                P.dve(lambda e: e.tensor_scalar(out=sm[:, 7:8], in0=sm[:, 5:6], scalar1=-1.0, scalar2=1e29,
                                                op0=ALU.add, op1=ALU.mult), reads=[Bsm], writes=[Bsm])
                P.dve(lambda e: e.tensor_tensor(out=sm[:, 0:1], in0=sm[:, 6:7], in1=sm[:, 7:8], op=ALU.add),
                      reads=[Bsm], writes=[Bsm])
                P.dve(lambda e, Iv=Iv: e.tensor_scalar(out=Iv, in0=Iv, scalar1=sm[:, 0:1], scalar2=None, op0=ALU.is_ge),
                      reads=[BI, Bsm], writes=[BI])
                for kbi, kb in enumerate(kbl):
                    tbk = 5
                    P.pe(lambda e, kbi=kbi: e.transpose(banks[5][:, 0:128], Isc[:, kbi * 128:(kbi + 1) * 128], ident[:]),
                         reads=[BI, B_const], writes=[BK[5]])
                    P.dve(lambda e: e.tensor_scalar(out=mtmp[:], in0=banks[5][:, 0:128], scalar1=-1e6, scalar2=1e6,
                                                    op0=ALU.mult, op1=ALU.add), reads=[BK[5]], writes=[Bmt])
                    P.dve(lambda e, kbi=kbi, kb=kb, j=j: e.tensor_scalar(
                        out=DmT[:, kbi, :], in0=posb[:, j * 128:(j + 1) * 128], scalar1=posf[:, kb:kb + 1], scalar2=None,
                        op0=ALU.subtract), reads=[Bk, B_const], writes=[BDm])
                    P.dve(lambda e, kbi=kbi: e.scalar_tensor_tensor(
                        out=DmT[:, kbi, :], in0=DmT[:, kbi, :], scalar=-1.0, in1=DmT[:, kbi, :], op0=ALU.mult,
                        op1=ALU.max), reads=[BDm], writes=[BDm])
                    P.dve(lambda e, kbi=kbi: e.tensor_tensor(out=DmT[:, kbi, :], in0=DmT[:, kbi, :], in1=mtmp[:],
                                                             op=ALU.add), reads=[BDm, Bmt], writes=[BDm])
                for hg in range(4):
                    ob, db = 6, 7
                    def emit_S(kbi, hg=hg, qa_=qa_, kbl=kbl):
                        sbk = kbi % 2
                        kb = kbl[kbi]
                        P.pe(lambda e, sbk=sbk, kb=kb, hg=hg, qa_=qa_: e.matmul(
                            banks[sbk][:], lhsT=kas[:, kb * 128:(kb + 1) * 128],
                            rhs=qa_[:, 4 * hg:4 * hg + 4, :], start=True, stop=True),
                            reads=[Bk, Bqa[j % 2]], writes=[BK[sbk]])
                    emit_S(0)
                    for kbi, kb in enumerate(kbl):
                        sbk = kbi % 2
                        if kbi + 1 < len(kbl):
                            emit_S(kbi + 1)
                        z = Zt[kbi % 2]
                        for hh in range(4):
                            h = 4 * hg + hh
                            P.dve(lambda e, z=z, hh=hh, h=h, kbi=kbi, sbk=sbk: e.scalar_tensor_tensor(
                                out=z[:, hh * 128:(hh + 1) * 128], in0=DmT[:, kbi, :], scalar=-slopes[h],
                                in1=banks[sbk][:, hh * 128:(hh + 1) * 128], op0=ALU.mult, op1=ALU.add),
                                reads=[BDm, BK[sbk]], writes=[BZ[kbi % 2][hh]])
                        p_ = Pt[kbi % 2]
                        P.act(lambda e, z=z, p_=p_: e.activation(out=p_[:], in_=z[:], func=AF.Exp),
                              reads=BZ[kbi % 2], writes=[BP[kbi % 2]])
                        P.pe(lambda e, p_=p_, kb=kb, kbi=kbi, nl=len(kbl): e.matmul(banks[ob][:], lhsT=vas[:, kb, :], rhs=p_[:],
                                                                       start=(kbi == 0), stop=(kbi == nl - 1)),
                             reads=[Bk, BP[kbi % 2]], writes=[BK[ob]])
                        P.pe(lambda e, p_=p_, kbi=kbi, nl=len(kbl): e.matmul(banks[db][:], lhsT=ones_bf[:], rhs=p_[:],
                                                                start=(kbi == 0), stop=(kbi == nl - 1)),
                             reads=[B_const, BP[kbi % 2]], writes=[BK[db]])
                    P.dve(lambda e: e.reciprocal(out=rden[:], in_=banks[db][:]), reads=[BK[db]], writes=[Brd])
                    oa = ostA[hg % 2]
                    P.dve(lambda e, oa=oa: e.tensor_tensor(out=oa[:].rearrange("p h t -> p (h t)"), in0=banks[ob][:],
                                                           in1=rden[:], op=ALU.mult),
                          reads=[BK[ob], Brd], writes=[BoA[hg % 2]])
                    P.dma("sp", mixTr[:, 4 * hg:4 * hg + 4, j * 128:(j + 1) * 128], oa[:], reads=[BoA[hg % 2]])
            P.end_stage(touch)

        with ExitStack() as st:
            def sb(name, shape, dt):
                return st.enter_context(sbt(name, list(shape), dt))

            krs = sb("krs", [64, 4096], BF16)
            Bk = Buf()
            P.dma("sp", krs[:], krT, writes=[Bk])
            ctb = sb("ctb", [128, 2048], F32)
            pb_i = sb("pb_i", [128, 2048], I32)
            P.dma("sp", pb_i[:], posr[:, 0:2048].partition_broadcast(128)[:, 0, :], writes=[Bk])
            P.dve(lambda e: e.tensor_scalar(out=pb_i[:], in0=pb_i[:], scalar1=6, scalar2=None,
                                            op0=ALU.arith_shift_right), reads=[Bk], writes=[Bk])
            P.dve(lambda e: e.tensor_copy(out=ctb[:], in_=pb_i[:]), reads=[Bk], writes=[Bk])
            cm = sb("cm", [128, 8, 512], BF16)
            Bcm = Buf()
            kn = [sb("kn%d" % i, [128, 4096], BF16) for i in range(2)]
            vbh = [sb("vbh%d" % i, [128, 32, 128], BF16) for i in range(2)]
            Bkn = [Buf() for _ in range(2)]
            qn = [sb("qn%d" % i, [128, 512], BF16) for i in range(2)]
            qr = [sb("qr%d" % i, [64, 512], BF16) for i in range(2)]
            Bq = [Buf() for _ in range(2)]
            Pt = [sb("PtB%d" % i, [128, 512], BF16) for i in range(3)]
            BP = [Buf() for _ in range(3)]
            rden = sb("rdenB", [128, 512], F32)
            Brd = Buf()
            ostB = [sb("ostB%d" % i, [128, 512], BF16) for i in range(2)]
            BoB = [Buf() for _ in range(2)]
            vbr = vb.rearrange("(b p) e -> p b e", p=128)
            hi_ = 0
            pi_ = 0
            for i in range(4):
                nkb = 4 * (i + 1)
                kbl = list(range(0, nkb)) + list(range(16, 16 + nkb))
                band = list(range(4 * i, 4 * i + 4)) + list(range(16 + 4 * i, 16 + 4 * i + 4))
                for bi_, kb in enumerate(band):
                    P.dve(lambda e, bi_=bi_, kb=kb, i=i: e.tensor_scalar(
                        out=cm[:, bi_, :], in0=ctb[:, i * 512:(i + 1) * 512], scalar1=chkf[:, kb:kb + 1], scalar2=None,
                        op0=ALU.is_ge), reads=[Bk, B_const], writes=[Bcm])
                for h in range(16):
                    s = hi_ % 2
                    hi_ += 1
                    P.dma("sp", kn[s][:, 0:nkb * 128], knT[h * 128:(h + 1) * 128, 0:nkb * 128], writes=[Bkn[s]])
                    P.dma("sp", kn[s][:, 2048:2048 + nkb * 128], knT[h * 128:(h + 1) * 128, 2048:2048 + nkb * 128],
                          writes=[Bkn[s]])
                    P.dma("sp", vbh[s][:, 0:nkb, :], vbr[:, 0:nkb, h * 128:(h + 1) * 128], writes=[Bkn[s]])
                    P.dma("sp", vbh[s][:, 16:16 + nkb, :], vbr[:, 16:16 + nkb, h * 128:(h + 1) * 128], writes=[Bkn[s]])
                    P.dma("sp", qn[s][:], qnT[h * 128:(h + 1) * 128, i * 512:(i + 1) * 512], writes=[Bq[s]])
                    P.dma("sp", qr[s][:], qrT[h * 64:(h + 1) * 64, i * 512:(i + 1) * 512], writes=[Bq[s]])
                    ob = 4 + (h % 2)
                    db = 6 + (h % 2)
                    def emit_QK(kbi, s=s, kbl=kbl):
                        sbk = kbi % 2
                        kb = kbl[kbi]
                        P.pe(lambda e, sbk=sbk, kb=kb, s=s: e.matmul(banks[sbk][:], lhsT=kn[s][:, kb * 128:(kb + 1) * 128],
                                                                     rhs=qn[s][:], start=True, stop=False),
                             reads=[Bkn[s], Bq[s]], writes=[BK[sbk]])
                        P.pe(lambda e, sbk=sbk, kb=kb, s=s: e.matmul(banks[sbk][:], lhsT=krs[:, kb * 128:(kb + 1) * 128],
                                                                     rhs=qr[s][:], start=False, stop=True),
                             reads=[Bk, Bq[s]], writes=[BK[sbk]])
                    emit_QK(0)
                    for kbi, kb in enumerate(kbl):
                        sbk = kbi % 2
                        if kbi + 1 < len(kbl):
                            emit_QK(kbi + 1)
                        pp = pi_ % 3
                        pi_ += 1
                        p_ = Pt[pp]
                        P.act(lambda e, p_=p_, sbk=sbk: e.activation(out=p_[:], in_=banks[sbk][:], func=AF.Exp),
                              reads=[BK[sbk]], writes=[BP[pp]])
                        if kb in band:
                            bi_ = band.index(kb)
                            P.dve(lambda e, p_=p_, bi_=bi_: e.tensor_tensor(out=p_[:], in0=p_[:], in1=cm[:, bi_, :],
                                                                            op=ALU.mult),
                                  reads=[BP[pp], Bcm], writes=[BP[pp]])
                        P.pe(lambda e, p_=p_, kb=kb, kbi=kbi, s=s, ob=ob, nl=len(kbl): e.matmul(
                            banks[ob][:], lhsT=vbh[s][:, kb, :], rhs=p_[:], start=(kbi == 0),
                            stop=(kbi == nl - 1)), reads=[Bkn[s], BP[pp]], writes=[BK[ob]])
                        P.pe(lambda e, p_=p_, kbi=kbi, db=db, nl=len(kbl): e.matmul(
                            banks[db][:], lhsT=ones_bf[:], rhs=p_[:], start=(kbi == 0), stop=(kbi == nl - 1)),
                            reads=[B_const, BP[pp]], writes=[BK[db]])
                    P.dve(lambda e, db=db: e.reciprocal(out=rden[:], in_=banks[db][:]), reads=[BK[db]], writes=[Brd])
                    o_ = ostB[h % 2]
                    P.dve(lambda e, o_=o_, ob=ob: e.tensor_tensor(out=o_[:], in0=banks[ob][:], in1=rden[:], op=ALU.mult),
                          reads=[BK[ob], Brd], writes=[BoB[h % 2]])
                    P.dma("sp", mixT[(16 + h) * 128:(17 + h) * 128, i * 512:(i + 1) * 512], o_[:], reads=[BoB[h % 2]])
            P.end_stage(touch)

        with ExitStack() as st:
            def sb(name, shape, dt):
                return st.enter_context(sbt(name, list(shape), dt))

            ws = WStream(st)
            xch = [sb("xch%d" % i, [128, 512], F32) for i in range(2)]
            Bxc = [Buf() for _ in range(2)]
            sq = [sb("sq%d" % i, [128, 512], BF16) for i in range(2)]
            Bsq = [Buf() for _ in range(2)]
            ci = [0]

            def wo_epi(ec):
                def epi(tt, b):
                    s = ci[0] % 2
                    ci[0] += 1
                    P.dma("sp", xch[s][:], xT[ec * 128:(ec + 1) * 128, tt * 512:(tt + 1) * 512], writes=[Bxc[s]])
                    P.dve(lambda e, s=s, b=b: e.scalar_tensor_tensor(
                        out=xch[s][:], in0=banks[b][:], scalar=gate1[:, ec:ec + 1], in1=xch[s][:], op0=ALU.mult,
                        op1=ALU.add), reads=[BK[b], B_mod, Bxc[s]], writes=[Bxc[s]])
                    P.dma("sp", x2T[ec * 128:(ec + 1) * 128, tt * 512:(tt + 1) * 512], xch[s][:], reads=[Bxc[s]])
                    P.act(lambda e, s=s: e.activation(out=sq[s][:], in_=xch[s][:], func=AF.Square), reads=[Bxc[s]],
                          writes=[Bsq[s]])
                    sbk_ = 4 + (tt % 2)
                    P.pe(lambda e, s=s, sbk_=sbk_: e.matmul(banks[sbk_][:], lhsT=ones_bf[:], rhs=sq[s][:], start=(ec == 0),
                                                            stop=(ec == 31)), reads=[B_const, Bsq[s]], writes=[BK[sbk_]])
                    if ec == 31:
                        rsqrt_ops(r2b[:, tt * 512:(tt + 1) * 512], banks[sbk_][:], 1.0 / D, [BK[sbk_]], [B_r2b])
                return epi

            jobs = [fm_job_pair(ws, w_o, ec * 128, 128, wo_epi(ec)) for ec in range(32)]
            run_gemm_pair(st, mixT, 32, 4, jobs)
            P.end_stage(touch)

        with ExitStack() as st:
            def sb(name, shape, dt):
                return st.enter_context(sbt(name, list(shape), dt))

            xch = [sb("xch%d" % i, [128, 512], F32) for i in range(3)]
            Bxc = [Buf() for _ in range(3)]
            hch = [sb("hch%d" % i, [128, 512], BF16) for i in range(3)]
            Bhc = [Buf() for _ in range(3)]
            n = 0
            for tt in range(4):
                for k in range(32):
                    s = n % 3
                    n += 1
                    P.dma("sp", xch[s][:], x2T[k * 128:(k + 1) * 128, tt * 512:(tt + 1) * 512], writes=[Bxc[s]])
                    P.dve(lambda e, s=s, k=k, tt=tt: e.scalar_tensor_tensor(
                        out=xch[s][:], in0=xch[s][:], scalar=gs2[:, k:k + 1], in1=r2b[:, tt * 512:(tt + 1) * 512],
                        op0=ALU.mult, op1=ALU.mult), reads=[Bxc[s], B_mod, B_r2b], writes=[Bxc[s]])
                    P.act(lambda e, s=s, k=k: e.activation(out=hch[s][:], in_=xch[s][:], func=AF.Identity,
                                                           bias=sh2[:, k:k + 1], scale=1.0),
                          reads=[Bxc[s], B_mod], writes=[Bhc[s]])
                    P.dma("sp", h2T[k * 128:(k + 1) * 128, tt * 512:(tt + 1) * 512], hch[s][:], reads=[Bhc[s]])
            P.end_stage(touch)

        with ExitStack() as st:
            def sb(name, shape, dt):
                return st.enter_context(sbt(name, list(shape), dt))

            ws = WStream(st)
            u = [sb("u%d" % i, [128, 512], BF16) for i in range(3)]
            Bu = [Buf() for _ in range(3)]
            ci = [0]

            def m1_epi(fc):
                def epi(tt, b):
                    s = ci[0] % 3
                    ci[0] += 1
                    P.act(lambda e, s=s, b=b: e.activation(out=u[s][:], in_=banks[b][:], func=AF.Relu),
                          reads=[BK[b]], writes=[Bu[s]])
                    P.dve(lambda e, s=s: e.tensor_tensor(out=u[s][:], in0=u[s][:], in1=u[s][:], op=ALU.mult),
                          reads=[Bu[s]], writes=[Bu[s]])
                    P.dma("sp", hidT[fc * 128:(fc + 1) * 128, tt * 512:(tt + 1) * 512], u[s][:], reads=[Bu[s]])
                return epi

            jobs = [fm_job_pair(ws, w1, fc * 128, 128, m1_epi(fc)) for fc in range(128)]
            run_gemm_pair(st, h2T, 32, 4, jobs)
            P.end_stage(touch)

        with ExitStack() as st:
            def sb(name, shape, dt):
                return st.enter_context(sbt(name, list(shape), dt))

            ws = WStream(st)
            xch = [sb("xch%d" % i, [128, 512], F32) for i in range(2)]
            Bxc = [Buf() for _ in range(2)]
            sq = [sb("sq%d" % i, [128, 512], BF16) for i in range(2)]
            Bsq = [Buf() for _ in range(2)]
            sscol = sb("sscol", [128, 16], F32)
            Bss = Buf()
            P.pool(lambda e: e.memset(sscol[:], 0.0), writes=[Bss])
            ci = [0]

            def m2_epi(ec):
                def epi(tt, b):
                    s = ci[0] % 2
                    ci[0] += 1
                    P.dma("sp", xch[s][:], x2T[ec * 128:(ec + 1) * 128, tt * 512:(tt + 1) * 512], writes=[Bxc[s]])
                    P.dve(lambda e, s=s, b=b: e.scalar_tensor_tensor(
                        out=xch[s][:], in0=banks[b][:], scalar=gate2[:, ec:ec + 1], in1=xch[s][:], op0=ALU.mult,
                        op1=ALU.add), reads=[BK[b], B_mod, Bxc[s]], writes=[Bxc[s]])
                    P.act(lambda e, s=s: e.activation(out=sq[s][:], in_=xch[s][:], func=AF.Square), reads=[Bxc[s]],
                          writes=[Bsq[s]])
                    for tb in range(4):
                        P.pe(lambda e, s=s, tb=tb: e.matmul(banks[4][:, tb:tb + 1], lhsT=sq[s][:, tb * 128:(tb + 1) * 128],
                                                            rhs=ones_bf[:, 0:1], start=True, stop=True),
                             reads=[B_const, Bsq[s]], writes=[BK[4]])
                    P.dve(lambda e, tt=tt: e.tensor_tensor(out=sscol[:, tt * 4:tt * 4 + 4], in0=sscol[:, tt * 4:tt * 4 + 4],
                                                           in1=banks[4][:, 0:4], op=ALU.add),
                          reads=[BK[4], Bss], writes=[Bss])
                    P.dve(lambda e, s=s: e.tensor_scalar(out=xch[s][:], in0=xch[s][:], scalar1=fg[:, ec:ec + 1],
                                                         scalar2=None, op0=ALU.mult),
                          reads=[Bxc[s], Bsq[s], B_const], writes=[Bxc[s]])
                    P.dma("sp", x3T[ec * 128:(ec + 1) * 128, tt * 512:(tt + 1) * 512], xch[s][:], reads=[Bxc[s]])
                    if ec == 31 and tt == 3:
                        rsqrt_ops(rstd3[:], sscol[:], 1.0 / D, [Bss], [B_r3])
                return epi

            jobs = [fm_job(ws, w2, 128, ec * 128, 128, m2_epi(ec)) for ec in range(32)]
            run_gemm(st, hidT, 128, 4, jobs, xbufs=1)
            P.end_stage(touch)

        with ExitStack() as st:
            def sb(name, shape, dt):
                return st.enter_context(sbt(name, list(shape), dt))

            x3s = [sb("x3s%d" % i, [128, 32, 128], F32) for i in range(2)]
            Bx3 = [Buf() for _ in range(2)]
            yst = [sb("yst%d" % i, [128, 4096], F32) for i in range(2)]
            By = [Buf() for _ in range(2)]
            x3Tr = x3T.rearrange("(k p) t -> p k t", p=128)
            for blk in range(16):
                s = blk % 2
                P.dma("sp", x3s[s][:], x3Tr[:, :, blk * 128:(blk + 1) * 128], writes=[Bx3[s]])
                for k in range(32):
                    b = (k // 4) % 2
                    P.pe(lambda e, s=s, k=k, b=b: e.transpose(banks[b][:, (k % 4) * 128:(k % 4 + 1) * 128], x3s[s][:, k, :],
                                                              ident[:]), reads=[Bx3[s], B_const], writes=[BK[b]])
                    if k % 4 == 3:
                        P.act(lambda e, s=s, k=k, b=b, blk=blk: e.activation(
                            out=yst[s][:, (k - 3) * 128:(k + 1) * 128], in_=banks[b][:], func=AF.Identity,
                            scale=rstd3[:, blk:blk + 1]), reads=[BK[b], B_r3], writes=[By[s]])
                P.dma("sp", y[blk * 128:(blk + 1) * 128, :], yst[s][:], reads=[By[s]])
            P.end_stage(touch)
    return nc


_NC_CACHE = {}


def kernel(x, c, positions, w_ada, b_ada, ln1_g, w_in, q_norm_g, kv_norm_g, w_uq, w_uk, w_uv, w_o, ln2_g,
           w_mlp_in, w_mlp_out, final_g):
    x = np.asarray(x)
    positions = np.asarray(positions)
    B, S, _ = x.shape

    def colT(v, k):
        return np.ascontiguousarray(np.asarray(v, dtype=np.float32).reshape(k, 128).T)

    shared = {
        "w_ada": np.ascontiguousarray(np.asarray(w_ada)[0]), "b_adaT": colT(np.asarray(b_ada)[0], 192),
        "g1T": colT(np.asarray(ln1_g)[0], 32), "w_in": np.ascontiguousarray(np.asarray(w_in)[0]),
        "qgT": colT(np.asarray(q_norm_g)[0], 8), "kvgT": colT(np.asarray(kv_norm_g)[0], 4),
        "w_uq": np.ascontiguousarray(np.asarray(w_uq)[0]), "w_uk": np.ascontiguousarray(np.asarray(w_uk)[0]),
        "w_uv": np.ascontiguousarray(np.asarray(w_uv)[0]), "w_o": np.ascontiguousarray(np.asarray(w_o)[0]),
        "g2T": colT(np.asarray(ln2_g)[0], 32), "w1": np.ascontiguousarray(np.asarray(w_mlp_in)[0]),
        "w2": np.ascontiguousarray(np.asarray(w_mlp_out)[0]), "fgT": colT(np.asarray(final_g), 32),
    }
    in_maps = []
    perms = []
    for core in range(8):
        b, p = core // 2, core % 2
        own = [2 * j + p for j in range(16)]
        oth = [2 * j + 1 - p for j in range(16)]
        blocks = own + oth
        idx = np.concatenate([np.arange(g * 128, (g + 1) * 128) for g in blocks])
        perms.append((b, own))
        m = dict(shared)
        m["xc"] = np.ascontiguousarray(x[b][idx])
        pp = np.ascontiguousarray(positions[b][idx].astype(np.int32))
        m["posr"] = pp.reshape(1, 4096)
        m["posc"] = np.ascontiguousarray(pp.reshape(32, 128).T)
        m["cT"] = colT(np.asarray(c)[b], 32)
        in_maps.append(m)
    if "nc" not in _NC_CACHE:
        _NC_CACHE["nc"] = build_nc()
    res = run_bass_kernel_spmd(_NC_CACHE["nc"], in_maps, core_ids=list(range(8)))
    out = np.empty((B, S, D), dtype=np.float32)
    for core in range(8):
        b, own = perms[core]
        yv = np.asarray(res.results[core]["y"]).reshape(16, 128, D)
        for j, g in enumerate(own):
            out[b, g * 128:(g + 1) * 128, :] = yv[j]
    return out
```

```python
import math
import os
from contextlib import ExitStack
import numpy as np
import concourse.bass as bass
import concourse.mybir as mybir
from concourse.bass_utils import run_bass_kernel_spmd

F32 = mybir.dt.float32
BF16 = mybir.dt.bfloat16
I32 = mybir.dt.int32
AF = mybir.ActivationFunctionType
ALU = mybir.AluOpType
AX = mybir.AxisListType

ENGS = ("pe", "act", "dve", "pool", "sp")
SAME_ENGINE_SYNC = os.environ.get("KSES", "1") == "1"
D = 4096
EPS = 1e-6
NBIS = 20


import os
MAXSTAGE = int(os.environ.get("KSTAGE", "99"))
DBG_OUT = set(filter(None, os.environ.get("KDBG", "").split(",")))


class StopBuild(Exception):
    pass


class Buf:
    __slots__ = ("writers", "readers")

    def __init__(self):
        self.writers = []
        self.readers = []


class Op:
    __slots__ = ("eng", "fn", "deps", "is_dma", "seq", "signals", "dsem", "dtarget", "emitted", "touch")

    def __init__(self, eng, fn, is_dma):
        self.eng = eng
        self.fn = fn
        self.deps = []
        self.is_dma = is_dma
        self.seq = None
        self.signals = False
        self.dsem = None
        self.dtarget = None
        self.emitted = False
        self.touch = False


class Prog:
    def __init__(self, nc, stack, n_dma_sems=10):
        self.nc = nc
        self.pending = {e: [] for e in ENGS}
        self.stage_deps = {e: [] for e in ENGS}
        self.csem = {e: stack.enter_context(nc.semaphore("c_" + e)) for e in ENGS}
        self.dsems = {e: [stack.enter_context(nc.semaphore("d_%s_%d" % (e, i))) for i in range(n_dma_sems)]
                      for e in ("sp", "pool")}
        self.cnt = {e: 0 for e in ENGS}
        self.dma_k = {e: 0 for e in self.dsems}
        self.dma_uses = {e: [0] * n_dma_sems for e in self.dsems}
        self.waited = {e: {} for e in ENGS}
        self.stage_no = 0

    def add(self, eng, fn, reads=(), writes=(), dma=False, extra_deps=()):
        op = Op(eng, fn, dma)
        deps = []
        for b in reads:
            deps.extend(b.writers)
        for b in writes:
            deps.extend(b.writers)
            deps.extend(b.readers)
        deps.extend(extra_deps)
        if self.stage_deps[eng]:
            deps.extend(self.stage_deps[eng])
            self.stage_deps[eng] = []
        seen = set()
        for d in deps:
            if d is op or id(d) in seen or (d.emitted and not d.touch):
                continue
            seen.add(id(d))
            op.deps.append(d)
            if not d.is_dma and not (d.eng == eng and (eng == "pe" or not SAME_ENGINE_SYNC)):
                d.signals = True
        for b in reads:
            b.readers.append(op)
            if len(b.readers) > 48:
                b.readers = b.readers[-48:]
        for b in writes:
            if b.readers:
                b.readers = []
                b.writers = [op]
            else:
                b.writers.append(op)
                if len(b.writers) > 48:
                    b.writers = b.writers[-48:]
        self.pending[eng].append(op)
        return op

    def pe(self, fn, reads=(), writes=(), **kw):
        return self.add("pe", fn, reads, writes, **kw)

    def act(self, fn, reads=(), writes=(), **kw):
        return self.add("act", fn, reads, writes, **kw)

    def dve(self, fn, reads=(), writes=(), **kw):
        return self.add("dve", fn, reads, writes, **kw)

    def pool(self, fn, reads=(), writes=(), **kw):
        return self.add("pool", fn, reads, writes, **kw)

    def dma(self, q, out, in_, reads=(), writes=(), slow=False, **kw):
        if slow:
            fn = lambda e: e.dma_start(out=out, in_=in_, allow_slow_non_contiguous=True)
        else:
            fn = lambda e: e.dma_start(out=out, in_=in_)
        return self.add(q, fn, reads, writes, dma=True, **kw)

    def end_stage(self, touch):
        nc = self.nc
        tops = []
        for e in ("act", "dve", "pool", "sp"):
            t = Op(e, touch[e], e == "sp")
            t.signals = True
            t.touch = True
            self.pending[e].append(("T", t))
            tops.append(t)
        for e in ENGS:
            for item in self.pending[e]:
                op = item[1] if isinstance(item, tuple) else item
                if op.is_dma:
                    i = self.dma_k[e] % len(self.dsems[e])
                    self.dma_k[e] += 1
                    self.dma_uses[e][i] += 1
                    op.dsem = (e, i)
                    op.dtarget = 16 * self.dma_uses[e][i]
                elif op.signals:
                    self.cnt[e] += 1
                    op.seq = self.cnt[e]
        with nc.Block() as block:
            engmap = {"pe": block.tensor, "act": block.scalar, "dve": block.vector, "pool": block.gpsimd,
                      "sp": block.sync}

            def make(ename):
                items = self.pending[ename]
                waited = self.waited[ename]

                def body(eng):
                    def wait(key, sem, val):
                        if waited.get(key, 0) >= val:
                            return
                        waited[key] = val
                        eng.wait_ge(sem, val)

                    def wait_all_dma():
                        if ename in self.dsems:
                            for i, s in enumerate(self.dsems[ename]):
                                tot = 16 * self.dma_uses[ename][i]
                                if tot:
                                    wait(("d", ename, i), s, tot)

                    for item in items:
                        if isinstance(item, tuple):
                            op = item[1]
                            if ename in self.dsems:
                                for i, s in enumerate(self.dsems[ename]):
                                    tot = 16 * self.dma_uses[ename][i]
                                    if op.is_dma and op.dsem == (ename, i):
                                        tot -= 16
                                    if tot:
                                        wait(("d", ename, i), s, tot)
                        else:
                            op = item
                        if op.is_dma and op.dtarget > 16:
                            wait(("d",) + op.dsem, self.dsems[op.dsem[0]][op.dsem[1]], op.dtarget - 16)
                        for d in op.deps:
                            if d.is_dma:
                                wait(("d",) + d.dsem, self.dsems[d.dsem[0]][d.dsem[1]], d.dtarget)
                            else:
                                if d.eng == ename and (ename == "pe" or not SAME_ENGINE_SYNC):
                                    continue
                                wait(("c", d.eng), self.csem[d.eng], d.seq)
                        ins = op.fn(eng)
                        if op.is_dma:
                            ins.then_inc(self.dsems[op.dsem[0]][op.dsem[1]], 16)
                        elif op.signals:
                            ins.then_inc(self.csem[ename], 1)
                    wait_all_dma()

                return body

            for e in ENGS:
                if self.pending[e]:
                    engmap[e](make(e))
        for e in ENGS:
            for item in self.pending[e]:
                (item[1] if isinstance(item, tuple) else item).emitted = True
        self.pending = {e: [] for e in ENGS}
        for e in ENGS:
            self.stage_deps[e] = list(tops)
        self.stage_no += 1
        if self.stage_no > MAXSTAGE:
            raise StopBuild()


def build_nc():
    nc = bass.Bass("TRN2", target_bir_lowering=False)

    def inp(name, shape, dt=F32):
        return nc.dram_tensor(name, list(shape), dt, kind="ExternalInput").ap()

    _uc = [0]

    def sbt(name, shape, dt):
        _uc[0] += 1
        return nc.sbuf_tensor("%s_u%d" % (name, _uc[0]), shape, dt)

    def scr(name, shape, dt):
        return nc.dram_tensor(name, list(shape), dt, kind=("ExternalOutput" if name in DBG_OUT else "Internal")).ap()

    xc = inp("xc", [4096, 4096])
    posr = inp("posr", [1, 4096], I32)
    posc = inp("posc", [128, 32], I32)
    cT = inp("cT", [128, 32])
    w_ada = inp("w_ada", [4096, 24576])
    b_adaT = inp("b_adaT", [128, 192])
    g1T = inp("g1T", [128, 32])
    w_in = inp("w_in", [4096, 8160])
    qgT = inp("qgT", [128, 8])
    kvgT = inp("kvgT", [128, 4])
    w_uq = inp("w_uq", [1024, 3072])
    w_uk = inp("w_uk", [512, 2048])
    w_uv = inp("w_uv", [512, 2048])
    w_o = inp("w_o", [4096, 4096])
    g2T = inp("g2T", [128, 32])
    w1 = inp("w1", [4096, 16384])
    w2 = inp("w2", [16384, 4096])
    fgT = inp("fgT", [128, 32])
    y = nc.dram_tensor("y", [2048, 4096], F32, kind="ExternalOutput").ap()

    h1T = scr("h1T", [4096, 4096], BF16)
    xT = scr("xT", [4096, 2048], F32)
    kaT = scr("kaT", [128, 4096], BF16)
    va = scr("va", [4096, 128], BF16)
    kiT = scr("kiT", [128, 4096], BF16)
    krT = scr("krT", [64, 4096], BF16)
    kvnT = scr("kvnT", [512, 4096], BF16)
    knT = scr("knT", [2048, 4096], BF16)
    vb = scr("vb", [4096, 2048], BF16)
    qaT = scr("qaT", [2048, 2048], BF16)
    qiT = scr("qiT", [4096, 2048], BF16)
    wi3 = scr("wi3", [16 * 32 * 128], F32)
    cqnT = scr("cqnT", [1024, 2048], BF16)
    qnT = scr("qnT", [2048, 2048], BF16)
    qrT = scr("qrT", [16 * 64, 2048], BF16)
    mixT = scr("mixT", [4096, 2048], BF16)
    x2T = scr("x2T", [4096, 2048], F32)
    h2T = scr("h2T", [4096, 2048], BF16)
    hidT = scr("hidT", [16384, 2048], BF16)
    x3T = scr("x3T", [4096, 2048], F32)
    tdr = scr("tdr", [1, 64], F32)

    try:
        _build_body(nc, locals())
    except StopBuild:
        pass
    return nc


def _build_body(nc, L):
    globals().update({k: v for k, v in L.items() if k not in ("nc",)})
    with ExitStack() as gst:
        P = Prog(nc, gst)

        def gsb(name, shape, dt):
            return gst.enter_context(sbt(name, list(shape), dt))

        ident = gsb("ident", [128, 128], F32)
        ones_bf = gsb("ones_bf", [128, 128], BF16)
        modT = gsb("modT", [128, 192], F32)
        gs1 = gsb("gs1", [128, 32], F32)
        gs2 = gsb("gs2", [128, 32], F32)
        fg = gsb("fg", [128, 32], F32)
        kvg = gsb("kvg", [128, 4], F32)
        qg = gsb("qg", [128, 8], F32)
        posf = gsb("posf", [128, 32], F32)
        chkf = gsb("chkf", [128, 32], F32)
        cosT = gsb("cosT", [128, 32, 32], F32)
        sinT = gsb("sinT", [128, 32, 32], F32)
        r2b = gsb("r2b", [128, 2048], F32)
        rstd3 = gsb("rstd3", [128, 16], F32)
        tch = gsb("tch", [128, 8], F32)
        B_const = Buf()
        B_mod = Buf()
        B_r2b = Buf()
        B_r3 = Buf()
        banks = [gst.enter_context(nc.psum_tensor("bank%d" % i, [128, 512], F32)) for i in range(8)]
        BK = [Buf() for _ in range(8)]
        sh1 = modT[:, 0:32]
        gate1 = modT[:, 64:96]
        sh2 = modT[:, 96:128]
        gate2 = modT[:, 160:192]

        touch = {
            "act": lambda e: e.activation(out=tch[0:1, 0:1], in_=tch[0:1, 1:2], func=AF.Copy),
            "dve": lambda e: e.tensor_copy(out=tch[0:1, 2:3], in_=tch[0:1, 3:4]),
            "pool": lambda e: e.memset(tch[0:1, 4:5], 0.0),
            "sp": lambda e: e.dma_start(out=tdr[0:1, 0:2], in_=tch[0:1, 6:8]),
        }

        def rsqrt_ops(dst, src, scale, reads, writes):
            P.dve(lambda e: e.tensor_scalar(out=dst, in0=src, scalar1=scale, scalar2=EPS, op0=ALU.mult, op1=ALU.add),
                  reads=reads, writes=writes)
            P.act(lambda e: e.activation(out=dst, in_=dst, func=AF.Sqrt), reads=writes, writes=writes)
            P.dve(lambda e: e.reciprocal(out=dst, in_=dst), reads=writes, writes=writes)

        with ExitStack() as st:
            def sb(name, shape, dt):
                return st.enter_context(sbt(name, list(shape), dt))

            io_i = sb("io_i", [128, 128], I32)
            P.pool(lambda e: e.iota(io_i[:], pattern=[[1, 128]], base=0, channel_multiplier=-1), writes=[B_const])
            P.dve(lambda e: e.tensor_copy(out=ident[:], in_=io_i[:]), reads=[B_const], writes=[B_const])
            P.dve(lambda e: e.tensor_scalar(out=ident[:], in0=ident[:], scalar1=0.0, scalar2=None, op0=ALU.is_equal),
                  reads=[B_const], writes=[B_const])
            P.pool(lambda e: e.memset(ones_bf[:], 1.0), writes=[B_const])
            P.pool(lambda e: e.memset(tch[:], 0.0), writes=[B_const])
            Bp = Buf()
            ct_sb = sb("ct_sb", [128, 32], F32)
            badaT = sb("badaT", [128, 192], F32)
            g1s = sb("g1s", [128, 32], F32)
            g2s = sb("g2s", [128, 32], F32)
            posc_i = sb("posc_i", [128, 32], I32)
            chk_i = sb("chk_i", [128, 32], I32)
            for (dst, src) in ((ct_sb, cT), (badaT, b_adaT), (g1s, g1T), (g2s, g2T), (fg, fgT), (kvg, kvgT),
                               (qg, qgT), (posc_i, posc)):
                P.dma("sp", dst[:], src, writes=[Bp])
            P.dve(lambda e: e.tensor_copy(out=posf[:], in_=posc_i[:]), reads=[Bp], writes=[B_const])
            P.dve(lambda e: e.tensor_scalar(out=chk_i[:], in0=posc_i[:], scalar1=6, scalar2=None,
                                            op0=ALU.arith_shift_right), reads=[Bp], writes=[Bp])
            P.dve(lambda e: e.tensor_copy(out=chkf[:], in_=chk_i[:]), reads=[Bp], writes=[B_const])
            inv_i = sb("inv_i", [128, 32], I32)
            invf = sb("invf", [128, 32], F32)
            for i_ in range(32):
                P.pool(lambda e, i_=i_: e.memset(invf[:, i_:i_ + 1], float(np.float32(10000.0) ** np.float32(-2.0 * i_ / 64.0))),
                       writes=[Bp])
            rr = sb("rr", [128, 32, 32], F32)
            ri2 = [sb("ri%d" % i_, [128, 32, 32], I32) for i_ in range(2)]
            rf2 = [sb("rf%d" % i_, [128, 32, 32], F32) for i_ in range(2)]
            Br = Buf()
            for blk in range(32):
                P.dve(lambda e, blk=blk: e.tensor_scalar(out=rr[:, blk, :], in0=invf[:], scalar1=posf[:, blk:blk + 1],
                                                         scalar2=1.0 / (2 * math.pi), op0=ALU.mult, op1=ALU.mult),
                      reads=[Bp, B_const], writes=[Br])
            Bt = Buf()
            for (tab, off) in ((cosT, 0.25), (sinT, 0.0)):
                rrf = rr[:].rearrange("p a b -> p (a b)")
                rif = ri2[0 if off else 1][:].rearrange("p a b -> p (a b)")
                rff = rf2[0 if off else 1][:].rearrange("p a b -> p (a b)")
                tabf = tab[:].rearrange("p a b -> p (a b)")
                P.dve(lambda e, off=off, rff=rff, rrf=rrf: e.tensor_scalar(out=rff, in0=rrf, scalar1=off, scalar2=None,
                                                                           op0=ALU.add), reads=[Br], writes=[Bt])
                P.dve(lambda e, rff=rff, rif=rif: e.tensor_copy(out=rif, in_=rff), reads=[Bt], writes=[Bt])
                P.dve(lambda e, tabf=tabf, rif=rif: e.tensor_copy(out=tabf, in_=rif), reads=[Bt], writes=[B_const])
                P.dve(lambda e, rff=rff, tabf=tabf: e.tensor_tensor(out=rff, in0=rff, in1=tabf, op=ALU.subtract),
                      reads=[Bt, B_const], writes=[Bt])
                P.dve(lambda e, rff=rff, tabf=tabf: e.tensor_scalar(out=tabf, in0=rff, scalar1=0.5, scalar2=None,
                                                                    op0=ALU.is_gt), reads=[Bt], writes=[B_const])
                P.dve(lambda e, rff=rff, tabf=tabf: e.tensor_tensor(out=rff, in0=rff, in1=tabf, op=ALU.subtract),
                      reads=[Bt, B_const], writes=[Bt])
                P.dve(lambda e, rff=rff, tabf=tabf: e.tensor_scalar(out=tabf, in0=rff, scalar1=-0.5, scalar2=None,
                                                                    op0=ALU.is_lt), reads=[Bt], writes=[B_const])
                P.dve(lambda e, rff=rff, tabf=tabf: e.tensor_tensor(out=rff, in0=rff, in1=tabf, op=ALU.add),
                      reads=[Bt, B_const], writes=[Bt])
                P.act(lambda e, rff=rff, tabf=tabf: e.activation(out=tabf, in_=rff, func=AF.Sin, scale=2 * math.pi),
                      reads=[Bt], writes=[B_const])
            if "dbgtab" in DBG_OUT:
                dbgtab = scr("dbgtab", [128, 3072], F32)
                P.dma("sp", dbgtab[:, 0:1024], cosT[:].rearrange("p a b -> p (a b)"), reads=[B_const, Bt])
                P.dma("sp", dbgtab[:, 1024:2048], sinT[:].rearrange("p a b -> p (a b)"), reads=[B_const, Bt])
                P.dma("sp", dbgtab[:, 2048:3072], rr[:].rearrange("p a b -> p (a b)"), reads=[Br, Bt])
            scT = sb("scT", [128, 32], BF16)
            P.act(lambda e: e.activation(out=scT[:], in_=ct_sb[:], func=AF.Silu), reads=[Bp], writes=[Bp])
            wts = [sb("wa%d" % i, [128, 32, 128], BF16) for i in range(3)]
            Bw = [Buf() for _ in range(3)]
            war = w_ada.rearrange("(k p) e -> p k e", p=128)
            for ec in range(192):
                s = ec % 3
                P.dma("pool", wts[s][:], war[:, :, ec * 128:(ec + 1) * 128], writes=[Bw[s]])
                for k in range(32):
                    P.pe(lambda e, s=s, k=k, ec=ec: e.matmul(banks[0][:, ec:ec + 1], lhsT=wts[s][:, k, :],
                                                             rhs=scT[:, k:k + 1], start=(k == 0), stop=(k == 31)),
                         reads=[Bw[s], Bp], writes=[BK[0]])
            P.dve(lambda e: e.tensor_tensor(out=modT[:], in0=banks[0][:, 0:192], in1=badaT[:], op=ALU.add),
                  reads=[BK[0], Bp], writes=[B_mod])
            P.dve(lambda e: e.scalar_tensor_tensor(out=gs1[:], in0=modT[:, 32:64], scalar=1.0, in1=g1s[:],
                                                   op0=ALU.add, op1=ALU.mult), reads=[B_mod, Bp], writes=[B_mod])
            P.dve(lambda e: e.scalar_tensor_tensor(out=gs2[:], in0=modT[:, 128:160], scalar=1.0, in1=g2s[:],
                                                   op0=ALU.add, op1=ALU.mult), reads=[B_mod, Bp], writes=[B_mod])
            P.end_stage(touch)

        with ExitStack() as st:
            def sb(name, shape, dt):
                return st.enter_context(sbt(name, list(shape), dt))

            xb = [sb("xb%d" % i, [128, 4096], F32) for i in range(2)]
            Bx = [Buf() for _ in range(2)]
            junk = sb("junk", [128, 4096], BF16)
            Bj = Buf()
            hst = [sb("hst%d" % i, [128, 32, 512], BF16) for i in range(2)]
            Bh = [Buf() for _ in range(2)]
            xst = [sb("xst%d" % i, [128, 32, 128], F32) for i in range(2)]
            Bxs = [Buf() for _ in range(2)]
            ssq = sb("ssq", [128, 32], F32)
            Bs = Buf()
            h1Tr = h1T.rearrange("(k p) t -> p k t", p=128)
            xTr = xT.rearrange("(k p) t -> p k t", p=128)
            bi = 0
            for blk in range(32):
                s = blk % 2
                x_ = xb[s]
                P.dma("sp", x_[:], xc[blk * 128:(blk + 1) * 128, :], writes=[Bx[s]])
                if blk < 16:
                    xs = xst[blk % 2]
                    for k in range(32):
                        b = bi % 2
                        bi_k = k % 4
                        P.pe(lambda e, x_=x_, k=k, b=b, bi_k=bi_k: e.transpose(
                            banks[b][:, bi_k * 128:(bi_k + 1) * 128], x_[:, k * 128:(k + 1) * 128], ident[:]),
                            reads=[Bx[s], B_const], writes=[BK[b]])
                        if bi_k == 3:
                            P.dve(lambda e, xs=xs, k=k, b=b: e.tensor_copy(
                                out=xs[:, k - 3:k + 1, :], in_=banks[b][:].rearrange("p (a t) -> p a t", a=4)),
                                reads=[BK[b]], writes=[Bxs[blk % 2]])
                            bi += 1
                    P.dma("sp", xTr[:, :, blk * 128:(blk + 1) * 128], xs[:], reads=[Bxs[blk % 2]])
                P.act(lambda e, x_=x_, blk=blk: e.activation(out=junk[:], in_=x_[:], func=AF.Square,
                                                             accum_out=ssq[:, blk:blk + 1]),
                      reads=[Bx[s]], writes=[Bj, Bs])
                rsqrt_ops(ssq[:, blk:blk + 1], ssq[:, blk:blk + 1], 1.0 / D, [Bs], [Bs])
                P.dve(lambda e, x_=x_, blk=blk: e.tensor_scalar(out=x_[:], in0=x_[:], scalar1=ssq[:, blk:blk + 1],
                                                                scalar2=None, op0=ALU.mult),
                      reads=[Bx[s], Bs], writes=[Bx[s]])
                hs = hst[(blk // 4) % 2]
                Bhs = Bh[(blk // 4) % 2]
                tb = blk % 4
                for k in range(32):
                    b = 2 + (bi % 2)
                    bi_k = k % 4
                    P.pe(lambda e, x_=x_, k=k, b=b, bi_k=bi_k: e.transpose(
                        banks[b][:, bi_k * 128:(bi_k + 1) * 128], x_[:, k * 128:(k + 1) * 128], ident[:]),
                        reads=[Bx[s], B_const], writes=[BK[b]])
                    if bi_k == 3:
                        for kk in range(k - 3, k + 1):
                            P.act(lambda e, hs=hs, kk=kk, b=b, tb=tb: e.activation(
                                out=hs[:, kk, tb * 128:(tb + 1) * 128], in_=banks[b][:, (kk % 4) * 128:(kk % 4 + 1) * 128],
                                func=AF.Identity, scale=gs1[:, kk:kk + 1], bias=sh1[:, kk:kk + 1]),
                                reads=[BK[b], B_mod], writes=[Bhs])
                        bi += 1
                if tb == 3:
                    t0 = (blk // 4) * 512
                    P.dma("sp", h1Tr[:, :, t0:t0 + 512], hs[:], reads=[Bhs])
            P.end_stage(touch)

        def run_gemm(st, XT, KC, n_tt, jobs, xbufs=1, pre_tt=None):
            xts = [st.enter_context(sbt("xt%d" % i, [128, KC, 512], BF16)) for i in range(xbufs)]
            Bxt = [Buf() for _ in range(xbufs)]
            XTr = XT.rearrange("(k p) t -> p k t", p=128)
            for tt in range(n_tt):
                s = tt % xbufs
                for k0 in range(0, KC, 32):
                    k1 = min(KC, k0 + 32)
                    P.dma("sp", xts[s][:, k0:k1, :], XTr[:, k0:k1, tt * 512:(tt + 1) * 512], writes=[Bxt[s]])
                if pre_tt is not None:
                    pre_tt(tt)
                for job in jobs:
                    job(xts[s], Bxt[s], tt)

        class WStream:
            def __init__(self, st, n=3):
                self.t = [st.enter_context(sbt("ws%d" % i, [128, 32, 128], BF16)) for i in range(n)]
                self.B = [Buf() for _ in range(n)]
                self.i = 0

            def load(self, W, r0, nk, c0, ncol):
                s = self.i % len(self.t)
                self.i += 1
                src = W[r0:r0 + nk * 128, c0:c0 + ncol].rearrange("(k p) e -> p k e", p=128)
                P.dma("pool", self.t[s][:, 0:nk, 0:ncol], src, writes=[self.B[s]])
                return self.t[s], self.B[s]

        gb = [0]

        def gbank():
            gb[0] += 1
            return gb[0] % 2

        def fm_job(ws, W, KC, c0, ncol, epi):
            def job(xt, Bxt, tt):
                b = gbank()
                for k0 in range(0, KC, 32):
                    nk = min(32, KC - k0)
                    wt, Bw = ws.load(W, k0 * 128, nk, c0, ncol)
                    for k in range(nk):
                        P.pe(lambda e, wt=wt, k=k, k0=k0, b=b: e.matmul(
                            banks[b][0:ncol, :], lhsT=wt[:, k, 0:ncol], rhs=xt[:, k0 + k, :],
                            start=(k0 + k == 0), stop=(k0 + k == KC - 1)), reads=[Bw, Bxt], writes=[BK[b]])
                epi(tt, b)
            return job

        def run_gemm_pair(st, XT, KC, n_tt, jobs):
            xts = [st.enter_context(sbt("xtp%d" % i, [128, KC, 512], BF16)) for i in range(2)]
            Bxt = [Buf() for _ in range(2)]
            XTr = XT.rearrange("(k p) t -> p k t", p=128)
            for tp_ in range(n_tt // 2):
                for s in range(2):
                    tt = 2 * tp_ + s
                    P.dma("sp", xts[s][:, 0:KC, :], XTr[:, 0:KC, tt * 512:(tt + 1) * 512], writes=[Bxt[s]])
                for job in jobs:
                    job(xts, Bxt, 2 * tp_)

        pbk = [0]

        def fm_job_pair(ws, W, c0, ncol, epi):
            def job(xts, Bxt, tt0):
                pbk[0] ^= 1
                bb = (0, 1) if pbk[0] else (2, 3)
                wt, Bw = ws.load(W, 0, 32, c0, ncol)
                for k in range(32):
                    for s in range(2):
                        b = bb[s]
                        P.pe(lambda e, wt=wt, k=k, b=b, s=s: e.matmul(
                            banks[b][0:ncol, :], lhsT=wt[:, k, 0:ncol], rhs=xts[s][:, k, :],
                            start=(k == 0), stop=(k == 31)), reads=[Bw, Bxt[s]], writes=[BK[b]])
                epi(tt0, bb[0])
                epi(tt0 + 1, bb[1])
            return job

        with ExitStack() as st:
            def sb(name, shape, dt):
                return st.enter_context(sbt(name, list(shape), dt))

            ws = WStream(st)
            ost = [sb("ost%d" % i, [128, 512], BF16) for i in range(3)]
            Bo = [Buf() for _ in range(3)]
            oi = [0]

            def copy_out(dst_fn, scale=1.0):
                def epi(tt, b):
                    s = oi[0] % 3
                    oi[0] += 1
                    P.act(lambda e, s=s, b=b: e.activation(out=ost[s][:], in_=banks[b][:], func=AF.Copy, scale=scale),
                          reads=[BK[b]], writes=[Bo[s]])
                    P.dma("sp", dst_fn(tt), ost[s][:], reads=[Bo[s]])
                return epi

            ckv = sb("ckv", [128, 4, 512], F32)
            sqb = sb("sqb", [128, 4, 512], BF16)
            rb = sb("rb", [128, 512], F32)
            kvn = sb("kvn", [128, 4, 512], BF16)
            Bc = Buf()
            Bsq = Buf()
            Brb = Buf()
            Bkvn = Buf()
            kvnTr = kvnT.rearrange("(k p) t -> p k t", p=128)

            def ckv_epi(j):
                def epi(tt, b):
                    P.dve(lambda e, b=b: e.tensor_copy(out=ckv[:, j, :], in_=banks[b][:]), reads=[BK[b]], writes=[Bc])
                    P.act(lambda e, b=b: e.activation(out=sqb[:, j, :], in_=ckv[:, j, :], func=AF.Square),
                          reads=[Bc], writes=[Bsq])
                    if j == 3:
                        for jj in range(4):
                            P.pe(lambda e, jj=jj: e.matmul(banks[4][:], lhsT=ones_bf[:], rhs=sqb[:, jj, :],
                                                           start=(jj == 0), stop=(jj == 3)),
                                 reads=[Bsq, B_const], writes=[BK[4]])
                        rsqrt_ops(rb[:], banks[4][:], 1.0 / 512, [BK[4]], [Brb])
                        for jj in range(4):
                            P.dve(lambda e, jj=jj: e.scalar_tensor_tensor(
                                out=kvn[:, jj, :], in0=ckv[:, jj, :], scalar=kvg[:, jj:jj + 1], in1=rb[:],
                                op0=ALU.mult, op1=ALU.mult), reads=[Bc, Brb, Bp0], writes=[Bkvn])
                        P.dma("sp", kvnTr[:, :, tt * 512:(tt + 1) * 512], kvn[:], reads=[Bkvn])
                return epi

            Bp0 = B_const
            wtm = sb("wtm", [128, 32, 192], BF16)
            Bwtm = Buf()
            w_in_r = w_in.rearrange("(k p) e -> p k e", p=128)
            P.dma("pool", wtm[:, :, 0:128], w_in_r[:, :, 2176:2304], writes=[Bwtm])
            P.dma("pool", wtm[:, :, 128:192], w_in_r[:, :, 8096:8160], writes=[Bwtm])
            vst = [sb("vst%d" % i, [128, 128], BF16) for i in range(2)]
            Bv = [Buf() for _ in range(2)]
            krf = sb("krf", [128, 64], F32)
            kro = sb("kro", [128, 64], F32)
            tmp1 = sb("tmp1", [128, 32], F32)
            Bkr = Buf()
            krst = sb("krst", [64, 512], BF16)
            Bkrst = Buf()

            def rope_tok(e_list, src, dst, blk, nh, tmp):
                c = cosT[:, blk, :].unsqueeze(1).to_broadcast([128, nh, 32])
                s_ = sinT[:, blk, :].unsqueeze(1).to_broadcast([128, nh, 32])
                x1 = src[:, :, 0:32]
                x2 = src[:, :, 32:64]
                ops = [
                    (dst[:, :, 0:32], x1, c, ALU.mult), (tmp, x2, s_, ALU.mult),
                    (dst[:, :, 0:32], dst[:, :, 0:32], tmp, ALU.subtract),
                    (dst[:, :, 32:64], x1, s_, ALU.mult), (tmp, x2, c, ALU.mult),
                    (dst[:, :, 32:64], dst[:, :, 32:64], tmp, ALU.add)]
                for (o, a, b_, op) in ops:
                    P.dve(lambda e, o=o, a=a, b_=b_, op=op: e.tensor_tensor(out=o, in0=a, in1=b_, op=op),
                          reads=e_list[0], writes=e_list[1])

            def tm_job_k(xt, Bxt, tt):
                for tb in range(4):
                    blk = tt * 4 + tb
                    b = 2 + (tb % 2)
                    for k in range(32):
                        P.pe(lambda e, k=k, b=b, tb=tb: e.matmul(banks[b][:, 0:192], lhsT=xt[:, k, tb * 128:(tb + 1) * 128],
                                                                 rhs=wtm[:, k, :], start=(k == 0), stop=(k == 31)),
                             reads=[Bxt, Bwtm], writes=[BK[b]])
                    s = blk % 2
                    P.act(lambda e, s=s, b=b: e.activation(out=vst[s][:], in_=banks[b][:, 0:128], func=AF.Copy),
                          reads=[BK[b]], writes=[Bv[s]])
                    P.dma("sp", va[blk * 128:(blk + 1) * 128, :], vst[s][:], reads=[Bv[s]])
                    P.act(lambda e, b=b: e.activation(out=krf[:], in_=banks[b][:, 128:192], func=AF.Copy), reads=[BK[b]],
                          writes=[Bkr])
                    rope_tok(([Bkr, B_const], [Bkr]), krf[:].unsqueeze(1), kro[:].unsqueeze(1), blk, 1,
                             tmp1[:].unsqueeze(1))
                    P.pe(lambda e: e.transpose(banks[5][0:64, 0:128], kro[:], ident[:]), reads=[Bkr, B_const],
                         writes=[BK[5]])
                    P.act(lambda e, tb=tb: e.activation(out=krst[:, tb * 128:(tb + 1) * 128], in_=banks[5][0:64, 0:128],
                                                        func=AF.Copy), reads=[BK[5]], writes=[Bkrst])
                P.dma("sp", krT[:, tt * 512:(tt + 1) * 512], krst[:], reads=[Bkrst])

            jobs = [fm_job(ws, w_in, 32, 2048, 128, copy_out(lambda tt: kaT[:, tt * 512:(tt + 1) * 512])),
                    fm_job(ws, w_in, 32, 6400, 128, copy_out(lambda tt: kiT[:, tt * 512:(tt + 1) * 512]))]
            for j in range(4):
                jobs.append(fm_job(ws, w_in, 32, 7584 + 128 * j, 128, ckv_epi(j)))
            jobs.append(tm_job_k)
            _sub = int(os.environ.get("KSUB", "255"))
            jobs = [jb for n_, jb in enumerate(jobs) if (_sub >> n_) & 1]
            run_gemm(st, h1T, 32, 8, jobs, xbufs=2)
            P.end_stage(touch)

        with ExitStack() as st:
            def sb(name, shape, dt):
                return st.enter_context(sbt(name, list(shape), dt))

            ws = WStream(st)
            ost = [sb("ost%d" % i, [128, 512], BF16) for i in range(3)]
            Bo = [Buf() for _ in range(3)]
            oi = [0]

            def kn_epi(h):
                def epi(tt, b):
                    s = oi[0] % 3
                    oi[0] += 1
                    P.act(lambda e, s=s, b=b: e.activation(out=ost[s][:], in_=banks[b][:], func=AF.Copy),
                          reads=[BK[b]], writes=[Bo[s]])
                    P.dma("sp", knT[h * 128:(h + 1) * 128, tt * 512:(tt + 1) * 512], ost[s][:], reads=[Bo[s]])
                return epi

            wuv = sb("wuv", [128, 4, 2048], BF16)
            Bwuv = Buf()
            P.dma("pool", wuv[:], w_uv.rearrange("(k p) e -> p k e", p=128), writes=[Bwuv])
            vbst = [sb("vbst%d" % i, [128, 2048], BF16) for i in range(2)]
            Bvb = [Buf() for _ in range(2)]

            def tm_job_vb(xt, Bxt, tt):
                for tb in range(4):
                    blk = tt * 4 + tb
                    s = blk % 2
                    for eg in range(4):
                        b = 2 + (eg % 2)
                        for k in range(4):
                            P.pe(lambda e, k=k, b=b, tb=tb, eg=eg: e.matmul(
                                banks[b][:], lhsT=xt[:, k, tb * 128:(tb + 1) * 128],
                                rhs=wuv[:, k, eg * 512:(eg + 1) * 512], start=(k == 0), stop=(k == 3)),
                                reads=[Bxt, Bwuv], writes=[BK[b]])
                        P.act(lambda e, s=s, b=b, eg=eg: e.activation(out=vbst[s][:, eg * 512:(eg + 1) * 512],
                                                                      in_=banks[b][:], func=AF.Copy),
                              reads=[BK[b]], writes=[Bvb[s]])
                    P.dma("sp", vb[blk * 128:(blk + 1) * 128, :], vbst[s][:], reads=[Bvb[s]])

            jobs = [fm_job(ws, w_uk, 4, h * 128, 128, kn_epi(h)) for h in range(16)]
            jobs.append(tm_job_vb)
            run_gemm(st, kvnT, 4, 8, jobs, xbufs=2)
            P.end_stage(touch)

        with ExitStack() as st:
            def sb(name, shape, dt):
                return st.enter_context(sbt(name, list(shape), dt))

            ws = WStream(st)
            ost = [sb("ost%d" % i, [128, 512], BF16) for i in range(3)]
            Bo = [Buf() for _ in range(3)]
            oi = [0]

            def copy_out(dst_fn, scale=1.0):
                def epi(tt, b):
                    s = oi[0] % 3
                    oi[0] += 1
                    P.act(lambda e, s=s, b=b: e.activation(out=ost[s][:], in_=banks[b][:], func=AF.Copy, scale=scale),
                          reads=[BK[b]], writes=[Bo[s]])
                    P.dma("sp", dst_fn(tt), ost[s][:], reads=[Bo[s]])
                return epi

            cq = sb("cq", [128, 8, 512], F32)
            sqb = sb("sqb", [128, 8, 512], BF16)
            rb = sb("rb", [128, 512], F32)
            cqn = sb("cqn", [128, 8, 512], BF16)
            Bc, Bsq, Brb, Bcqn = Buf(), Buf(), Buf(), Buf()
            cqnTr = cqnT.rearrange("(k p) t -> p k t", p=128)

            def cq_epi(j):
                def epi(tt, b):
                    P.dve(lambda e, b=b: e.tensor_copy(out=cq[:, j, :], in_=banks[b][:]), reads=[BK[b]], writes=[Bc])
                    P.act(lambda e, b=b: e.activation(out=sqb[:, j, :], in_=cq[:, j, :], func=AF.Square),
                          reads=[Bc], writes=[Bsq])
                    if j == 7:
                        for jj in range(8):
                            P.pe(lambda e, jj=jj: e.matmul(banks[4][:], lhsT=ones_bf[:], rhs=sqb[:, jj, :],
                                                           start=(jj == 0), stop=(jj == 7)),
                                 reads=[Bsq, B_const], writes=[BK[4]])
                        rsqrt_ops(rb[:], banks[4][:], 1.0 / 1024, [BK[4]], [Brb])
                        for jj in range(8):
                            P.dve(lambda e, jj=jj: e.scalar_tensor_tensor(
                                out=cqn[:, jj, :], in0=cq[:, jj, :], scalar=qg[:, jj:jj + 1], in1=rb[:],
                                op0=ALU.mult, op1=ALU.mult), reads=[Bc, Brb, B_const], writes=[Bcqn])
                        P.dma("sp", cqnTr[:, :, tt * 512:(tt + 1) * 512], cqn[:], reads=[Bcqn])
                return epi

            wist = sb("wist", [32, 512], F32)
            Bwi = Buf()
            IDX_SCALE = (128 ** -0.5) * (32 ** -0.5)

            def wi_epi(tt, b):
                P.act(lambda e, b=b: e.activation(out=wist[:], in_=banks[b][0:32, :], func=AF.Copy, scale=IDX_SCALE),
                      reads=[BK[b]], writes=[Bwi])
                dst = wi3[tt * 4 * 4096:(tt + 1) * 4 * 4096].rearrange("(bg f h) -> h bg f", h=32, f=4)
                P.dma("sp", dst, wist[:].rearrange("h (bg f) -> h bg f", f=4), reads=[Bwi], slow=True)

            jobs = []
            for h in range(16):
                jobs.append(fm_job(ws, w_in, 32, h * 128, 128,
                                   copy_out(lambda tt, h=h: qaT[h * 128:(h + 1) * 128, tt * 512:(tt + 1) * 512],
                                            scale=128 ** -0.5)))
            for h in range(32):
                jobs.append(fm_job(ws, w_in, 32, 2304 + h * 128, 128,
                                   copy_out(lambda tt, h=h: qiT[h * 128:(h + 1) * 128, tt * 512:(tt + 1) * 512])))
            jobs.append(fm_job(ws, w_in, 32, 6528, 32, wi_epi))
            for j in range(8):
                jobs.append(fm_job(ws, w_in, 32, 6560 + 128 * j, 128, cq_epi(j)))
            run_gemm(st, h1T, 32, 4, jobs, xbufs=2)
            P.end_stage(touch)

        with ExitStack() as st:
            def sb(name, shape, dt):
                return st.enter_context(sbt(name, list(shape), dt))

            QS = 192 ** -0.5
            wq = sb("wq", [128, 8, 3072], BF16)
            Bwq = Buf()
            wqr = w_uq.rearrange("(k p) e -> p k e", p=128)
            for k in range(8):
                P.dma("pool", wq[:, k, :], wqr[:, k, :], writes=[Bwq])
            ost = [sb("ost%d" % i, [128, 512], BF16) for i in range(3)]
            Bo = [Buf() for _ in range(3)]
            oi = [0]
            qrf = sb("qrf", [128, 8, 64], F32)
            qro = sb("qro", [128, 8, 64], F32)
            tmp8 = sb("tmp8", [128, 8, 32], F32)
            Bqr = Buf()
            qrst = sb("qrst", [64, 16, 512], BF16)
            Bqrst = Buf()
            wqrp = sb("wqrp", [128, 8, 16, 64], BF16)
            for k in range(8):
                P.dma("pool", wqrp[:, k, :, :], wqr[:, k, :].rearrange("p (h c) -> p h c", c=192)[:, :, 128:192],
                      writes=[Bwq])

            def qup_job(xt, Bxt, tt):
                for h in range(16):
                    b = gbank()
                    for k in range(8):
                        P.pe(lambda e, k=k, b=b, h=h: e.matmul(banks[b][:], lhsT=wq[:, k, h * 192:h * 192 + 128],
                                                               rhs=xt[:, k, :], start=(k == 0), stop=(k == 7)),
                             reads=[Bxt, Bwq], writes=[BK[b]])
                    s = oi[0] % 3
                    oi[0] += 1
                    P.act(lambda e, s=s, b=b: e.activation(out=ost[s][:], in_=banks[b][:], func=AF.Copy, scale=QS),
                          reads=[BK[b]], writes=[Bo[s]])
                    P.dma("sp", qnT[h * 128:(h + 1) * 128, tt * 512:(tt + 1) * 512], ost[s][:], reads=[Bo[s]])
                for tb in range(4):
                    blk = tt * 4 + tb
                    for hg in range(2):
                        b = 2 + hg
                        for k in range(8):
                            P.pe(lambda e, k=k, b=b, tb=tb, hg=hg: e.matmul(
                                banks[b][:],
                                lhsT=xt[:, k, tb * 128:(tb + 1) * 128], rhs=wqrp[:, k, hg * 8:(hg + 1) * 8, :],
                                start=(k == 0), stop=(k == 7)), reads=[Bxt, Bwq], writes=[BK[b]])
                        P.act(lambda e, b=b: e.activation(out=qrf[:].rearrange("p h c -> p (h c)"), in_=banks[b][:],
                                                          func=AF.Copy, scale=QS), reads=[BK[b]], writes=[Bqr])
                        rope_tok(([Bqr, B_const], [Bqr]), qrf[:], qro[:], blk, 8, tmp8[:])
                        for hh in range(8):
                            h = hg * 8 + hh
                            pb = 4 + (hh % 2)
                            P.pe(lambda e, hh=hh, pb=pb: e.transpose(banks[pb][0:64, 0:128], qro[:, hh, :], ident[:]),
                                 reads=[Bqr, B_const], writes=[BK[pb]])
                            P.act(lambda e, h=h, pb=pb, tb=tb: e.activation(
                                out=qrst[:, h, tb * 128:(tb + 1) * 128], in_=banks[pb][0:64, 0:128], func=AF.Copy),
                                reads=[BK[pb]], writes=[Bqrst])
                P.dma("sp", qrT.rearrange("(h c) t -> c h t", c=64)[:, :, tt * 512:(tt + 1) * 512], qrst[:],
                      reads=[Bqrst])

            run_gemm(st, cqnT, 8, 4, [qup_job], xbufs=2)
            P.end_stage(touch)

        slopes = [2.0 ** (-8.0 * (h + 1) / 16.0) for h in range(16)]

        with ExitStack() as st:
            def sb(name, shape, dt):
                return st.enter_context(sbt(name, list(shape), dt))

            kis = sb("kis", [128, 4096], BF16)
            kas = sb("kas", [128, 4096], BF16)
            vas = sb("vas", [128, 32, 128], BF16)
            Bk = Buf()
            P.dma("sp", kis[:], kiT, writes=[Bk])
            P.dma("sp", kas[:], kaT, writes=[Bk])
            P.dma("sp", vas[:], va.rearrange("(b p) d -> p b d", p=128), writes=[Bk])
            chkb = sb("chkb", [128, 4096], F32)
            posb = sb("posb", [128, 2048], F32)
            pb_i = sb("pb_i", [128, 4096], I32)
            Bpb = Buf()
            P.dma("sp", pb_i[:], posr.partition_broadcast(128)[:, 0, :], writes=[Bpb])
            P.dve(lambda e: e.tensor_copy(out=posb[:], in_=pb_i[:, 0:2048]), reads=[Bpb], writes=[Bk])
            P.dve(lambda e: e.tensor_scalar(out=pb_i[:], in0=pb_i[:], scalar1=6, scalar2=None,
                                            op0=ALU.arith_shift_right), reads=[Bpb, Bk], writes=[Bpb])
            P.dve(lambda e: e.tensor_copy(out=chkb[:], in_=pb_i[:]), reads=[Bpb], writes=[Bk])
            d4i = sb("d4i", [128, 4], I32)
            d4 = sb("d4", [128, 4], F32)
            d4b = sb("d4b", [128, 4], F32)
            P.pool(lambda e: e.iota(d4i[:], pattern=[[-32, 4]], base=0, channel_multiplier=1), writes=[Bk])
            P.dve(lambda e: e.tensor_copy(out=d4[:], in_=d4i[:]), reads=[Bk], writes=[Bk])
            P.dve(lambda e: e.tensor_scalar(out=d4b[:], in0=d4[:], scalar1=31.0, scalar2=None, op0=ALU.is_le),
                  reads=[Bk], writes=[Bk])
            P.dve(lambda e: e.tensor_scalar(out=d4[:], in0=d4[:], scalar1=0.0, scalar2=None, op0=ALU.is_ge),
                  reads=[Bk], writes=[Bk])
            P.dve(lambda e: e.tensor_tensor(out=d4[:], in0=d4[:], in1=d4b[:], op=ALU.mult), reads=[Bk], writes=[Bk])
            wbd = sb("wbd", [128, 32, 128], BF16)
            Bwbd = Buf()
            P.pool(lambda e: e.memset(wbd[:], 0.0), writes=[Bwbd])
            WT = sb("WT", [128, 32], F32)
            BWT = Buf()
            qib = [sb("qib%d" % i, [128, 128, 32], BF16) for i in range(2)]
            Bqi = [Buf() for _ in range(2)]
            qil = sb("qil", [128, 32, 128], BF16)
            Bqil = Buf()
            qab = [sb("qab%d" % i, [128, 16, 128], BF16) for i in range(2)]
            Bqa = [Buf() for _ in range(2)]
            Rt = [sb("Rt%d" % i, [128, 512], BF16) for i in range(4)]
            BR = [Buf() for _ in range(4)]
            Isc = sb("Isc", [128, 4096], F32)
            BI = Buf()
            pen = sb("pen", [128, 512], F32)
            Bpen = Buf()
            jk = sb("jk", [128, 4096], BF16)
            Bjk = Buf()
            sm = sb("sm", [128, 16], F32)
            Bsm = Buf()
            DmT = sb("DmT", [128, 32, 128], F32)
            BDm = Buf()
            mtmp = sb("mtmp", [128, 128], F32)
            Bmt = Buf()
            Zt = [sb("Zt%d" % i, [128, 512], F32) for i in range(2)]
            BZ = [[Buf() for _ in range(4)] for _ in range(2)]
            Pt = [sb("Pt%d" % i, [128, 512], BF16) for i in range(2)]
            BP = [Buf() for _ in range(2)]
            rden = sb("rden", [128, 512], F32)
            Brd = Buf()
            ostA = [sb("ostA%d" % i, [128, 4, 128], BF16) for i in range(2)]
            BoA = [Buf() for _ in range(2)]
            qiTr = qiT.rearrange("(h d) t -> d h t", d=128)
            qaTr = qaT.rearrange("(h d) t -> d h t", d=128)
            mixTr = mixT.rearrange("(h d) t -> d h t", d=128)
            ri_ = [0]
            for j in range(16):
                i = j // 4
                nkb = 4 * (i + 1)
                kbl = list(range(0, nkb)) + list(range(16, 16 + nkb))
                kgl = [kb for kb in kbl if kb % 4 == 0]
                nk = len(kbl) * 128
                qi_ = qib[j % 2]
                qa_ = qab[j % 2]
                P.dma("sp", qil[:], qiTr[:, :, j * 128:(j + 1) * 128], writes=[Bqil])
                P.pool(lambda e, qi_=qi_: e.tensor_copy(out=qi_[:], in_=qil[:].rearrange("p h t -> p t h")),
                       reads=[Bqil], writes=[Bqi[j % 2]])
                P.dma("sp", qa_[:], qaTr[:, :, j * 128:(j + 1) * 128], writes=[Bqa[j % 2]])
                P.dma("sp", WT[:], wi3[j * 4096:(j + 1) * 4096].rearrange("(g p) -> p g", p=128), writes=[BWT],
                      slow=True)
                for c4 in range(4):
                    dst = wbd[:].rearrange("p g c -> p (g c)")[:, c4:4096:132]
                    P.dve(lambda e, dst=dst, c4=c4: e.tensor_scalar(out=dst, in0=WT[:], scalar1=d4[:, c4:c4 + 1],
                                                                    scalar2=None, op0=ALU.mult),
                          reads=[BWT, Bk], writes=[Bwbd])
                for gi, kb0 in enumerate(kgl):
                    def emit_L(g, kb0=kb0, qi_=qi_):
                        lb = 2 + (g % 2)
                        P.pe(lambda e, g=g, lb=lb, kb0=kb0, qi_=qi_: e.matmul(
                            banks[lb][:], lhsT=qi_[:, 4 * g:4 * g + 4, :], rhs=kis[:, kb0 * 128:kb0 * 128 + 512],
                            start=True, stop=True), reads=[Bqi[j % 2], Bk], writes=[BK[lb]])
                    emit_L(0)
                    for g in range(32):
                        lb = 2 + (g % 2)
                        if g + 1 < 32:
                            emit_L(g + 1)
                        r = ri_[0] % 4
                        ri_[0] += 1
                        if r % 2 == 0:
                            P.act(lambda e, r=r, lb=lb: e.activation(out=Rt[r][:], in_=banks[lb][:], func=AF.Relu),
                                  reads=[BK[lb]], writes=[BR[r]])
                        else:
                            P.dve(lambda e, r=r, lb=lb: e.tensor_scalar(out=Rt[r][:], in0=banks[lb][:], scalar1=0.0,
                                                                        scalar2=None, op0=ALU.max),
                                  reads=[BK[lb]], writes=[BR[r]])
                        P.pe(lambda e, g=g, r=r: e.matmul(banks[4][:], lhsT=wbd[:, g, :], rhs=Rt[r][:],
                                                          start=(g == 0), stop=(g == 31)),
                             reads=[Bwbd, BR[r]], writes=[BK[4]])
                    P.dve(lambda e, kb0=kb0, j=j: e.tensor_scalar(
                        out=pen[:], in0=chkb[:, kb0 * 128:kb0 * 128 + 512], scalar1=chkf[:, j:j + 1], scalar2=-1e30,
                        op0=ALU.is_gt, op1=ALU.mult), reads=[Bk, B_const], writes=[Bpen])
                    P.dve(lambda e, gi=gi: e.tensor_tensor(out=Isc[:, gi * 512:(gi + 1) * 512], in0=banks[4][:],
                                                           in1=pen[:], op=ALU.add),
                          reads=[BK[4], Bpen], writes=[BI])
                Iv = Isc[:, 0:nk]
                P.dve(lambda e, Iv=Iv: e.reduce_max(out=sm[:, 0:1], in_=Iv, axis=AX.X), reads=[BI], writes=[Bsm])
                P.dve(lambda e: e.tensor_scalar(out=sm[:, 0:1], in0=sm[:, 0:1], scalar1=-7.5, scalar2=None,
                                                op0=ALU.add), reads=[Bsm], writes=[Bsm])
                wdt = 8.5
                for it in range(NBIS):
                    P.dve(lambda e, Iv=Iv, nk=nk: e.tensor_scalar(out=jk[:, 0:nk], in0=Iv, scalar1=sm[:, 0:1],
                                                                  scalar2=None, op0=ALU.is_ge, op1=ALU.add,
                                                                  accum_out=sm[:, 2:3]),
                          reads=[BI, Bsm], writes=[Bjk, Bsm])
                    nw = wdt * 0.5 if it < NBIS - 1 else wdt
                    mul = 2.0 * nw if it < NBIS - 1 else wdt
                    P.dve(lambda e, mul=mul: e.tensor_scalar(out=sm[:, 3:4], in0=sm[:, 2:3], scalar1=255.5, scalar2=mul,
                                                             op0=ALU.is_ge, op1=ALU.mult), reads=[Bsm], writes=[Bsm])
                    P.dve(lambda e, nw=nw: e.scalar_tensor_tensor(out=sm[:, 0:1], in0=sm[:, 0:1], scalar=-nw,
                                                                  in1=sm[:, 3:4], op0=ALU.add, op1=ALU.add),
                          reads=[Bsm], writes=[Bsm])
                    wdt = nw
                P.dve(lambda e, Iv=Iv, nk=nk: e.tensor_scalar(out=jk[:, 0:nk], in0=Iv, scalar1=-1e29, scalar2=None,
                                                              op0=ALU.is_ge, op1=ALU.add, accum_out=sm[:, 4:5]),
                      reads=[BI, Bsm], writes=[Bjk, Bsm])
                P.dve(lambda e: e.tensor_scalar(out=sm[:, 5:6], in0=sm[:, 4:5], scalar1=256.5, scalar2=None,
                                                op0=ALU.is_gt), reads=[Bsm], writes=[Bsm])
                P.dve(lambda e: e.tensor_tensor(out=sm[:, 6:7], in0=sm[:, 0:1], in1=sm[:, 5:6], op=ALU.mult),
                      reads=[Bsm], writes=[Bsm])
                P.dve(lambda e: e.tensor_scalar(out=sm[:, 7:8], in0=sm[:, 5:6], scalar1=-1.0, scalar2=1e29,
                                                op0=ALU.add, op1=ALU.mult), reads=[Bsm], writes=[Bsm])
                P.dve(lambda e: e.tensor_tensor(out=sm[:, 0:1], in0=sm[:, 6:7], in1=sm[:, 7:8], op=ALU.add),
                      reads=[Bsm], writes=[Bsm])
                P.dve(lambda e, Iv=Iv: e.tensor_scalar(out=Iv, in0=Iv, scalar1=sm[:, 0:1], scalar2=None, op0=ALU.is_ge),
                      reads=[BI, Bsm], writes=[BI])
                for kbi, kb in enumerate(kbl):
                    tbk = 5
                    P.pe(lambda e, kbi=kbi: e.transpose(banks[5][:, 0:128], Isc[:, kbi * 128:(kbi + 1) * 128], ident[:]),
                         reads=[BI, B_const], writes=[BK[5]])
                    P.dve(lambda e: e.tensor_scalar(out=mtmp[:], in0=banks[5][:, 0:128], scalar1=-1e6, scalar2=1e6,
                                                    op0=ALU.mult, op1=ALU.add), reads=[BK[5]], writes=[Bmt])
                    P.dve(lambda e, kbi=kbi, kb=kb, j=j: e.tensor_scalar(
                        out=DmT[:, kbi, :], in0=posb[:, j * 128:(j + 1) * 128], scalar1=posf[:, kb:kb + 1], scalar2=None,
                        op0=ALU.subtract), reads=[Bk, B_const], writes=[BDm])
                    P.dve(lambda e, kbi=kbi: e.scalar_tensor_tensor(
                        out=DmT[:, kbi, :], in0=DmT[:, kbi, :], scalar=-1.0, in1=DmT[:, kbi, :], op0=ALU.mult,
                        op1=ALU.max), reads=[BDm], writes=[BDm])
                    P.dve(lambda e, kbi=kbi: e.tensor_tensor(out=DmT[:, kbi, :], in0=DmT[:, kbi, :], in1=mtmp[:],
                                                             op=ALU.add), reads=[BDm, Bmt], writes=[BDm])
                for hg in range(4):
                    ob, db = 6, 7
                    def emit_S(kbi, hg=hg, qa_=qa_, kbl=kbl):
                        sbk = kbi % 2
                        kb = kbl[kbi]
                        P.pe(lambda e, sbk=sbk, kb=kb, hg=hg, qa_=qa_: e.matmul(
                            banks[sbk][:], lhsT=kas[:, kb * 128:(kb + 1) * 128],
                            rhs=qa_[:, 4 * hg:4 * hg + 4, :], start=True, stop=True),
                            reads=[Bk, Bqa[j % 2]], writes=[BK[sbk]])
                    emit_S(0)
                    for kbi, kb in enumerate(kbl):
                        sbk = kbi % 2
                        if kbi + 1 < len(kbl):
                            emit_S(kbi + 1)
                        z = Zt[kbi % 2]
                        for hh in range(4):
                            h = 4 * hg + hh
                            P.dve(lambda e, z=z, hh=hh, h=h, kbi=kbi, sbk=sbk: e.scalar_tensor_tensor(
                                out=z[:, hh * 128:(hh + 1) * 128], in0=DmT[:, kbi, :], scalar=-slopes[h],
                                in1=banks[sbk][:, hh * 128:(hh + 1) * 128], op0=ALU.mult, op1=ALU.add),
                                reads=[BDm, BK[sbk]], writes=[BZ[kbi % 2][hh]])
                        p_ = Pt[kbi % 2]
                        P.act(lambda e, z=z, p_=p_: e.activation(out=p_[:], in_=z[:], func=AF.Exp),
                              reads=BZ[kbi % 2], writes=[BP[kbi % 2]])
                        P.pe(lambda e, p_=p_, kb=kb, kbi=kbi, nl=len(kbl): e.matmul(banks[ob][:], lhsT=vas[:, kb, :], rhs=p_[:],
                                                                       start=(kbi == 0), stop=(kbi == nl - 1)),
                             reads=[Bk, BP[kbi % 2]], writes=[BK[ob]])
                        P.pe(lambda e, p_=p_, kbi=kbi, nl=len(kbl): e.matmul(banks[db][:], lhsT=ones_bf[:], rhs=p_[:],
                                                                start=(kbi == 0), stop=(kbi == nl - 1)),
                             reads=[B_const, BP[kbi % 2]], writes=[BK[db]])
                    P.dve(lambda e: e.reciprocal(out=rden[:], in_=banks[db][:]), reads=[BK[db]], writes=[Brd])
                    oa = ostA[hg % 2]
                    P.dve(lambda e, oa=oa: e.tensor_tensor(out=oa[:].rearrange("p h t -> p (h t)"), in0=banks[ob][:],
                                                           in1=rden[:], op=ALU.mult),
                          reads=[BK[ob], Brd], writes=[BoA[hg % 2]])
                    P.dma("pool", mixTr[:, 4 * hg:4 * hg + 4, j * 128:(j + 1) * 128], oa[:], reads=[BoA[hg % 2]])
            P.end_stage(touch)

        with ExitStack() as st:
            def sb(name, shape, dt):
                return st.enter_context(sbt(name, list(shape), dt))

            krs = sb("krs", [64, 4096], BF16)
            Bk = Buf()
            P.dma("sp", krs[:], krT, writes=[Bk])
            ctb = sb("ctb", [128, 2048], F32)
            pb_i = sb("pb_i", [128, 2048], I32)
            P.dma("sp", pb_i[:], posr[:, 0:2048].partition_broadcast(128)[:, 0, :], writes=[Bk])
            P.dve(lambda e: e.tensor_scalar(out=pb_i[:], in0=pb_i[:], scalar1=6, scalar2=None,
                                            op0=ALU.arith_shift_right), reads=[Bk], writes=[Bk])
            P.dve(lambda e: e.tensor_copy(out=ctb[:], in_=pb_i[:]), reads=[Bk], writes=[Bk])
            cm = sb("cm", [128, 8, 512], BF16)
            Bcm = Buf()
            kn = [sb("kn%d" % i, [128, 4096], BF16) for i in range(2)]
            vbh = [sb("vbh%d" % i, [128, 32, 128], BF16) for i in range(2)]
            Bkn = [Buf() for _ in range(2)]
            qn = [sb("qn%d" % i, [128, 512], BF16) for i in range(2)]
            qr = [sb("qr%d" % i, [64, 512], BF16) for i in range(2)]
            Bq = [Buf() for _ in range(2)]
            Pt = [sb("PtB%d" % i, [128, 512], BF16) for i in range(3)]
            BP = [Buf() for _ in range(3)]
            rden = sb("rdenB", [128, 512], F32)
            Brd = Buf()
            ostB = [sb("ostB%d" % i, [128, 512], BF16) for i in range(2)]
            BoB = [Buf() for _ in range(2)]
            vbr = vb.rearrange("(b p) e -> p b e", p=128)
            hi_ = 0
            pi_ = 0
            for i in range(4):
                nkb = 4 * (i + 1)
                kbl = list(range(0, nkb)) + list(range(16, 16 + nkb))
                band = list(range(4 * i, 4 * i + 4)) + list(range(16 + 4 * i, 16 + 4 * i + 4))
                for bi_, kb in enumerate(band):
                    P.dve(lambda e, bi_=bi_, kb=kb, i=i: e.tensor_scalar(
                        out=cm[:, bi_, :], in0=ctb[:, i * 512:(i + 1) * 512], scalar1=chkf[:, kb:kb + 1], scalar2=None,
                        op0=ALU.is_ge), reads=[Bk, B_const], writes=[Bcm])
                for h in range(16):
                    s = hi_ % 2
                    hi_ += 1
                    P.dma("sp", kn[s][:, 0:nkb * 128], knT[h * 128:(h + 1) * 128, 0:nkb * 128], writes=[Bkn[s]])
                    P.dma("sp", kn[s][:, 2048:2048 + nkb * 128], knT[h * 128:(h + 1) * 128, 2048:2048 + nkb * 128],
                          writes=[Bkn[s]])
                    P.dma("sp", vbh[s][:, 0:nkb, :], vbr[:, 0:nkb, h * 128:(h + 1) * 128], writes=[Bkn[s]])
                    P.dma("sp", vbh[s][:, 16:16 + nkb, :], vbr[:, 16:16 + nkb, h * 128:(h + 1) * 128], writes=[Bkn[s]])
                    P.dma("sp", qn[s][:], qnT[h * 128:(h + 1) * 128, i * 512:(i + 1) * 512], writes=[Bq[s]])
                    P.dma("sp", qr[s][:], qrT[h * 64:(h + 1) * 64, i * 512:(i + 1) * 512], writes=[Bq[s]])
                    ob = 4 + (h % 2)
                    db = 6 + (h % 2)
                    def emit_QK(kbi, s=s, kbl=kbl):
                        sbk = kbi % 2
                        kb = kbl[kbi]
                        P.pe(lambda e, sbk=sbk, kb=kb, s=s: e.matmul(banks[sbk][:], lhsT=kn[s][:, kb * 128:(kb + 1) * 128],
                                                                     rhs=qn[s][:], start=True, stop=False),
                             reads=[Bkn[s], Bq[s]], writes=[BK[sbk]])
                        P.pe(lambda e, sbk=sbk, kb=kb, s=s: e.matmul(banks[sbk][:], lhsT=krs[:, kb * 128:(kb + 1) * 128],
                                                                     rhs=qr[s][:], start=False, stop=True),
                             reads=[Bk, Bq[s]], writes=[BK[sbk]])
                    emit_QK(0)
                    for kbi, kb in enumerate(kbl):
                        sbk = kbi % 2
                        if kbi + 1 < len(kbl):
                            emit_QK(kbi + 1)
                        pp = pi_ % 3
                        pi_ += 1
                        p_ = Pt[pp]
                        P.act(lambda e, p_=p_, sbk=sbk: e.activation(out=p_[:], in_=banks[sbk][:], func=AF.Exp),
                              reads=[BK[sbk]], writes=[BP[pp]])
                        if kb in band:
                            bi_ = band.index(kb)
                            P.dve(lambda e, p_=p_, bi_=bi_: e.tensor_tensor(out=p_[:], in0=p_[:], in1=cm[:, bi_, :],
                                                                            op=ALU.mult),
                                  reads=[BP[pp], Bcm], writes=[BP[pp]])
                        P.pe(lambda e, p_=p_, kb=kb, kbi=kbi, s=s, ob=ob, nl=len(kbl): e.matmul(
                            banks[ob][:], lhsT=vbh[s][:, kb, :], rhs=p_[:], start=(kbi == 0),
                            stop=(kbi == nl - 1)), reads=[Bkn[s], BP[pp]], writes=[BK[ob]])
                        P.pe(lambda e, p_=p_, kbi=kbi, db=db, nl=len(kbl): e.matmul(
                            banks[db][:], lhsT=ones_bf[:], rhs=p_[:], start=(kbi == 0), stop=(kbi == nl - 1)),
                            reads=[B_const, BP[pp]], writes=[BK[db]])
                    P.dve(lambda e, db=db: e.reciprocal(out=rden[:], in_=banks[db][:]), reads=[BK[db]], writes=[Brd])
                    o_ = ostB[h % 2]
                    P.dve(lambda e, o_=o_, ob=ob: e.tensor_tensor(out=o_[:], in0=banks[ob][:], in1=rden[:], op=ALU.mult),
                          reads=[BK[ob], Brd], writes=[BoB[h % 2]])
                    P.dma("pool", mixT[(16 + h) * 128:(17 + h) * 128, i * 512:(i + 1) * 512], o_[:], reads=[BoB[h % 2]])
            P.end_stage(touch)

        with ExitStack() as st:
            def sb(name, shape, dt):
                return st.enter_context(sbt(name, list(shape), dt))

            ws = WStream(st)
            xch = [sb("xch%d" % i, [128, 512], F32) for i in range(2)]
            Bxc = [Buf() for _ in range(2)]
            sq = [sb("sq%d" % i, [128, 512], BF16) for i in range(2)]
            Bsq = [Buf() for _ in range(2)]
            ci = [0]

            def wo_epi(ec):
                def epi(tt, b):
                    s = ci[0] % 2
                    ci[0] += 1
                    P.dma("sp", xch[s][:], xT[ec * 128:(ec + 1) * 128, tt * 512:(tt + 1) * 512], writes=[Bxc[s]])
                    P.dve(lambda e, s=s, b=b: e.scalar_tensor_tensor(
                        out=xch[s][:], in0=banks[b][:], scalar=gate1[:, ec:ec + 1], in1=xch[s][:], op0=ALU.mult,
                        op1=ALU.add), reads=[BK[b], B_mod, Bxc[s]], writes=[Bxc[s]])
                    P.dma("sp", x2T[ec * 128:(ec + 1) * 128, tt * 512:(tt + 1) * 512], xch[s][:], reads=[Bxc[s]])
                    P.act(lambda e, s=s: e.activation(out=sq[s][:], in_=xch[s][:], func=AF.Square), reads=[Bxc[s]],
                          writes=[Bsq[s]])
                    sbk_ = 4 + (tt % 2)
                    P.pe(lambda e, s=s, sbk_=sbk_: e.matmul(banks[sbk_][:], lhsT=ones_bf[:], rhs=sq[s][:], start=(ec == 0),
                                                            stop=(ec == 31)), reads=[B_const, Bsq[s]], writes=[BK[sbk_]])
                    if ec == 31:
                        rsqrt_ops(r2b[:, tt * 512:(tt + 1) * 512], banks[sbk_][:], 1.0 / D, [BK[sbk_]], [B_r2b])
                return epi

            jobs = [fm_job_pair(ws, w_o, ec * 128, 128, wo_epi(ec)) for ec in range(32)]
            run_gemm_pair(st, mixT, 32, 4, jobs)
            P.end_stage(touch)

        with ExitStack() as st:
            def sb(name, shape, dt):
                return st.enter_context(sbt(name, list(shape), dt))

            xch = [sb("xch%d" % i, [128, 512], F32) for i in range(3)]
            Bxc = [Buf() for _ in range(3)]
            hch = [sb("hch%d" % i, [128, 512], BF16) for i in range(3)]
            Bhc = [Buf() for _ in range(3)]
            n = 0
            for tt in range(4):
                for k in range(32):
                    s = n % 3
                    n += 1
                    P.dma("sp", xch[s][:], x2T[k * 128:(k + 1) * 128, tt * 512:(tt + 1) * 512], writes=[Bxc[s]])
                    P.dve(lambda e, s=s, k=k, tt=tt: e.scalar_tensor_tensor(
                        out=xch[s][:], in0=xch[s][:], scalar=gs2[:, k:k + 1], in1=r2b[:, tt * 512:(tt + 1) * 512],
                        op0=ALU.mult, op1=ALU.mult), reads=[Bxc[s], B_mod, B_r2b], writes=[Bxc[s]])
                    P.act(lambda e, s=s, k=k: e.activation(out=hch[s][:], in_=xch[s][:], func=AF.Identity,
                                                           bias=sh2[:, k:k + 1], scale=1.0),
                          reads=[Bxc[s], B_mod], writes=[Bhc[s]])
                    P.dma("sp", h2T[k * 128:(k + 1) * 128, tt * 512:(tt + 1) * 512], hch[s][:], reads=[Bhc[s]])
            P.end_stage(touch)

        with ExitStack() as st:
            def sb(name, shape, dt):
                return st.enter_context(sbt(name, list(shape), dt))

            ws = WStream(st)
            u = [sb("u%d" % i, [128, 512], BF16) for i in range(3)]
            Bu = [Buf() for _ in range(3)]
            ci = [0]

            def m1_epi(fc):
                def epi(tt, b):
                    s = ci[0] % 3
                    ci[0] += 1
                    P.act(lambda e, s=s, b=b: e.activation(out=u[s][:], in_=banks[b][:], func=AF.Relu),
                          reads=[BK[b]], writes=[Bu[s]])
                    P.dve(lambda e, s=s: e.tensor_tensor(out=u[s][:], in0=u[s][:], in1=u[s][:], op=ALU.mult),
                          reads=[Bu[s]], writes=[Bu[s]])
                    P.dma("sp", hidT[fc * 128:(fc + 1) * 128, tt * 512:(tt + 1) * 512], u[s][:], reads=[Bu[s]])
                return epi

            jobs = [fm_job_pair(ws, w1, fc * 128, 128, m1_epi(fc)) for fc in range(128)]
            run_gemm_pair(st, h2T, 32, 4, jobs)
            P.end_stage(touch)

        with ExitStack() as st:
            def sb(name, shape, dt):
                return st.enter_context(sbt(name, list(shape), dt))

            ws = WStream(st)
            xch = [sb("xch%d" % i, [128, 512], F32) for i in range(2)]
            Bxc = [Buf() for _ in range(2)]
            sq = [sb("sq%d" % i, [128, 512], BF16) for i in range(2)]
            Bsq = [Buf() for _ in range(2)]
            sscol = sb("sscol", [128, 16], F32)
            Bss = Buf()
            P.pool(lambda e: e.memset(sscol[:], 0.0), writes=[Bss])
            ci = [0]

            def m2_epi(ec):
                def epi(tt, b):
                    s = ci[0] % 2
                    ci[0] += 1
                    P.dma("sp", xch[s][:], x2T[ec * 128:(ec + 1) * 128, tt * 512:(tt + 1) * 512], writes=[Bxc[s]])
                    P.dve(lambda e, s=s, b=b: e.scalar_tensor_tensor(
                        out=xch[s][:], in0=banks[b][:], scalar=gate2[:, ec:ec + 1], in1=xch[s][:], op0=ALU.mult,
                        op1=ALU.add), reads=[BK[b], B_mod, Bxc[s]], writes=[Bxc[s]])
                    P.act(lambda e, s=s: e.activation(out=sq[s][:], in_=xch[s][:], func=AF.Square), reads=[Bxc[s]],
                          writes=[Bsq[s]])
                    for tb in range(4):
                        P.pe(lambda e, s=s, tb=tb: e.matmul(banks[4][:, tb:tb + 1], lhsT=sq[s][:, tb * 128:(tb + 1) * 128],
                                                            rhs=ones_bf[:, 0:1], start=True, stop=True),
                             reads=[B_const, Bsq[s]], writes=[BK[4]])
                    P.dve(lambda e, tt=tt: e.tensor_tensor(out=sscol[:, tt * 4:tt * 4 + 4], in0=sscol[:, tt * 4:tt * 4 + 4],
                                                           in1=banks[4][:, 0:4], op=ALU.add),
                          reads=[BK[4], Bss], writes=[Bss])
                    P.dve(lambda e, s=s: e.tensor_scalar(out=xch[s][:], in0=xch[s][:], scalar1=fg[:, ec:ec + 1],
                                                         scalar2=None, op0=ALU.mult),
                          reads=[Bxc[s], Bsq[s], B_const], writes=[Bxc[s]])
                    P.dma("sp", x3T[ec * 128:(ec + 1) * 128, tt * 512:(tt + 1) * 512], xch[s][:], reads=[Bxc[s]])
                    if ec == 31 and tt == 3:
                        rsqrt_ops(rstd3[:], sscol[:], 1.0 / D, [Bss], [B_r3])
                return epi

            jobs = [fm_job(ws, w2, 128, ec * 128, 128, m2_epi(ec)) for ec in range(32)]
            run_gemm(st, hidT, 128, 4, jobs, xbufs=1)
            P.end_stage(touch)

        with ExitStack() as st:
            def sb(name, shape, dt):
                return st.enter_context(sbt(name, list(shape), dt))

            x3s = [sb("x3s%d" % i, [128, 32, 128], F32) for i in range(2)]
            Bx3 = [Buf() for _ in range(2)]
            yst = [sb("yst%d" % i, [128, 4096], F32) for i in range(2)]
            By = [Buf() for _ in range(2)]
            x3Tr = x3T.rearrange("(k p) t -> p k t", p=128)
            for blk in range(16):
                s = blk % 2
                P.dma("sp", x3s[s][:], x3Tr[:, :, blk * 128:(blk + 1) * 128], writes=[Bx3[s]])
                for k in range(32):
                    b = (k // 4) % 2
                    P.pe(lambda e, s=s, k=k, b=b: e.transpose(banks[b][:, (k % 4) * 128:(k % 4 + 1) * 128], x3s[s][:, k, :],
                                                              ident[:]), reads=[Bx3[s], B_const], writes=[BK[b]])
                    if k % 4 == 3:
                        P.act(lambda e, s=s, k=k, b=b, blk=blk: e.activation(
                            out=yst[s][:, (k - 3) * 128:(k + 1) * 128], in_=banks[b][:], func=AF.Identity,
                            scale=rstd3[:, blk:blk + 1]), reads=[BK[b], B_r3], writes=[By[s]])
                P.dma("sp", y[blk * 128:(blk + 1) * 128, :], yst[s][:], reads=[By[s]])
            P.end_stage(touch)
    return nc


_NC_CACHE = {}


def kernel(x, c, positions, w_ada, b_ada, ln1_g, w_in, q_norm_g, kv_norm_g, w_uq, w_uk, w_uv, w_o, ln2_g,
           w_mlp_in, w_mlp_out, final_g):
    x = np.asarray(x)
    positions = np.asarray(positions)
    B, S, _ = x.shape

    def colT(v, k):
        return np.ascontiguousarray(np.asarray(v, dtype=np.float32).reshape(k, 128).T)

    shared = {
        "w_ada": np.ascontiguousarray(np.asarray(w_ada)[0]), "b_adaT": colT(np.asarray(b_ada)[0], 192),
        "g1T": colT(np.asarray(ln1_g)[0], 32), "w_in": np.ascontiguousarray(np.asarray(w_in)[0]),
        "qgT": colT(np.asarray(q_norm_g)[0], 8), "kvgT": colT(np.asarray(kv_norm_g)[0], 4),
        "w_uq": np.ascontiguousarray(np.asarray(w_uq)[0]), "w_uk": np.ascontiguousarray(np.asarray(w_uk)[0]),
        "w_uv": np.ascontiguousarray(np.asarray(w_uv)[0]), "w_o": np.ascontiguousarray(np.asarray(w_o)[0]),
        "g2T": colT(np.asarray(ln2_g)[0], 32), "w1": np.ascontiguousarray(np.asarray(w_mlp_in)[0]),
        "w2": np.ascontiguousarray(np.asarray(w_mlp_out)[0]), "fgT": colT(np.asarray(final_g), 32),
    }
    in_maps = []
    perms = []
    for core in range(8):
        b, p = core // 2, core % 2
        own = [2 * j + p for j in range(16)]
        oth = [2 * j + 1 - p for j in range(16)]
        blocks = own + oth
        idx = np.concatenate([np.arange(g * 128, (g + 1) * 128) for g in blocks])
        perms.append((b, own))
        m = dict(shared)
        m["xc"] = np.ascontiguousarray(x[b][idx])
        pp = np.ascontiguousarray(positions[b][idx].astype(np.int32))
        m["posr"] = pp.reshape(1, 4096)
        m["posc"] = np.ascontiguousarray(pp.reshape(32, 128).T)
        m["cT"] = colT(np.asarray(c)[b], 32)
        in_maps.append(m)
    if "nc" not in _NC_CACHE:
        _NC_CACHE["nc"] = build_nc()
    res = run_bass_kernel_spmd(_NC_CACHE["nc"], in_maps, core_ids=list(range(8)))
    out = np.empty((B, S, D), dtype=np.float32)
    for core in range(8):
        b, own = perms[core]
        yv = np.asarray(res.results[core]["y"]).reshape(16, 128, D)
        for j, g in enumerate(own):
            out[b, g * 128:(g + 1) * 128, :] = yv[j]
    return out
```
